# Optimizing a Trainium2 kernel written in Bass

```python
import math
import numpy as np
import jax, jax.numpy as jnp
from jax import lax

D_MODEL = 1024
BATCH = 8
SEQ = 2048
DEPTH = 2

N_META = 16
EPS = 1e-6
A_HEADS = 4
A_HEAD_DIM = 64
A_WIDTH = A_HEADS * A_HEAD_DIM
A_DECAY_RANK = 64
A_ICLR_RANK = 64
A_GATE_RANK = 128
A_GN_EPS = 64e-5
A_DECAY_SCALE = 0.6065306597126334
A_COLS = (A_WIDTH, A_WIDTH, A_WIDTH, A_DECAY_RANK, A_DECAY_RANK, A_ICLR_RANK, A_ICLR_RANK, A_GATE_RANK)
A_TOTAL = 3 * A_WIDTH + 2 * A_DECAY_RANK + 2 * A_ICLR_RANK + A_GATE_RANK
B_HEADS = 4
B_QK_DIM = 64
B_V_DIM = 2 * B_QK_DIM
B_WIDTH = B_HEADS * B_V_DIM
B_COLS = (B_HEADS * 2 * B_QK_DIM, B_HEADS * 2 * B_QK_DIM, B_WIDTH)
B_TOTAL = 2 * B_HEADS * 2 * B_QK_DIM + B_WIDTH
Q_BLOCK = 128
ROPE_THETA = 10000.0
C_HEADS = 4
C_DK = 32
C_DV = 64
C_WIDTH = C_HEADS * C_DV
C_GATE_RANK = 16
C_GATE_TEMP = 16.0
C_CHUNK = 64
C_COLS = (C_HEADS * C_DK, C_HEADS * C_DK, C_WIDTH, C_WIDTH, C_GATE_RANK, C_GATE_RANK)
C_TOTAL = 2 * C_HEADS * C_DK + 2 * C_WIDTH + 2 * C_GATE_RANK
IN_COLS = A_TOTAL + B_TOTAL + C_TOTAL
MIX_WIDTH = A_WIDTH + B_WIDTH + C_WIDTH
N_EXPERTS = 16
EC_FACTOR = 2
EXPERT_FF = 1408

kernel_name = "hybrid_rwkv7_diffattn_gla_ec_moe_encoder"


def _split(x, sizes):
    idx = np.cumsum(sizes)[:-1].tolist()
    return jnp.split(x, idx, axis=-1)


def rms_norm(x, g, eps=EPS):
    xf = x.astype(jnp.float32)
    y = xf * lax.rsqrt(jnp.mean(xf * xf, axis=-1, keepdims=True) + eps)
    return (y * g.astype(jnp.float32)).astype(x.dtype)


def rotary(x, pos):
    half = x.shape[-1] // 2
    inv = ROPE_THETA ** (-jnp.arange(half, dtype=jnp.float32) / half)
    ang = pos[:, None] * inv[None, :]
    cos, sin = jnp.cos(ang), jnp.sin(ang)
    xf = x.astype(jnp.float32)
    x1, x2 = xf[..., :half], xf[..., half:]
    return jnp.concatenate([x1 * cos - x2 * sin, x2 * cos + x1 * sin], axis=-1).astype(x.dtype)


def centred_shift(p, mu):
    prev = jnp.pad(p, ((0, 0), (1, 0), (0, 0)))[:, :-1]
    nxt = jnp.pad(p, ((0, 0), (0, 1), (0, 0)))[:, 1:]
    return p + mu[0] * (prev - p) + mu[1] * (nxt - p)


def rwkv7_group(p, shift_mu, w0, w_up, a0, a_up, g_up, k_k, k_a, r_k, gn_g, gn_b):
    B, L, _ = p.shape
    f32 = jnp.float32
    p = centred_shift(p, shift_mu)
    r, k, v, wd_f, wd_b, ad_f, ad_b, gd = _split(p, A_COLS)
    heads = lambda t: t.reshape(t.shape[:-1] + (A_HEADS, A_HEAD_DIM))
    wd = jnp.stack([wd_f, wd_b])
    ad = jnp.stack([ad_f, ad_b])
    z_w = w0[:, None, None] + jnp.einsum("dblr,drc->dblc", jnp.tanh(wd), w_up)
    decay = jnp.exp(-A_DECAY_SCALE * jax.nn.sigmoid(z_w.astype(f32)))
    iclr = jax.nn.sigmoid((a0[:, None, None] + jnp.einsum("dblr,drc->dblc", ad, a_up)).astype(f32))
    gate = jnp.einsum("blr,rc->blc", jax.nn.sigmoid(gd), g_up)
    kk = heads((k * k_k).astype(f32))
    kk = kk / jnp.maximum(jnp.sqrt(jnp.sum(kk * kk, axis=-1, keepdims=True)), 1e-12)
    k_dir = heads(k.astype(f32)[None] * (1.0 + (iclr - 1.0) * k_a.astype(f32)))
    r_h, v_h = heads(r.astype(f32)), heads(v.astype(f32))
    both = lambda t: jnp.broadcast_to(t[None], (2,) + t.shape)

    def to_scan(t):
        t = jnp.stack([t[0], t[1][:, ::-1]])
        return jnp.moveaxis(t, 2, 0)

    xs = tuple(to_scan(t) for t in (both(r_h), heads(decay), k_dir, both(v_h), both(kk), both(kk) * heads(iclr)))

    def step(S, inp):
        r_t, w_t, k_t, v_t, kk_t, ka_t = inp
        sa = jnp.einsum("dbhvk,dbhk->dbhv", S, kk_t)
        S = S * w_t[..., None, :] - sa[..., :, None] * ka_t[..., None, :] + v_t[..., :, None] * k_t[..., None, :]
        return S, jnp.einsum("dbhvk,dbhk->dbhv", S, r_t)

    S0 = jnp.zeros((2, B, A_HEADS, A_HEAD_DIM, A_HEAD_DIM), f32)
    _, y = lax.scan(step, S0, xs)
    y = jnp.moveaxis(y, 0, 2)
    o = y[0] + y[1][:, ::-1]
    mu = jnp.mean(o, axis=-1, keepdims=True)
    var = jnp.mean(jnp.square(o - mu), axis=-1, keepdims=True)
    o = ((o - mu) * lax.rsqrt(var + A_GN_EPS)).reshape(B, L, A_WIDTH) * gn_g.astype(f32) + gn_b.astype(f32)
    bonus = jnp.sum(r_h[None] * k_dir * heads(r_k.astype(f32)), axis=-1, keepdims=True) * v_h[None]
    o = o + jnp.sum(bonus, axis=0).reshape(B, L, A_WIDTH)
    return (o * gate.astype(f32)).astype(p.dtype)


def diff_attention_group(p, pos, lam_p, subln_g, lam_init):
    B, L, _ = p.shape
    f32 = jnp.float32
    q, k, v = _split(p, B_COLS)
    q = rotary(q.reshape(B, L, B_HEADS, 2, B_QK_DIM).transpose(0, 2, 3, 1, 4), pos) * (B_QK_DIM ** -0.5)
    k = rotary(k.reshape(B, L, B_HEADS, 2, B_QK_DIM).transpose(0, 2, 3, 1, 4), pos)
    v = v.reshape(B, L, B_HEADS, B_V_DIM).transpose(0, 2, 1, 3)
    lam_p = lam_p.astype(f32)
    lam = jnp.exp(jnp.sum(lam_p[0] * lam_p[1])) - jnp.exp(jnp.sum(lam_p[2] * lam_p[3])) + lam_init

    def attend(qb):
        s = jnp.einsum("bhcqd,bhckd->bhcqk", qb, k).astype(f32)
        pr = jax.nn.softmax(s, axis=-1)
        w = pr[:, :, 0] - lam * pr[:, :, 1]
        return jnp.einsum("bhqk,bhkv->bhqv", w.astype(v.dtype), v)

    o_meta = attend(q[:, :, :, :N_META])
    n_blk = (L - N_META) // Q_BLOCK
    q_real = jnp.moveaxis(q[:, :, :, N_META:].reshape(B, B_HEADS, 2, n_blk, Q_BLOCK, B_QK_DIM), 3, 0)
    o_real = lax.map(attend, q_real)
    o_real = jnp.moveaxis(o_real, 0, 2).reshape(B, B_HEADS, n_blk * Q_BLOCK, B_V_DIM)
    o = jnp.concatenate([o_meta, o_real], axis=2)
    o = rms_norm(o, subln_g) * (1.0 - lam_init)
    return o.transpose(0, 2, 1, 3).reshape(B, L, B_WIDTH).astype(p.dtype)


def gla_chunked(q, k, v, log_a):
    B, H, T, dk = q.shape
    dv = v.shape[-1]
    n = T // C_CHUNK
    f32 = jnp.float32
    ch = lambda t: t.astype(f32).reshape(B, H, n, C_CHUNK, t.shape[-1])
    q, k, v, g = ch(q), ch(k), ch(v), ch(log_a)
    b = jnp.cumsum(g, axis=3)
    b_last = b[:, :, :, -1:, :]
    q_dec = q * jnp.exp(b)
    k_inv = k * jnp.exp(-b)
    k_end = k * jnp.exp(b_last - b)
    mask = jnp.tril(jnp.ones((C_CHUNK, C_CHUNK), dtype=bool))
    att = jnp.where(mask, jnp.einsum("bhntd,bhnsd->bhnts", q_dec, k_inv), 0.0)
    o = jnp.einsum("bhnts,bhnsv->bhntv", att, v)
    kv = jnp.einsum("bhnsd,bhnsv->bhndv", k_end, v)
    dec = jnp.exp(b_last[:, :, :, 0])

    def step(S, inp):
        dec_n, kv_n = inp
        return dec_n[..., None] * S + kv_n, S

    _, S_prev = lax.scan(step, jnp.zeros((B, H, dk, dv), f32), (jnp.moveaxis(dec, 2, 0), jnp.moveaxis(kv, 2, 0)))
    S_prev = jnp.moveaxis(S_prev, 0, 2)
    o = o + jnp.einsum("bhntd,bhndv->bhntv", q_dec, S_prev)
    return o.reshape(B, H, T, dv)


def gla_group(p, gate_up, gate_b, norm_g):
    B, L, _ = p.shape
    q, k, v, g, ad_f, ad_b = _split(p, C_COLS)
    heads_t = lambda t, d: t.reshape(B, L, C_HEADS, d).transpose(0, 2, 1, 3)
    q = heads_t(q, C_DK) * (C_DK ** -0.5)
    k = heads_t(k, C_DK)
    v = heads_t(v, C_DV)
    ad = jnp.stack([ad_f, ad_b])
    z = jnp.einsum("dblr,drc->dblc", ad, gate_up) + gate_b[:, None, None]
    log_a = jax.nn.log_sigmoid(z.astype(jnp.float32)) / C_GATE_TEMP
    la_f, la_b = heads_t(log_a[0], C_DK), heads_t(log_a[1], C_DK)
    pad = C_CHUNK - N_META
    padt = lambda t: jnp.pad(t, ((0, 0), (0, 0), (pad, 0), (0, 0)))
    flip = lambda t: t[:, :, ::-1]
    qp, kp, vp = padt(q), padt(k), padt(v)
    o_f = gla_chunked(qp, kp, vp, padt(la_f))
    o_b = flip(gla_chunked(flip(qp), flip(kp), flip(vp), flip(padt(la_b))))
    o = (o_f + o_b)[:, :, pad:]
    o = rms_norm(o.transpose(0, 2, 1, 3), norm_g).reshape(B, L, C_WIDTH)
    return (o * jax.nn.silu(g.astype(jnp.float32))).astype(p.dtype)


def expert_choice_ffn(u, router, w1, w3, w2):
    B, L, D = u.shape
    cap = EC_FACTOR * L // N_EXPERTS
    aff = jax.nn.softmax(jnp.einsum("bld,de->ble", u, router).astype(jnp.float32), axis=-1)
    gates, idx = lax.top_k(jnp.swapaxes(aff, 1, 2), cap)
    xs = jax.vmap(lambda ub, ib: ub[ib])(u, idx)
    hid = jax.nn.silu(jnp.einsum("becd,edf->becf", xs, w1)) * jnp.einsum("becd,edf->becf", xs, w3)
    ys = jnp.einsum("becf,efd->becd", hid, w2) * gates[..., None].astype(u.dtype)
    return jax.vmap(lambda yb, ib: jnp.zeros((L, D), yb.dtype).at[ib].add(yb))(ys, idx)


def setup_inputs(seed: int = 0) -> dict:
    key = jax.random.key(seed)
    ks = iter(jax.random.split(key, 32))
    nrm = lambda shape, scale: jax.random.normal(next(ks), shape, jnp.float32) * scale
    gain = lambda shape: 1.0 + nrm(shape, 0.02)
    D = D_MODEL
    return {
        "x": nrm((BATCH, SEQ, D), 1.0),
        "meta": nrm((N_META, D), 1.0),
        "norm1_g": gain((DEPTH, D)),
        "w_in": nrm((DEPTH, D, IN_COLS), D ** -0.5),
        "rw_shift": jax.random.uniform(next(ks), (DEPTH, 2, A_TOTAL), jnp.float32, 0.0, 0.5),
        "rw_w0": nrm((DEPTH, 2, A_WIDTH), 1.0),
        "rw_w_up": nrm((DEPTH, 2, A_DECAY_RANK, A_WIDTH), A_DECAY_RANK ** -0.5),
        "rw_a0": nrm((DEPTH, 2, A_WIDTH), 0.5),
        "rw_a_up": nrm((DEPTH, 2, A_ICLR_RANK, A_WIDTH), A_ICLR_RANK ** -0.5),
        "rw_g_up": nrm((DEPTH, A_GATE_RANK, A_WIDTH), A_GATE_RANK ** -0.5),
        "rw_k_k": 0.85 + nrm((DEPTH, A_WIDTH), 0.05),
        "rw_k_a": 1.0 + nrm((DEPTH, A_WIDTH), 0.05),
        "rw_r_k": nrm((DEPTH, A_WIDTH), 0.1),
        "rw_gn_g": gain((DEPTH, A_WIDTH)),
        "rw_gn_b": nrm((DEPTH, A_WIDTH), 0.02),
        "df_lam": nrm((DEPTH, 4, B_QK_DIM), 0.1),
        "df_subln_g": gain((DEPTH, B_V_DIM)),
        "gl_gate_up": nrm((DEPTH, 2, C_GATE_RANK, C_HEADS * C_DK), C_GATE_RANK ** -0.5),
        "gl_gate_b": nrm((DEPTH, 2, C_HEADS * C_DK), 0.5),
        "gl_norm_g": gain((DEPTH, C_DV)),
        "w_out": nrm((DEPTH, MIX_WIDTH, D), MIX_WIDTH ** -0.5),
        "norm2_g": gain((DEPTH, D)),
        "router": nrm((DEPTH, D, N_EXPERTS), D ** -0.5),
        "e_w1": nrm((DEPTH, N_EXPERTS, D, EXPERT_FF), D ** -0.5),
        "e_w3": nrm((DEPTH, N_EXPERTS, D, EXPERT_FF), D ** -0.5),
        "e_w2": nrm((DEPTH, N_EXPERTS, EXPERT_FF, D), EXPERT_FF ** -0.5),
        "final_g": gain((D,)),
    }


def reference(x, meta, norm1_g, w_in, rw_shift, rw_w0, rw_w_up, rw_a0, rw_a_up, rw_g_up, rw_k_k, rw_k_a,
              rw_r_k, rw_gn_g, rw_gn_b, df_lam, df_subln_g, gl_gate_up, gl_gate_b, gl_norm_g, w_out,
              norm2_g, router, e_w1, e_w3, e_w2, final_g):
    B = x.shape[0]
    h = jnp.concatenate([jnp.broadcast_to(meta[None].astype(x.dtype), (B, N_META, D_MODEL)), x], axis=1)
    L = h.shape[1]
    pos = jnp.arange(L, dtype=jnp.float32)
    for l in range(DEPTH):
        lam_init = 0.8 - 0.6 * math.exp(-0.3 * l)
        u = rms_norm(h, norm1_g[l])
        p = jnp.einsum("bld,dp->blp", u, w_in[l])
        p_a, p_b, p_c = _split(p, (A_TOTAL, B_TOTAL, C_TOTAL))
        y_a = rwkv7_group(p_a, rw_shift[l], rw_w0[l], rw_w_up[l], rw_a0[l], rw_a_up[l], rw_g_up[l],
                          rw_k_k[l], rw_k_a[l], rw_r_k[l], rw_gn_g[l], rw_gn_b[l])
        y_b = diff_attention_group(p_b, pos, df_lam[l], df_subln_g[l], lam_init)
        y_c = gla_group(p_c, gl_gate_up[l], gl_gate_b[l], gl_norm_g[l])
        mix = jnp.concatenate([y_a, y_b, y_c], axis=-1).astype(h.dtype)
        h = h + jnp.einsum("blc,cd->bld", mix, w_out[l])
        h = h + expert_choice_ffn(rms_norm(h, norm2_g[l]), router[l], e_w1[l], e_w3[l], e_w2[l])
    return rms_norm(h, final_g)[:, N_META:]
```

```python
import contextlib
import math
import numpy as np
import concourse.bass as bass
import concourse.mybir as mybir
from concourse.bass_utils import run_bass_kernel_spmd

F32 = mybir.dt.float32
BF16 = mybir.dt.bfloat16
AF = mybir.ActivationFunctionType
ALU = mybir.AluOpType
AX = mybir.AxisListType

import os as _os
EPOCH = int(_os.environ.get("K_EPOCH", "160"))
N_DMA_SEM = 24


class Buf:
    __slots__ = ("name", "w", "r")

    def __init__(self, name):
        self.name = name
        self.w = None
        self.r = {}


class KB:
    MAXEP = 16

    def __init__(self):
        self.nc = bass.Bass("TRN2", target_bir_lowering=False)
        nc = self.nc
        self.es = contextlib.ExitStack()
        self.eng = {"pe": nc.tensor, "act": nc.scalar, "dve": nc.vector, "pool": nc.gpsimd, "sp": nc.sync}
        self.cnt = {k: 0 for k in self.eng}
        self.sems = {}
        for e in ("pe", "act", "dve", "pool"):
            for ep in range(self.MAXEP):
                self.sems[(e, ep)] = self.es.enter_context(nc.semaphore(f"s_{e}_{ep}"))
        self.waited = {}
        self.bufs = {}
        self.dma_sems = [self.es.enter_context(nc.semaphore(f"dq{i}")) for i in range(N_DMA_SEM)]
        self.dma_tgt = [0] * N_DMA_SEM
        self.dma_rr = 0
        self.bar = [[self.es.enter_context(nc.semaphore(f"bar{p}{q}")) for q in range(2)] for p in range(2)]
        self.bgen = 0
        self.n_rb = 0
        self.inputs = {}
        self.outputs = {}
        self.same_engine_sync = True
        self.n_ins = 0
        self.pe_open = False

    def inp(self, name, shape, dt=F32):
        t = self.nc.dram_tensor(name, list(shape), dt, kind="ExternalInput")
        self.inputs[name] = (tuple(shape), dt)
        return t.ap()

    def outp(self, name, shape, dt=F32):
        t = self.nc.dram_tensor(name, list(shape), dt, kind="ExternalOutput")
        self.outputs[name] = (tuple(shape), dt)
        return t.ap()

    def dram(self, name, shape, dt=F32):
        return self.nc.dram_tensor(name, list(shape), dt, kind="Internal").ap()

    def sb(self, st, name, shape, dt=F32):
        self.uid = getattr(self, "uid", 0) + 1
        return st.enter_context(self.nc.sbuf_tensor(f"{name}_{self.uid}", list(shape), dt))

    def ps(self, st, name, shape, dt=F32):
        self.uid = getattr(self, "uid", 0) + 1
        return st.enter_context(self.nc.psum_tensor(f"{name}_{self.uid}", list(shape), dt))

    def _sem(self, e, ep):
        return self.sems[(e, ep)]

    def _buf(self, ap):
        name = ap if isinstance(ap, str) else getattr(ap, "tensor", ap).name
        b = self.bufs.get(name)
        if b is None:
            b = self.bufs[name] = Buf(name)
        return b

    def _wait(self, e, ev, force=False):
        kind = ev[0]
        if kind == "E":
            _, pe_, ep, val = ev
            if not force and pe_ == e and (e == "pe" or not self.same_engine_sync):
                return
            key = (e, "E", pe_, ep)
            if self.waited.get(key, 0) >= val:
                return
            self.eng[e].wait_ge(self._sem(pe_, ep), val)
            self.waited[key] = val
        else:
            _, slot, val = ev
            key = (e, "D", slot)
            if self.waited.get(key, 0) >= val:
                return
            self.eng[e].wait_ge(self.dma_sems[slot], val)
            self.waited[key] = val

    def _deps(self, e, reads, writes):
        for b in reads:
            if b.w is not None:
                self._wait(e, b.w)
            if b.name.startswith("psb"):
                for k_, ev in b.r.items():
                    if not (ev[0] == "E" and ev[1] == e):
                        self._wait(e, ev)
        for b in writes:
            if b.w is not None:
                self._wait(e, b.w)
            for ev in b.r.values():
                self._wait(e, ev)

    def _commit(self, ev, rkey, reads, writes):
        for b in reads:
            b.r[rkey] = ev
        for b in writes:
            b.w = ev
            b.r = {}

    def _maybe_reset(self, e=None):
        lim = EPOCH * (self.MAXEP - 1)
        if any(c >= lim for c in self.cnt.values()) or max(self.dma_tgt) >= 208:
            self.barrier(reset=True)

    def op(self, e, fn, reads, writes):
        self._maybe_reset()
        reads = [self._buf(x) for x in reads]
        writes = [self._buf(x) for x in writes]
        self._deps(e, reads, writes)
        ins = fn(self.eng[e])
        c = self.cnt[e]
        ep, val = c // EPOCH, c % EPOCH + 1
        ins.then_inc(self._sem(e, ep), 1)
        self.cnt[e] = c + 1
        self.n_ins += 1
        ev = ("E", e, ep, val)
        self._commit(ev, ("E", e), reads, writes)
        return ev

    def dma(self, q, out, in_, extra_r=(), extra_w=()):
        self._maybe_reset()
        reads = [self._buf(in_)] + [self._buf(x) for x in extra_r]
        writes = [self._buf(out)] + [self._buf(x) for x in extra_w]
        self._deps(q, reads, writes)
        slot = self.dma_rr
        self.dma_rr = (self.dma_rr + 1) % N_DMA_SEM
        if self.dma_tgt[slot] > 0:
            self._wait(q, ("D", slot, self.dma_tgt[slot]))
        ins = self.eng[q].dma_start(out=out, in_=in_)
        self.dma_tgt[slot] += 16
        ins.then_inc(self.dma_sems[slot], 16)
        self.n_ins += 1
        ev = ("D", slot, self.dma_tgt[slot])
        self._commit(ev, ("D", slot), reads, writes)
        return ev

    def init_staging(self, st, n=4, width=512):
        self.stg = [self.sb(st, f"stg{i}", [128, width], F32) for i in range(n)]
        self.stg_w = width
        self.stg_i = 0

    def load_cast(self, dst, src, parts=128):
        n = dst.shape[-1]
        assert len(dst.shape) == 2 and len(src.shape) == 2
        for c0 in range(0, n, self.stg_w):
            c1 = min(n, c0 + self.stg_w)
            sg = self.stg[self.stg_i % len(self.stg)]
            self.stg_i += 1
            self.dma("sp", sg[0:parts, 0:c1 - c0], src[:, c0:c1])
            self.copy(dst[:, c0:c1], sg[0:parts, 0:c1 - c0], eng=("pool" if self.stg_i % 2 else "act"))

    def barrier(self, reset=False):
        evs = []
        for e, c in self.cnt.items():
            if c > 0:
                cc = c - 1
                evs.append(("E", e, cc // EPOCH, cc % EPOCH + 1))
        for s, t in enumerate(self.dma_tgt):
            if t > 0:
                evs.append(("D", s, t))
        for e in self.eng:
            for ev in evs:
                self._wait(e, ev, force=True)
        for b in self.bufs.values():
            b.w = None
            b.r = {}
        if not reset:
            return
        self.n_rb += 1
        p = self.bgen % 2
        self.bgen += 1
        A, R = self.bar[p]
        A2, R2 = self.bar[1 - p]
        master = "sp"
        others = [e for e in self.eng if e != master]
        for e in others:
            self.eng[e].sem_inc(A, 1)
        m = self.eng[master]
        m.wait_ge(A, len(others))
        for sem in list(self.sems.values()) + self.dma_sems + [A2, R2]:
            m.sem_clear(sem)
        m.sem_inc(R, 1)
        for e in others:
            self.eng[e].wait_ge(R, 1)
        self.cnt = {k: 0 for k in self.eng}
        self.dma_tgt = [0] * N_DMA_SEM
        self.waited = {}

    def pe_fence(self):
        c = self.cnt["pe"]
        if c > 0:
            cc = c - 1
            self._wait("pe", ("E", "pe", cc // EPOCH, cc % EPOCH + 1), force=True)

    def finish(self):
        self.barrier()

    def mm(self, out, lhsT, rhs, start=True, stop=True, **kw):
        return self.op("pe", lambda e: e.matmul(out, lhsT, rhs, start=start, stop=stop, **kw), [lhsT, rhs], [out])

    def tr(self, out, in_, ident):
        return self.op("pe", lambda e: e.transpose(out, in_, ident), [in_, ident], [out])

    def act(self, out, in_, func, bias=None, scale=None, accum=None, eng="act"):
        kw = {}
        rd = [in_]
        if bias is not None:
            kw["bias"] = bias
            if not isinstance(bias, (int, float)):
                rd.append(bias)
        if scale is not None:
            kw["scale"] = scale
            if not isinstance(scale, (int, float)):
                rd.append(scale)
        wr = [out]
        if accum is not None:
            kw["accum_out"] = accum
            wr.append(accum)
        return self.op(eng, lambda e: e.activation(out=out, in_=in_, func=func, **kw), rd, wr)

    def tt(self, out, a, b, op, eng="dve"):
        return self.op(eng, lambda e: e.tensor_tensor(out=out, in0=a, in1=b, op=op), [a, b], [out])

    def ts(self, out, a, s1, op0, s2=None, op1=None, eng="dve"):
        rd = [a] + [s for s in (s1, s2) if s is not None and not isinstance(s, (int, float))]
        if op1 is None:
            return self.op(eng, lambda e: e.tensor_scalar(out=out, in0=a, scalar1=s1, scalar2=None, op0=op0), rd, [out])
        return self.op(eng, lambda e: e.tensor_scalar(out=out, in0=a, scalar1=s1, scalar2=s2, op0=op0, op1=op1), rd, [out])

    def stt(self, out, a, s, b, op0, op1, eng="dve"):
        rd = [a, b] + ([] if isinstance(s, (int, float)) else [s])
        return self.op(eng, lambda e: e.scalar_tensor_tensor(out=out, in0=a, scalar=s, in1=b, op0=op0, op1=op1), rd, [out])

    def copy(self, out, in_, eng="dve"):
        if eng == "act":
            return self.op("act", lambda e: e.copy(out=out, in_=in_), [in_], [out])
        return self.op(eng, lambda e: e.tensor_copy(out=out, in_=in_), [in_], [out])

    def rsqrt(self, out, in_, scale, bias):
        self.act(out, in_, AF.Sqrt, bias=bias, scale=scale)
        return self.op("dve", lambda e: e.reciprocal(out=out, in_=out), [out], [out])

    def rsqrt_act(self, out, in_, scale, bias):
        self.act(out, in_, AF.Ln, bias=bias, scale=scale)
        return self.act(out, out, AF.Exp, scale=-0.5)

    def scan(self, out, d0, d1, init, op0, op1):
        rd = [d0, d1] + ([] if isinstance(init, (int, float)) else [init])
        return self.op("dve", lambda e: e.tensor_tensor_scan(out=out, data0=d0, data1=d1, initial=init, op0=op0, op1=op1), rd, [out])

    def memset(self, ap, val, eng="dve"):
        return self.op(eng, lambda e: e.memset(ap, val), [], [ap])

    def reduce(self, out, in_, op, axis=AX.X, eng="dve"):
        return self.op(eng, lambda e: e.tensor_reduce(out=out, in_=in_, axis=axis, op=op), [in_], [out])


D = 1024
SEQ = 2048
NM = 16
L = SEQ + NM
DEPTH = 2
A_TOT, B_TOT, C_TOT = 1152, 1536, 800
IN_COLS = 3488
NE, CAP, FF = 16, 258, 1408
TT = [(0, 16)] + [(16 + 128 * j, 144 + 128 * j) for j in range(16)]
CH = [(0, 16)] + [(16 + 64 * i, 80 + 64 * i) for i in range(32)]
NB = [(0, 512), (512, 1024), (1024, 1536), (1536, 2048), (2048, 2064)]
EPS = 1e-6


def _consts():
    c = {}
    c["ident"] = np.eye(128, dtype=np.float32)
    return c


class Model:
    def __init__(self, debug=None, layers=(0, 1), phases="ABCDEGZ", bstop=0, bdirs=2, n_experts=NE, h_init=None):
        self.n_experts = n_experts
        self.h_init = h_init
        self.kb = KB()
        self.bstop = bstop
        self.bdirs = bdirs
        self.debug = debug or ()
        self.layers = layers
        self.phases = phases
        self.build()

    def build(self):
        kb = self.kb
        self.top = contextlib.ExitStack()
        top = self.top
        ph = self.phases
        self.x = kb.inp("x", [SEQ, D])
        self.meta = kb.inp("meta", [NM, D])
        self.c_ident = kb.inp("c_ident", [128, 128])
        self.norm1_bc = kb.inp("norm1_bc", [DEPTH, 128, D])
        if any(p in ph for p in "BCD"):
            self.w_in = kb.inp("w_in", [DEPTH, D, IN_COLS])
        if "B" in ph:
            self.rw_cols = kb.inp("rw_cols", [DEPTH, 128, C_NCOL])
            self.rw_wup = kb.inp("rw_wup", [DEPTH, 128, 256])
            self.rw_aup = kb.inp("rw_aup", [DEPTH, 128, 256])
            self.rw_gup = kb.inp("rw_gup", [DEPTH, 128, 256])
            self.c_blockones = kb.inp("c_blockones", [128, 128])
            self.c_masks = kb.inp("c_masks", [64, 2, 6, 256])
            self.c_rm = kb.inp("c_rm", [128, 512])
        if "C" in ph:
            self.c_cos = kb.inp("c_cos", [128, L])
            self.c_sin = kb.inp("c_sin", [128, L])
            self.c_rotperm = kb.inp("c_rotperm", [128, 128])
            self.df_lam_bc = kb.inp("df_lam_bc", [DEPTH, 128, 256])
            self.df_subln_bc = kb.inp("df_subln_bc", [DEPTH, 128, 128])
        if "D" in ph:
            if "B" not in ph:
                self.c_masks = kb.inp("c_masks", [64, 2, 6, 256])
            self.c_kvmask = kb.inp("c_kvmask", [128, 256])
            self.c_headmask = kb.inp("c_headmask", [128, 4])
            self.c_rmL = kb.inp("c_rmL", [128, L])
            self.gl_cols = kb.inp("gl_cols", [DEPTH, 128, 2])
            self.gl_norm_bc = kb.inp("gl_norm_bc", [DEPTH, 64, 256])
            self.gl_gate_up = kb.inp("gl_gate_up", [DEPTH, 2, 16, 128])
        if "E" in ph:
            self.w_out = kb.inp("w_out", [DEPTH, D, D])
        if "G" in ph:
            self.norm2_bc = kb.inp("norm2_bc", [DEPTH, 128, D])
            self.router = kb.inp("router", [DEPTH, D, NE])
            self.e_w1 = kb.inp("e_w1", [DEPTH, NE, D, FF])
            self.e_w3 = kb.inp("e_w3", [DEPTH, NE, D, FF])
            self.e_w2 = kb.inp("e_w2", [DEPTH, NE, FF, D])
            self.c_iota_c = kb.inp("c_iota_c", [128, CAP])
            self.c_iota_p = kb.inp("c_iota_p", [128, 3])
        if "Z" in ph:
            self.final_bc = kb.inp("final_bc", [128, D])
            self.out = kb.outp("out", [SEQ, D])
        self.dbg_out = {}
        for nm, shp, dt in (("route", [2, 128, 17, 16], F32), ("h", [L, D], F32), ("pT", [128, 9, L], BF16), ("ofm", [128, 2, L], F32), ("uT", [128, 8, L], BF16), ("mixT", [128, 8, L], BF16)):
            if nm in self.debug:
                self.dbg_out[nm] = kb.outp("dbg_" + nm, shp, dt)
        self.h = kb.dram("h_scr", [L, D])
        self.uT = kb.dram("uT_scr", [128, 8, L], BF16)
        self.mixT = kb.dram("mixT_scr", [128, 8, L], BF16)
        self.ident_bf = kb.sb(top, "ident_bf", [128, 128], BF16)
        self.ident_f = kb.sb(top, "ident_f", [128, 128], F32)
        kb.init_staging(top)
        kb.load_cast(self.ident_bf[:, :], self.c_ident[:, :])
        kb.dma("sp", self.ident_f[:], self.c_ident[:, :])
        self.psb = [kb.ps(top, f"psb{i}", [128, 512], F32) for i in range(8)]
        if self.h_init:
            self.h_in = kb.inp("h_in", [L, D])
            kb.dma("sp", self.h[:, :], self.h_in[:, :])
        else:
            kb.dma("sp", self.h[0:NM, :], self.meta[:, :])
            kb.dma("sp", self.h[NM:L, :], self.x[:, :])
        for l in self.layers:
            if "A" in ph:
                self.phase_norm(l, self.norm1_bc, self.uT)
            if "B" in ph:
                self.phase_rwkv(l)
            if "C" in ph:
                self.phase_attn(l)
            if "D" in ph:
                self.phase_gla(l)
            if "E" in ph:
                self.phase_outproj(l)
            if "G" in ph:
                self.phase_moe(l)
        if "Z" in ph:
            self.phase_final()
        kb.barrier()
        if "h" in self.debug:
            kb.dma("sp", self.dbg_out["h"][:, :], self.h[:, :])
        if "uT" in self.debug:
            kb.dma("sp", self.dbg_out["uT"][:, :, :], self.uT[:, :, :])
        if "mixT" in self.debug:
            for (c0, c1, pp) in ((0, 2, "B"), (2, 6, "C"), (6, 8, "D")):
                if pp in ph:
                    kb.dma("sp", self.dbg_out["mixT"][:, c0:c1, :], self.mixT[:, c0:c1, :])
        kb.finish()

    def phase_norm(self, l, g_bc_in, uT_out):
        kb = self.kb
        kb.barrier()
        with contextlib.ExitStack() as st:
            g_bc = kb.sb(st, "A_g", [128, D], F32)
            kb.dma("sp", g_bc[:], g_bc_in[l, :, :])
            uT_sb = kb.sb(st, "A_uT", [128, 8, L], BF16)
            hts = [kb.sb(st, f"A_h{i}", [128, D], F32) for i in range(3)]
            uns = [kb.sb(st, f"A_un{i}", [128, D], BF16) for i in range(2)]
            junk = kb.sb(st, "A_junk", [128, D], BF16)
            ssqs = [kb.sb(st, f"A_ssq{i}", [128, 1], F32) for i in range(3)]
            pst = [kb.ps(st, f"A_pst{i}", [128, 8, 128], BF16) for i in range(2)] if False else None
            for i, (t0, t1) in enumerate(TT):
                R = t1 - t0
                ht = hts[i % 3]
                un = uns[i % 2]
                ssq = ssqs[i % 3]
                kb.dma("sp", ht[:R, :], self.h[t0:t1, :])
                kb.act(junk[:R, :], ht[:R, :], AF.Square, accum=ssq[:R, :])
                kb.rsqrt(ssq[:R, :], ssq[:R, :], 1.0 / D, EPS)
                kb.stt(un[:R, :], ht[:R, :], ssq[:R, 0:1], g_bc[:R, :], ALU.mult, ALU.mult)
                ps = self.psb[i % 2].bitcast(BF16)
                for c in range(8):
                    kb.tr(ps[:, c * 128:c * 128 + R], un[:R, c * 128:(c + 1) * 128], self.ident_bf[:R, :R])
                psv = ps.rearrange("p (c t) -> p c t", c=8)
                kb.copy(uT_sb[:, :, t0:t1], psv[:, :, 0:R], eng="act" if i % 2 else "dve")
            kb.dma("sp", uT_out[:, :, :], uT_sb[:, :, :])
        kb.barrier()


def _chunkcols(v):
    v = np.asarray(v, np.float32)
    n = v.shape[-1] // 128
    return np.moveaxis(v.reshape(v.shape[:-1] + (n, 128)), -1, 0)


def _prep_shared(inputs):
    m = {}
    m["meta"] = np.ascontiguousarray(inputs["meta"])
    m["c_ident"] = np.eye(128, dtype=np.float32)
    m["norm1_bc"] = np.ascontiguousarray(np.broadcast_to(inputs["norm1_g"][:, None, :], (DEPTH, 128, D)))
    m["w_in"] = np.ascontiguousarray(inputs["w_in"])
    cols = np.zeros((DEPTH, 128, C_NCOL), np.float32)
    for l in range(DEPTH):
        sh = _chunkcols(inputs["rw_shift"][l])
        cols[l, :, C_MU0:C_MU0 + 9] = sh[:, 0]
        cols[l, :, C_MU1:C_MU1 + 9] = sh[:, 1]
        cols[l, :, C_W0:C_W0 + 4] = _chunkcols(inputs["rw_w0"][l]).reshape(128, 4)
        cols[l, :, C_A0:C_A0 + 4] = _chunkcols(inputs["rw_a0"][l]).reshape(128, 4)
        for nm, ci in (("rw_k_k", C_KK), ("rw_k_a", C_KA), ("rw_r_k", C_RK), ("rw_gn_g", C_GNG), ("rw_gn_b", C_GNB)):
            cols[l, :, ci:ci + 2] = _chunkcols(inputs[nm][l])
    m["rw_cols"] = cols
    m["rw_wup"] = np.ascontiguousarray(inputs["rw_w_up"].reshape(DEPTH, 128, 256))
    m["rw_aup"] = np.ascontiguousarray(inputs["rw_a_up"].reshape(DEPTH, 128, 256))
    m["rw_gup"] = np.ascontiguousarray(inputs["rw_g_up"])
    m.update(_rwkv_consts())
    m.update(_attn_consts())
    m["df_lam_bc"] = np.ascontiguousarray(np.broadcast_to(inputs["df_lam"].reshape(DEPTH, 1, 256), (DEPTH, 128, 256)))
    m["df_subln_bc"] = np.ascontiguousarray(np.broadcast_to(inputs["df_subln_g"][:, None, :], (DEPTH, 128, 128)))
    m["w_out"] = np.ascontiguousarray(inputs["w_out"])
    m.update(_gla_consts())
    m.update(_moe_consts())
    m["norm2_bc"] = np.ascontiguousarray(np.broadcast_to(inputs["norm2_g"][:, None, :], (DEPTH, 128, D)))
    m["final_bc"] = np.ascontiguousarray(np.broadcast_to(inputs["final_g"][None, :], (128, D)))
    for k_ in ("router", "e_w1", "e_w3", "e_w2"):
        if k_ in inputs:
            m[k_] = np.ascontiguousarray(inputs[k_])
    m["gl_cols"] = np.ascontiguousarray(np.transpose(inputs["gl_gate_b"], (0, 2, 1)))
    m["gl_norm_bc"] = np.ascontiguousarray(np.broadcast_to(np.tile(inputs["gl_norm_g"], (1, 4))[:, None, :], (DEPTH, 64, 256)))
    m["gl_gate_up"] = np.ascontiguousarray(inputs["gl_gate_up"])
    return m


def _prep_inputs(inputs, b, shared=None):
    m = dict(shared if shared is not None else _prep_shared(inputs))
    m["x"] = np.ascontiguousarray(inputs["x"][b])
    return m


C_MU0, C_MU1, C_W0, C_A0, C_KK, C_KA, C_RK, C_GNG, C_GNB, C_NCOL = 0, 9, 18, 22, 26, 28, 30, 32, 34, 36
A_DECAY_SCALE = 0.6065306597126334
GROUPS = [[0]] + [list(range(1 + 4 * g, 5 + 4 * g)) for g in range(8)]
PBLK = [(0, 16)] + [(16 + 512 * b, 16 + 512 * (b + 1)) for b in range(4)]


def _rwkv_consts():
    c = {}
    bo = np.zeros((128, 128), np.float32)
    bo[:64, :64] = 1
    bo[64:, 64:] = 1
    c["c_blockones"] = bo
    i = np.arange(64)
    U = (i[:, None] < i[None, :]).astype(np.float32)
    Lo = (i[:, None] > i[None, :]).astype(np.float32)
    Ui = (i[:, None] <= i[None, :]).astype(np.float32)
    Li = (i[:, None] >= i[None, :]).astype(np.float32)
    I = np.eye(64, dtype=np.float32)
    t4 = lambda m: np.tile(m, (1, 4))
    m = np.zeros((64, 2, 6, 256), np.float32)
    m[:, 0, 0], m[:, 0, 1], m[:, 0, 2], m[:, 0, 3], m[:, 0, 4], m[:, 0, 5] = t4(-U), t4(-Lo), t4(U), t4(Ui), t4(Ui), t4(I)
    m[:, 1, 0], m[:, 1, 1], m[:, 1, 2], m[:, 1, 3], m[:, 1, 4], m[:, 1, 5] = t4(-Lo), t4(-U), t4(Lo), t4(Li), t4(Li), t4(I)
    c["c_masks"] = m
    rm = np.ones((128, 512), np.float32)
    rm[:, ::64] = 0.0
    c["c_rm"] = rm
    return c


def phase_rwkv(self, l):
    kb = self.kb
    kb.barrier()
    psb = self.psb
    pi = [0]

    def nps():
        pi[0] = (pi[0] + 1) % 8
        return psb[pi[0]]

    with contextlib.ExitStack() as st:
        cols = kb.sb(st, "B_cols", [128, C_NCOL], F32)
        kb.dma("sp", cols[:], self.rw_cols[l, :, :])
        muc = kb.sb(st, "B_muc", [128, 9], F32)
        kb.ts(muc[:], cols[:, C_MU0:C_MU0 + 9], -1.0, ALU.mult, 1.0, ALU.add)
        kb.tt(muc[:], muc[:], cols[:, C_MU1:C_MU1 + 9], ALU.subtract)
        omka = kb.sb(st, "B_omka", [128, 2], F32)
        kb.ts(omka[:], cols[:, C_KA:C_KA + 2], -1.0, ALU.mult, 1.0, ALU.add)
        wup = kb.sb(st, "B_wup", [128, 256], BF16)
        aup = kb.sb(st, "B_aup", [128, 256], BF16)
        gup = kb.sb(st, "B_gup", [128, 256], BF16)
        kb.load_cast(wup[:, :], self.rw_wup[l, :, :])
        kb.load_cast(aup[:, :], self.rw_aup[l, :, :])
        kb.load_cast(gup[:, :], self.rw_gup[l, :, :])
        bo = kb.sb(st, "B_bo", [128, 128], BF16)
        kb.load_cast(bo[:, :], self.c_blockones[:, :])
        masks = kb.sb(st, "B_masks", [64, 2, 6, 256], BF16)
        for dd in range(2):
            kb.load_cast(masks[:, dd, :, :].rearrange("p a b -> p (a b)"), self.c_masks[:, dd, :, :].rearrange("p a b -> p (a b)"), parts=64)
        rm = kb.sb(st, "B_rm", [128, 512], F32)
        kb.dma("sp", rm[:], self.c_rm[:, :])

        pT = kb.sb(st, "B_pT", [128, 9, L], BF16)
        with contextlib.ExitStack() as s1:
            uT = kb.sb(s1, "B_uT", [128, 8, L], BF16)
            kb.dma("sp", uT[:], self.uT[:, :, :])
            wa = kb.sb(s1, "B_wa", [128, 8, A_TOT], BF16)
            for c in range(8):
                kb.load_cast(wa[:, c, :], self.w_in[l, c * 128:(c + 1) * 128, 0:A_TOT])
            tmp = [kb.sb(s1, f"B_tmp{i}", [128, 512], F32) for i in range(2)]
            k = 0
            import os
            BIS = os.environ.get("K_BISECT", "")
            for j in range(9 if not BIS else int(BIS)):
                mu0 = cols[:, C_MU0 + j:C_MU0 + j + 1]
                mu1 = cols[:, C_MU1 + j:C_MU1 + j + 1]
                for b in range(5):
                    o0 = 510 * b
                    o1 = min(o0 + 510, L)
                    lo, hi = max(o0 - 1, 0), min(o1 + 1, L)
                    n, no, off = hi - lo, o1 - o0, o0 - lo
                    ps = nps()
                    for c in range(8):
                        kb.mm(ps[:, 0:n], wa[:, c, j * 128:(j + 1) * 128], uT[:, c, lo:hi], start=(c == 0), stop=(c == 7))
                    t = tmp[k % 2]
                    k += 1
                    kb.ts(t[:, 0:no], ps[:, off:off + no], muc[:, j:j + 1], ALU.mult)
                    a = 1 if o0 == 0 else 0
                    kb.stt(t[:, a:no], ps[:, off - 1 + a:off - 1 + no], mu0, t[:, a:no], ALU.mult, ALU.add)
                    nn = no - 1 if o1 == L else no
                    kb.stt(pT[:, j, o0:o0 + nn], ps[:, off + 1:off + 1 + nn], mu1, t[:, 0:nn], ALU.mult, ALU.add)
                    if nn < no:
                        kb.copy(pT[:, j, L - 1:L], t[:, no - 1:no])
        kb.barrier()
        if "pT" in self.debug:
            kb.dma("sp", self.dbg_out["pT"][:, :, :], pT[:, :, :])
            kb.barrier()
        if self.bstop == 1:
            return

        tw = kb.sb(st, "B_tw", [128, L], BF16)
        sg = kb.sb(st, "B_sg", [128, L], BF16)
        kb.act(tw[:], pT[:, 6, :], AF.Tanh)
        kb.act(sg[:], pT[:, 8, :], AF.Sigmoid)
        kk = kb.sb(st, "B_kk", [128, 2, L], BF16)
        gate = kb.sb(st, "B_gate", [128, 2, L], BF16)
        kdsum = kb.sb(st, "B_kdsum", [128, 2, L], BF16)
        o_fm = kb.sb(st, "B_ofm", [128, 2, L], F32)
        kb.memset(o_fm[:], 0.0)
        with contextlib.ExitStack() as s2:
            kkr = kb.sb(s2, "B_kkr", [128, L], BF16)
            sq = kb.sb(s2, "B_sq", [128, L], BF16)
            rn = [kb.sb(s2, f"B_rn{i}", [128, 512], F32) for i in range(2)]
            k = 0
            for j in range(2):
                kb.ts(kkr[:], pT[:, 2 + j, :], cols[:, C_KK + j:C_KK + j + 1], ALU.mult)
                kb.tt(sq[:], kkr[:], kkr[:], ALU.mult)
                for (b0, b1) in NB:
                    n = b1 - b0
                    ps = nps()
                    kb.mm(ps[:, 0:n], bo[:], sq[:, b0:b1])
                    r_ = rn[k % 2]
                    k += 1
                    kb.rsqrt_act(r_[:, 0:n], ps[:, 0:n], 1.0, 1e-30)
                    kb.tt(kk[:, j, b0:b1], kkr[:, b0:b1], r_[:, 0:n], ALU.mult)
                for (b0, b1) in NB:
                    n = b1 - b0
                    ps = nps()
                    kb.mm(ps[:, 0:n], gup[:, j * 128:(j + 1) * 128], sg[:, b0:b1])
                    kb.copy(gate[:, j, b0:b1], ps[:, 0:n], eng="act")
        kb.barrier()
        if self.bstop == 2:
            return

        for d in range(self.bdirs):
            with contextlib.ExitStack() as sd:
                PhiT = kb.sb(sd, "B_PhiT", [128, 33, 2, 128], BF16)
                Hs = kb.sb(sd, "B_Hs", [128, 34, 2, 128], BF16)
                QT = kb.sb(sd, "B_QT", [128, 2, L], BF16)
                WCc = kb.sb(sd, "B_WCc", [128, 2, 33], F32)
                fmn = ["ApT", "BpT", "CpT", "RpT", "BnT"]
                fm = {nm: kb.sb(sd, f"B_{nm}", [128, 2, 512], BF16) for nm in fmn}
                tf = {nm: kb.sb(sd, f"B_t_{nm}", [128, 512], F32) for nm in ["lw", "pre", "xp", "TP", "TX", "ex"]}
                tb = {nm: kb.sb(sd, f"B_t_{nm}", [128, 512], BF16) for nm in ["icl", "kd", "ka", "tb"]}
                NS = 3
                tm_all = [kb.sb(sd, f"B_tm{s}", [64, 4, 256], BF16) for s in range(NS)]
                Vpad = [kb.sb(sd, f"B_Vpad{s}", [64, 4, 128], BF16) for s in range(NS)]
                PP = [[kb.sb(sd, f"B_PP{s}_{q}", [64, 2, 256], BF16) for q in range(2)] for s in range(NS)]
                XX = [[kb.sb(sd, f"B_XX{s}_{q}", [64, 256], BF16) for q in range(2)] for s in range(NS)]
                MP = [kb.sb(sd, f"B_MP{s}", [64, 3, 256], BF16) for s in range(NS)]
                M2Vn = [kb.sb(sd, f"B_M2Vn{s}", [64, 256], BF16) for s in range(NS)]
                GU = [kb.sb(sd, f"B_GU{s}", [64, 2, 256], BF16) for s in range(NS)]
                GUpad = [kb.sb(sd, f"B_GUpad{s}", [64, 2, 4, 128], BF16) for s in range(NS)]
                for s in range(NS):
                    kb.memset(Vpad[s][:], 0.0)
                    kb.memset(GUpad[s][:], 0.0)
                kb.memset(Hs[:, 0 if d == 0 else 33, :, :], 0.0)

                for bi, (b0, b1) in enumerate(PBLK):
                    n = b1 - b0
                    chunks = [0] if bi == 0 else list(range(1 + 8 * (bi - 1), 9 + 8 * (bi - 1)))
                    for j in range(2):
                        lw, pre, xp, TP, TX, ex = (tf[x] for x in ["lw", "pre", "xp", "TP", "TX", "ex"])
                        icl, kd, ka, tbb = (tb[x] for x in ["icl", "kd", "ka", "tb"])
                        ps = nps()
                        kb.mm(ps[:, 0:n], wup[d * 64:(d + 1) * 64, j * 128:(j + 1) * 128], tw[d * 64:(d + 1) * 64, b0:b1])
                        kb.act(lw[:, 0:n], ps[:, 0:n], AF.Sigmoid, bias=cols[:, C_W0 + d * 2 + j:C_W0 + d * 2 + j + 1])
                        kb.ts(lw[:, 0:n], lw[:, 0:n], -A_DECAY_SCALE, ALU.mult)
                        ps = nps()
                        kb.mm(ps[:, 0:n], aup[d * 64:(d + 1) * 64, j * 128:(j + 1) * 128], pT[d * 64:(d + 1) * 64, 7, b0:b1])
                        kb.act(icl[:, 0:n], ps[:, 0:n], AF.Sigmoid, bias=cols[:, C_A0 + d * 2 + j:C_A0 + d * 2 + j + 1])
                        kb.ts(tbb[:, 0:n], icl[:, 0:n], cols[:, C_KA + j:C_KA + j + 1], ALU.mult, omka[:, j:j + 1], ALU.add)
                        kb.tt(kd[:, 0:n], tbb[:, 0:n], pT[:, 2 + j, b0:b1], ALU.mult)
                        kb.tt(ka[:, 0:n], kk[:, j, b0:b1], icl[:, 0:n], ALU.mult)
                        if d == 0:
                            kb.copy(kdsum[:, j, b0:b1], kd[:, 0:n])
                        else:
                            kb.tt(kdsum[:, j, b0:b1], kdsum[:, j, b0:b1], kd[:, 0:n], ALU.add)
                        kb.scan(pre[:, 0:n], rm[:, 0:n], lw[:, 0:n], 0.0, ALU.mult, ALU.add)
                        kb.tt(xp[:, 0:n], pre[:, 0:n], lw[:, 0:n], ALU.subtract)
                        if bi == 0:
                            tot = pre[:, n - 1:n].to_broadcast([128, n])
                            kb.tt(TP[:, 0:n], tot, pre[:, 0:n], ALU.subtract)
                            kb.tt(TX[:, 0:n], tot, xp[:, 0:n], ALU.subtract)
                            kb.act(WCc[:, j, 0:1], pre[:, n - 1:n], AF.Exp)
                        else:
                            v3 = lambda t: t[:, 0:512].rearrange("p (n c) -> p n c", c=64)
                            tot = v3(pre)[:, :, 63:64].to_broadcast([128, 8, 64])
                            kb.tt(v3(TP), tot, v3(pre), ALU.subtract)
                            kb.tt(v3(TX), tot, v3(xp), ALU.subtract)
                            kb.act(WCc[:, j, chunks[0]:chunks[0] + 8], v3(pre)[:, :, 63], AF.Exp)
                        r_ = pT[:, 0 + j, b0:b1]
                        kkj = kk[:, j, b0:b1]
                        if d == 0:
                            srcs = dict(E1=(TP, 1.0), E2=(TP, -1.0), E2w=(TX, -1.0), E3=(pre, 1.0), E3w=(xp, 1.0))
                        else:
                            srcs = dict(E1=(xp, 1.0), E2=(xp, -1.0), E2w=(pre, -1.0), E3=(TX, 1.0), E3w=(TP, 1.0))
                        kb.act(ex[:, 0:n], srcs["E1"][0][:, 0:n], AF.Exp, scale=srcs["E1"][1])
                        kb.tt(fm["ApT"][:, j, 0:n], ka[:, 0:n], ex[:, 0:n], ALU.mult)
                        kb.tt(fm["CpT"][:, j, 0:n], kd[:, 0:n], ex[:, 0:n], ALU.mult)
                        kb.act(ex[:, 0:n], srcs["E2"][0][:, 0:n], AF.Exp, scale=srcs["E2"][1])
                        kb.tt(fm["RpT"][:, j, 0:n], r_, ex[:, 0:n], ALU.mult)
                        kb.act(ex[:, 0:n], srcs["E2w"][0][:, 0:n], AF.Exp, scale=srcs["E2w"][1])
                        kb.tt(fm["BpT"][:, j, 0:n], kkj, ex[:, 0:n], ALU.mult)
                        kb.act(ex[:, 0:n], srcs["E3"][0][:, 0:n], AF.Exp, scale=srcs["E3"][1])
                        kb.tt(QT[:, j, b0:b1], r_, ex[:, 0:n], ALU.mult)
                        kb.act(ex[:, 0:n], srcs["E3w"][0][:, 0:n], AF.Exp, scale=srcs["E3w"][1])
                        kb.stt(fm["BnT"][:, j, 0:n], kkj, -1.0, ex[:, 0:n], ALU.mult, ALU.mult)

                    for g0 in range(0, len(chunks), NS):
                        grp = chunks[g0:g0 + NS]
                        info = []
                        for s, cn in enumerate(grp):
                            c0, c1 = CH[cn]
                            info.append((s, cn, c1 - c0, c0 - b0))
                        hv = lambda ap, C: ap.rearrange("p (h c) -> p h c", h=4)[:, :, 0:C]
                        for (s, cn, C, o) in info:
                            ps = nps().bitcast(BF16)
                            for ti, nm in enumerate(["BnT", "ApT", "CpT", None]):
                                for j in range(2):
                                    src = fm[nm][:, j, o:o + C] if nm else pT[:, 4 + j, b0 + o:b0 + o + C]
                                    kb.tr(ps[0:C, ti * 256 + j * 128:ti * 256 + (j + 1) * 128], src, self.ident_bf[:, :])
                            kb.copy(tm_all[s][0:C, :, :], ps[0:C, :].rearrange("p (a b) -> p a b", a=4), eng="act")
                            for hp in range(2):
                                vsrc = tm_all[s][0:C, 3, :].rearrange("p (j q k) -> p j q k", j=2, q=2)[:, :, hp, :]
                                vdst = Vpad[s][0:C, :, :].rearrange("p (j q) m -> p j q m", q=2)[:, :, hp, hp * 64:(hp + 1) * 64]
                                kb.copy(vdst, vsrc, eng="dve")
                        for (s, cn, C, o) in info:
                            pa, pb, pc = nps(), nps(), nps()
                            for h in (0, 2, 1, 3):
                                j, q = h // 2, (h % 2) * 64
                                if h == 1:
                                    kb.pe_fence()
                                A_ = fm["ApT"][q:q + 64, j, o:o + C]
                                B_ = fm["BpT"][q:q + 64, j, o:o + C]
                                C_ = fm["CpT"][q:q + 64, j, o:o + C]
                                R_ = fm["RpT"][q:q + 64, j, o:o + C]
                                kb.mm(pa[0:C, h * 64:h * 64 + C], A_, B_)
                                kb.mm(pa[0:C, 256 + h * 64:256 + h * 64 + C], B_, A_)
                                kb.mm(pb[0:C, h * 64:h * 64 + C], C_, B_)
                                kb.mm(pb[0:C, 256 + h * 64:256 + h * 64 + C], A_, R_)
                                kb.mm(pc[0:C, h * 64:h * 64 + C], C_, R_)
                            P0 = PP[s][0]
                            kb.tt(hv(P0[0:C, 0, :], C), hv(pa[0:C, 0:256], C), hv(masks[0:C, d, 0, :], C), ALU.mult)
                            kb.tt(hv(P0[0:C, 1, :], C), hv(pa[0:C, 256:512], C), hv(masks[0:C, d, 1, :], C), ALU.mult)
                            kb.tt(hv(XX[s][0][0:C, :], C), hv(P0[0:C, 0, :], C), hv(masks[0:C, d, 5, :], C), ALU.add)
                            kb.tt(hv(MP[s][0:C, 0, :], C), hv(pb[0:C, 0:256], C), hv(masks[0:C, d, 2, :], C), ALU.mult)
                            kb.tt(hv(MP[s][0:C, 1, :], C), hv(pb[0:C, 256:512], C), hv(masks[0:C, d, 3, :], C), ALU.mult)
                            kb.tt(hv(MP[s][0:C, 2, :], C), hv(pc[0:C, 0:256], C), hv(masks[0:C, d, 4, :], C), ALU.mult)
                        nlev = 5 if len(grp) > 1 or grp[0] != 0 else 3
                        for lev in range(nlev):
                            cur, nxt = lev % 2, (lev + 1) % 2
                            last = lev == nlev - 1
                            for (s, cn, C, o) in info:
                                ps = nps()
                                Pc, Pn = PP[s][cur], PP[s][nxt]
                                for h in range(4):
                                    P_ = Pc[0:C, 0, h * 64:h * 64 + C]
                                    PT_ = Pc[0:C, 1, h * 64:h * 64 + C]
                                    kb.mm(ps[0:C, 256 + h * 64:256 + h * 64 + C], P_, PT_)
                                    if not last:
                                        kb.mm(ps[0:C, h * 64:h * 64 + C], PT_, P_)
                                if last:
                                    kb.copy(hv(Pn[0:C, 1, :], C), hv(ps[0:C, 256:512], C), eng="act")
                                else:
                                    kb.copy(Pn[0:C, :, :].rearrange("p a (h c) -> p a h c", h=4)[:, :, :, 0:C],
                                            ps[0:C, :].rearrange("p (a h c) -> p a h c", a=2, h=4)[:, :, :, 0:C], eng="act")
                            for (s, cn, C, o) in info:
                                ps = nps()
                                Pn = PP[s][nxt]
                                Xc, Xn = XX[s][cur], XX[s][nxt]
                                for h in range(4):
                                    sl = slice(h * 64, h * 64 + C)
                                    kb.mm(ps[0:C, sl], self.ident_bf[0:C, 0:C], Xc[0:C, sl], start=True, stop=False)
                                    kb.mm(ps[0:C, sl], Pn[0:C, 1, sl], Xc[0:C, sl], start=False, stop=True)
                                kb.copy(hv(Xn[0:C, :], C), hv(ps[0:C, 0:256], C), eng="dve")
                        xf = nlev % 2
                        for (s, cn, C, o) in info:
                            ps = nps()
                            for h in range(4):
                                sl = slice(h * 64, h * 64 + C)
                                kb.mm(ps[0:C, h * 64:(h + 1) * 64], MP[s][0:C, 0, sl], tm_all[s][0:C, 3, h * 64:(h + 1) * 64])
                            kb.act(M2Vn[s][0:C, :], ps[0:C, 0:256], AF.Copy, scale=-1.0)
                        for (s, cn, C, o) in info:
                            ps = nps()
                            TT_ = XX[s][xf]
                            for h in range(4):
                                sl = slice(h * 64, h * 64 + C)
                                kb.mm(ps[0:C, h * 64:(h + 1) * 64], TT_[0:C, sl], tm_all[s][0:C, 0, h * 64:(h + 1) * 64])
                                kb.mm(ps[0:C, 256 + h * 64:256 + (h + 1) * 64], TT_[0:C, sl], M2Vn[s][0:C, h * 64:(h + 1) * 64])
                            kb.copy(GU[s][0:C, :, :], ps[0:C, :].rearrange("p (a b) -> p a b", a=2), eng="act")
                            for hp in range(2):
                                src = GU[s][0:C, :, :].rearrange("p a (j q k) -> p a j q k", j=2, q=2)[:, :, :, hp, :]
                                dst = GUpad[s][0:C, :, :, :].rearrange("p a (j q) m -> p a j q m", q=2)[:, :, :, hp, hp * 64:(hp + 1) * 64]
                                kb.copy(dst, src, eng="dve")
                        for (s, cn, C, o) in info:
                            ps = nps()
                            for j in range(2):
                                pr = slice(j * 128, (j + 1) * 128)
                                kb.mm(ps[:, j * 128:(j + 1) * 128], GU[s][0:C, 0, pr], tm_all[s][0:C, 1, pr])
                                kb.mm(ps[:, 256 + j * 128:256 + (j + 1) * 128], tm_all[s][0:C, 1, pr], GU[s][0:C, 1, pr], start=True, stop=False)
                                kb.mm(ps[:, 256 + j * 128:256 + (j + 1) * 128], tm_all[s][0:C, 2, pr], tm_all[s][0:C, 3, pr], start=False, stop=True)
                            bo2 = bo[:, :].to_broadcast([128, 128]) if False else None
                            hslot = cn + 1 if d == 0 else cn
                            for j in range(2):
                                kb.tt(PhiT[:, cn, j, :], ps[:, j * 128:(j + 1) * 128], bo[:, :], ALU.mult)
                                kb.stt(PhiT[:, cn, j, :], self.ident_bf[:, :], WCc[:, j, cn:cn + 1], PhiT[:, cn, j, :], ALU.mult, ALU.add)
                                kb.tt(Hs[:, hslot, j, :], ps[:, 256 + j * 128:256 + (j + 1) * 128], bo[:, :], ALU.mult)
                        for (s, cn, C, o) in info:
                            ps = nps()
                            for j in range(2):
                                for q in range(2):
                                    h = 2 * j + q
                                    kb.mm(ps[:, j * 64:j * 64 + C], GUpad[s][0:C, 0, h, :], MP[s][0:C, 1, h * 64:h * 64 + C],
                                          start=(q == 0), stop=(q == 1))
                            c0 = CH[cn][0]
                            qv = QT[:, :, c0:c0 + C]
                            kb.tt(qv, qv, ps[:, 0:128].rearrange("p (j c) -> p j c", j=2)[:, :, 0:C], ALU.add)
                        for (s, cn, C, o) in info:
                            ps = nps()
                            for j in range(2):
                                for q in range(2):
                                    h = 2 * j + q
                                    kb.mm(ps[:, j * 64:j * 64 + C], GUpad[s][0:C, 1, h, :], MP[s][0:C, 1, h * 64:h * 64 + C],
                                          start=(q == 0), stop=False)
                                    kb.mm(ps[:, j * 64:j * 64 + C], Vpad[s][0:C, h, :], MP[s][0:C, 2, h * 64:h * 64 + C],
                                          start=False, stop=(q == 1))
                            c0 = CH[cn][0]
                            ov = o_fm[:, :, c0:c0 + C]
                            kb.tt(ov, ov, ps[:, 0:128].rearrange("p (j c) -> p j c", j=2)[:, :, 0:C], ALU.add)

                order = list(range(33)) if d == 0 else list(range(32, -1, -1))
                for cn in order:
                    hin = cn if d == 0 else cn + 1
                    hout = cn + 1 if d == 0 else cn
                    ps = nps()
                    for j in range(2):
                        kb.mm(ps[:, j * 128:(j + 1) * 128], PhiT[:, cn, j, :], Hs[:, hin, j, :])
                    hv_ = Hs[:, hout, :, :]
                    kb.tt(hv_, hv_, ps[:, 0:256].rearrange("p (j m) -> p j m", j=2), ALU.add)
                for cn in range(33):
                    c0, c1 = CH[cn]
                    C = c1 - c0
                    hin = cn if d == 0 else cn + 1
                    ps = nps()
                    for j in range(2):
                        kb.mm(ps[:, j * 64:j * 64 + C], Hs[:, hin, j, :], QT[:, j, c0:c1])
                    ov = o_fm[:, :, c0:c1]
                    kb.tt(ov, ov, ps[:, 0:128].rearrange("p (j c) -> p j c", j=2)[:, :, 0:C], ALU.add)
            kb.barrier()
        if "ofm" in self.debug:
            kb.dma("sp", self.dbg_out["ofm"][:, :, :], o_fm[:, :, :])
            kb.barrier()
        if self.bstop == 3:
            return

        with contextlib.ExitStack() as s3:
            ob = kb.sb(s3, "B_ob", [128, L], BF16)
            cen = kb.sb(s3, "B_cen", [128, L], F32)
            sqb = kb.sb(s3, "B_sqb", [128, L], BF16)
            rstd = kb.sb(s3, "B_rstd", [128, L], F32)
            prod = kb.sb(s3, "B_prod", [128, L], BF16)
            ya = kb.sb(s3, "B_ya", [128, 2, L], BF16)
            for j in range(2):
                kb.copy(ob[:], o_fm[:, j, :], eng="act")
                for (b0, b1) in NB:
                    n = b1 - b0
                    ps = nps()
                    kb.mm(ps[:, 0:n], bo[:], ob[:, b0:b1])
                    kb.stt(cen[:, b0:b1], ps[:, 0:n], -1.0 / 64, o_fm[:, j, b0:b1], ALU.mult, ALU.add)
                kb.tt(sqb[:], cen[:], cen[:], ALU.mult)
                for (b0, b1) in NB:
                    n = b1 - b0
                    ps = nps()
                    kb.mm(ps[:, 0:n], bo[:], sqb[:, b0:b1])
                    kb.rsqrt_act(rstd[:, b0:b1], ps[:, 0:n], 1.0 / 64, 64e-5)
                kb.tt(cen[:], cen[:], rstd[:], ALU.mult)
                kb.ts(cen[:], cen[:], cols[:, C_GNG + j:C_GNG + j + 1], ALU.mult, cols[:, C_GNB + j:C_GNB + j + 1], ALU.add)
                kb.stt(prod[:], pT[:, 0 + j, :], cols[:, C_RK + j:C_RK + j + 1], kdsum[:, j, :], ALU.mult, ALU.mult)
                for (b0, b1) in NB:
                    n = b1 - b0
                    ps = nps()
                    kb.mm(ps[:, 0:n], bo[:], prod[:, b0:b1])
                    kb.tt(rstd[:, b0:b1], ps[:, 0:n], pT[:, 4 + j, b0:b1], ALU.mult)
                kb.tt(cen[:], cen[:], rstd[:], ALU.add)
                kb.tt(ya[:, j, :], cen[:], gate[:, j, :], ALU.mult)
            kb.dma("sp", self.mixT[:, 0:2, :], ya[:, :, :])
    kb.barrier()


Model.phase_rwkv = phase_rwkv


ROPE_THETA = 10000.0


def _attn_consts():
    c = {}
    pos = np.arange(L, dtype=np.float32)
    inv = (ROPE_THETA ** (-np.arange(32, dtype=np.float32) / 32)).astype(np.float32)
    ang = pos[None, :] * inv[:, None]
    cos = np.cos(ang).astype(np.float32)
    sin = np.sin(ang).astype(np.float32)
    c["c_cos"] = np.ascontiguousarray(np.tile(cos, (4, 1)))
    c["c_sin"] = np.ascontiguousarray(np.tile(sin, (4, 1)))
    P = np.zeros((128, 128), np.float32)
    for blk in range(2):
        o = blk * 64
        for i in range(32):
            P[o + i, o + 32 + i] = -1.0
            P[o + 32 + i, o + i] = 1.0
    c["c_rotperm"] = np.ascontiguousarray(P.T)
    return c


def phase_attn(self, l):
    kb = self.kb
    kb.barrier()
    psb = self.psb
    lam_init = 0.8 - 0.6 * math.exp(-0.3 * l)
    pi = [0]

    def nps():
        pi[0] = (pi[0] + 1) % 8
        return psb[pi[0]]

    with contextlib.ExitStack() as st:
        lamb = kb.sb(st, "C_lamb", [128, 256], F32)
        kb.dma("sp", lamb[:], self.df_lam_bc[l, :, :])
        sub_g = kb.sb(st, "C_subg", [128, 128], F32)
        kb.dma("sp", sub_g[:], self.df_subln_bc[l, :, :])
        kb.ts(sub_g[:], sub_g[:], 1.0 - lam_init, ALU.mult)
        lt = kb.sb(st, "C_lt", [128, 128], F32)
        ee = kb.sb(st, "C_ee", [128, 2], F32)
        kb.tt(lt[:, 0:64], lamb[:, 0:64], lamb[:, 64:128], ALU.mult)
        kb.tt(lt[:, 64:128], lamb[:, 128:192], lamb[:, 192:256], ALU.mult)
        kb.reduce(ee[:, :], lt[:, :].rearrange("p (a b) -> p a b", a=2), ALU.add)
        kb.act(ee[:, :], ee[:, :], AF.Exp)
        nlam = kb.sb(st, "C_nlam", [128, 1], F32)
        kb.tt(nlam[:], ee[:, 1:2], ee[:, 0:1], ALU.subtract)
        kb.ts(nlam[:], nlam[:], -lam_init, ALU.add)
        cos = kb.sb(st, "C_cos", [128, L], F32)
        sin = kb.sb(st, "C_sin", [128, L], F32)
        kb.dma("sp", cos[:], self.c_cos[:, :])
        kb.dma("sp", sin[:], self.c_sin[:, :])
        perm = kb.sb(st, "C_perm", [128, 128], BF16)
        kb.load_cast(perm[:, :], self.c_rotperm[:, :])
        qkT = kb.sb(st, "C_qkT", [128, 8, L], BF16)
        v_aug = kb.sb(st, "C_vaug", [128, 17, 4, 132], BF16)
        kb.memset(v_aug[:, :, :, 128:129], 1.0)
        with contextlib.ExitStack() as s1:
            uT = kb.sb(s1, "C_uT", [128, 8, L], BF16)
            kb.dma("sp", uT[:], self.uT[:, :, :])
            wb = kb.sb(s1, "C_wb", [128, 8, B_TOT], BF16)
            for c in range(8):
                kb.load_cast(wb[:, c, :], self.w_in[l, c * 128:(c + 1) * 128, A_TOT:A_TOT + B_TOT])
            raw = [kb.sb(s1, f"C_raw{i}", [128, 512], BF16) for i in range(2)]
            t1 = [kb.sb(s1, f"C_t1{i}", [128, 512], F32) for i in range(2)]
            t2 = [kb.sb(s1, f"C_t2{i}", [128, 512], F32) for i in range(2)]
            k = 0
            for jj in range(8):
                for (b0, b1) in NB:
                    n = b1 - b0
                    ps = nps()
                    for c in range(8):
                        kb.mm(ps[:, 0:n], wb[:, c, jj * 128:(jj + 1) * 128], uT[:, c, b0:b1], start=(c == 0), stop=(c == 7))
                    r_, a_, b_ = raw[k % 2], t1[k % 2], t2[k % 2]
                    k += 1
                    kb.act(r_[:, 0:n], ps[:, 0:n], AF.Copy, scale=(0.125 if jj < 4 else 1.0))
                    ps2 = nps()
                    kb.mm(ps2[:, 0:n], perm[:, :], r_[:, 0:n])
                    kb.tt(a_[:, 0:n], r_[:, 0:n], cos[:, b0:b1], ALU.mult)
                    kb.tt(b_[:, 0:n], ps2[:, 0:n], sin[:, b0:b1], ALU.mult)
                    kb.tt(qkT[:, jj, b0:b1], a_[:, 0:n], b_[:, 0:n], ALU.add)
            for i, (t0, t1_) in enumerate(TT):
                R = t1_ - t0
                ps = nps()
                for c in range(8):
                    kb.mm(ps[0:R, :], uT[:, c, t0:t1_], wb[:, c, 1024:1536], start=(c == 0), stop=(c == 7))
                kb.copy(v_aug[0:R, i, :, 0:128], ps[0:R, :].rearrange("p (h d) -> p h d", h=4), eng="act")
        kb.barrier()
        yb = kb.sb(st, "C_yb", [128, 4, L], BF16)
        pTs = [kb.sb(st, f"C_pT{i}", [128, 512], BF16) for i in range(3)]
        oc = [[kb.sb(st, f"C_oc{c}_{q}", [128, 132], F32) for q in range(4)] for c in range(2)]
        rr = kb.sb(st, "C_rr", [128, 4], F32)
        od = [kb.sb(st, f"C_od{q}", [128, 128], F32) for q in range(2)]
        ob = [kb.sb(st, f"C_ob{q}", [128, 128], BF16) for q in range(2)]
        junk = kb.sb(st, "C_junk", [128, 128], F32)
        ssq = kb.sb(st, "C_ssq", [128, 2], F32)
        S_banks = [psb[0], psb[1]]
        A_banks = [psb[2], psb[3], psb[4], psb[5]]
        T_banks = [psb[6], psb[7]]
        si = 0
        pk = 0
        dk = 0
        for h in range(4):
            for (b0, b1) in NB:
                nq = b1 - b0
                subs = [(q0, min(q0 + 128, nq)) for q0 in range(0, nq, 128)]
                for c in range(2):
                    kb.pe_fence()
                    for i, (t0, t1_) in enumerate(TT):
                        R = t1_ - t0
                        S = S_banks[si % 2]
                        si += 1
                        kb.mm(S[0:R, 0:nq], qkT[c * 64:(c + 1) * 64, 4 + h, t0:t1_], qkT[c * 64:(c + 1) * 64, h, b0:b1])
                        p_ = pTs[pk % 3]
                        pk += 1
                        kb.act(p_[0:R, 0:nq], S[0:R, 0:nq], AF.Exp)
                        for qi, (q0, q1) in enumerate(subs):
                            kb.mm(A_banks[qi][0:q1 - q0, 0:129], p_[0:R, q0:q1], v_aug[0:R, i, h, 0:129], start=(i == 0), stop=(i == 16))
                    for qi, (q0, q1) in enumerate(subs):
                        kb.copy(oc[c][qi][0:q1 - q0, 0:129], A_banks[qi][0:q1 - q0, 0:129], eng=("act" if qi % 2 else "dve"))
                for qi, (q0, q1) in enumerate(subs):
                    m = q1 - q0
                    o0, o1 = oc[0][qi], oc[1][qi]
                    d_ = od[dk % 2]
                    b_ = ob[dk % 2]
                    dk += 1
                    kb.op("dve", lambda e, o0=o0, m=m: e.reciprocal(out=rr[0:m, 0:1], in_=o0[0:m, 128:129]), [o0], [rr])
                    kb.op("dve", lambda e, o1=o1, m=m: e.reciprocal(out=rr[0:m, 1:2], in_=o1[0:m, 128:129]), [o1], [rr])
                    kb.tt(rr[0:m, 1:2], rr[0:m, 1:2], nlam[0:m, 0:1], ALU.mult)
                    kb.ts(d_[0:m, :], o0[0:m, 0:128], rr[0:m, 0:1], ALU.mult)
                    kb.stt(d_[0:m, :], o1[0:m, 0:128], rr[0:m, 1:2], d_[0:m, :], ALU.mult, ALU.add)
                    kb.act(junk[0:m, :], d_[0:m, :], AF.Square, accum=ssq[0:m, 0:1])
                    kb.rsqrt(ssq[0:m, 0:1], ssq[0:m, 0:1], 1.0 / 128, EPS)
                    kb.stt(b_[0:m, :], d_[0:m, :], ssq[0:m, 0:1], sub_g[0:m, :], ALU.mult, ALU.mult)
                    tb_ = T_banks[dk % 2].bitcast(BF16)
                    kb.tr(tb_[:, 0:m], b_[0:m, :], self.ident_bf[0:m, 0:m])
                    kb.copy(yb[:, h, b0 + q0:b0 + q1], tb_[:, 0:m], eng="act")
        kb.dma("sp", self.mixT[:, 2:6, :], yb[:, :, :])
    kb.barrier()


Model.phase_attn = phase_attn


def phase_outproj(self, l):
    kb = self.kb
    kb.barrier()
    with contextlib.ExitStack() as st:
        mixT = kb.sb(st, "E_mixT", [128, 8, L], BF16)
        kb.dma("sp", mixT[:], self.mixT[:, :, :])
        wo = kb.sb(st, "E_wo", [128, 8, D], BF16)
        for c in range(8):
            kb.load_cast(wo[:, c, :], self.w_out[l, c * 128:(c + 1) * 128, :])
        hts = [kb.sb(st, f"E_h{i}", [128, D], F32) for i in range(3)]
        k = 0
        for i, (t0, t1) in enumerate(TT):
            R = t1 - t0
            ht = hts[i % 3]
            kb.dma("sp", ht[0:R, :], self.h[t0:t1, :])
            for half in range(2):
                ps = self.psb[k % 4]
                k += 1
                for c in range(8):
                    kb.mm(ps[0:R, :], mixT[:, c, t0:t1], wo[:, c, half * 512:(half + 1) * 512], start=(c == 0), stop=(c == 7))
                kb.tt(ht[0:R, half * 512:(half + 1) * 512], ht[0:R, half * 512:(half + 1) * 512], ps[0:R, :], ALU.add)
            kb.dma("sp", self.h[t0:t1, :], ht[0:R, :])
    kb.barrier()


Model.phase_outproj = phase_outproj


def _gla_consts():
    c = {}
    kvm = np.zeros((128, 256), np.float32)
    hm = np.zeros((128, 4), np.float32)
    for h in range(4):
        kvm[h * 32:(h + 1) * 32, h * 64:(h + 1) * 64] = 1.0
        hm[h * 32:(h + 1) * 32, h] = 1.0
    c["c_kvmask"] = kvm
    c["c_headmask"] = hm
    rm = np.ones((128, L), np.float32)
    for (c0, c1) in CH:
        rm[:, c0] = 0.0
    c["c_rmL"] = rm
    return c


def phase_gla(self, l):
    kb = self.kb
    kb.barrier()
    psb = self.psb
    pi = [0]

    def nps():
        pi[0] = (pi[0] + 1) % 8
        return psb[pi[0]]

    with contextlib.ExitStack() as st:
        gcols = kb.sb(st, "D_gcols", [128, 2], F32)
        kb.dma("sp", gcols[:], self.gl_cols[l, :, :])
        kb.ts(gcols[:], gcols[:], -1.0, ALU.mult)
        ng = kb.sb(st, "D_ng", [64, 256], F32)
        kb.dma("sp", ng[:], self.gl_norm_bc[l, :, :])
        kvm = kb.sb(st, "D_kvm", [128, 256], F32)
        kb.dma("sp", kvm[:], self.c_kvmask[:, :])
        hm = kb.sb(st, "D_hm", [128, 4], F32)
        kb.dma("sp", hm[:], self.c_headmask[:, :])
        rmL = kb.sb(st, "D_rmL", [128, L], F32)
        kb.dma("sp", rmL[:], self.c_rmL[:, :])
        amask = kb.sb(st, "D_amask", [64, 2, 256], BF16)
        for d in range(2):
            kb.load_cast(amask[:, d, :], self.c_masks[:, d, 3, :], parts=64)
        gu = [kb.sb(st, f"D_gu{d}", [16, 128], BF16) for d in range(2)]
        for d in range(2):
            kb.load_cast(gu[d][:, :], self.gl_gate_up[l, d, :, :], parts=16)
        qT = kb.sb(st, "D_qT", [128, L], F32)
        kT = kb.sb(st, "D_kT", [128, L], F32)
        adT = [kb.sb(st, f"D_adT{d}", [16, L], BF16) for d in range(2)]
        v_tm = kb.sb(st, "D_vtm", [64, 33, 256], BF16)
        sg_tm = kb.sb(st, "D_sgtm", [64, 33, 256], BF16)
        with contextlib.ExitStack() as s1:
            uT = kb.sb(s1, "D_uT", [128, 8, L], BF16)
            kb.dma("sp", uT[:], self.uT[:, :, :])
            wc = kb.sb(s1, "D_wc", [128, 8, C_TOT], BF16)
            for c in range(8):
                kb.load_cast(wc[:, c, :], self.w_in[l, c * 128:(c + 1) * 128, A_TOT + B_TOT:IN_COLS])
            for (dst, col0, ncol, scale) in ((qT, 0, 128, 32 ** -0.5), (kT, 128, 128, 1.0), (adT[0], 768, 16, 1.0), (adT[1], 784, 16, 1.0)):
                for (b0, b1) in NB:
                    n = b1 - b0
                    ps = nps()
                    for c in range(8):
                        kb.mm(ps[0:ncol, 0:n], wc[:, c, col0:col0 + ncol], uT[:, c, b0:b1], start=(c == 0), stop=(c == 7))
                    kb.act(dst[0:ncol, b0:b1], ps[0:ncol, 0:n], AF.Copy, scale=scale)
            for n_, (c0, c1) in enumerate(CH):
                C = c1 - c0
                ps = nps()
                for c in range(8):
                    kb.mm(ps[0:C, :], uT[:, c, c0:c1], wc[:, c, 256:768], start=(c == 0), stop=(c == 7))
                kb.copy(v_tm[0:C, n_, :], ps[0:C, 0:256], eng="dve")
                kb.act(sg_tm[0:C, n_, :], ps[0:C, 256:512], AF.Silu)
        kb.barrier()
        qd = [kb.sb(st, f"D_qd{d}", [128, L], BF16) for d in range(2)]
        kim = [[kb.sb(st, f"D_kim{d}_{h}", [128, L], BF16) for h in range(4)] for d in range(2)]
        Ss = [kb.sb(st, f"D_Ss{d}", [128, 34, 256], BF16) for d in range(2)]
        with contextlib.ExitStack() as s2:
            lw = kb.sb(s2, "D_lw", [128, L], F32)
            pre = kb.sb(s2, "D_pre", [128, L], F32)
            a1 = kb.sb(s2, "D_a1", [128, L], F32)
            ex = kb.sb(s2, "D_ex", [128, L], F32)
            ki = kb.sb(s2, "D_ki", [128, L], BF16)
            ke = kb.sb(s2, "D_ke", [128, L], BF16)
            dec = kb.sb(s2, "D_dec", [128, 33], F32)
            ketm = [kb.sb(s2, f"D_ketm{i}", [64, 128], BF16) for i in range(2)]
            for d in range(2):
                for (b0, b1) in NB:
                    n = b1 - b0
                    ps = nps()
                    kb.mm(ps[:, 0:n], gu[d][:, :], adT[d][:, b0:b1])
                    kb.act(lw[:, b0:b1], ps[:, 0:n], AF.Exp, scale=-1.0, bias=gcols[:, d:d + 1])
                kb.act(lw[:], lw[:], AF.Ln, bias=1.0)
                kb.ts(lw[:], lw[:], -1.0 / 16.0, ALU.mult)
                kb.scan(pre[:], rmL[:], lw[:], 0.0, ALU.mult, ALU.add)
                v3 = lambda t: t[:, 16:L].rearrange("p (n c) -> p n c", c=64)
                tot3 = v3(pre)[:, :, 63:64].to_broadcast([128, 32, 64])
                tot0 = pre[:, 15:16].to_broadcast([128, 16])
                kb.act(dec[:, 0:1], pre[:, 15:16], AF.Exp)
                kb.act(dec[:, 1:33], v3(pre)[:, :, 63], AF.Exp)
                if d == 0:
                    kb.act(ex[:], pre[:], AF.Exp)
                    kb.tt(qd[d][:], qT[:], ex[:], ALU.mult)
                    kb.act(ex[:], pre[:], AF.Exp, scale=-1.0)
                    kb.tt(ki[:], kT[:], ex[:], ALU.mult)
                    kb.tt(v3(a1), tot3, v3(pre), ALU.subtract)
                    kb.tt(a1[:, 0:16], tot0, pre[:, 0:16], ALU.subtract)
                    kb.act(ex[:], a1[:], AF.Exp)
                    kb.tt(ke[:], kT[:], ex[:], ALU.mult)
                else:
                    kb.tt(pre[:], pre[:], lw[:], ALU.subtract)
                    kb.act(ex[:], pre[:], AF.Exp)
                    kb.tt(ke[:], kT[:], ex[:], ALU.mult)
                    kb.tt(a1[:], pre[:], lw[:], ALU.add)
                    a3 = v3(a1)[:, :, 63:64].to_broadcast([128, 32, 64])
                    a0 = a1[:, 15:16].to_broadcast([128, 16])
                    kb.tt(v3(lw), a3, v3(pre), ALU.subtract)
                    kb.tt(lw[:, 0:16], a0, pre[:, 0:16], ALU.subtract)
                    kb.act(ex[:], lw[:], AF.Exp)
                    kb.tt(qd[d][:], qT[:], ex[:], ALU.mult)
                    kb.act(ex[:], lw[:], AF.Exp, scale=-1.0)
                    kb.tt(ki[:], kT[:], ex[:], ALU.mult)
                for h in range(4):
                    kb.ts(kim[d][h][:], ki[:], hm[:, h:h + 1], ALU.mult)
                for n_, (c0, c1) in enumerate(CH):
                    C = c1 - c0
                    tb_ = nps().bitcast(BF16)
                    kb.tr(tb_[0:C, 0:128], ke[:, c0:c1], self.ident_bf[:, :])
                    kt_ = ketm[n_ % 2]
                    kb.copy(kt_[0:C, :], tb_[0:C, 0:128], eng="act")
                    ps = nps()
                    kb.mm(ps[:, 0:256], kt_[0:C, :], v_tm[0:C, n_, :])
                    kb.tt(Ss[d][:, (n_ + 1 if d == 0 else n_), :], ps[:, 0:256], kvm[:, :], ALU.mult)
                if d == 0:
                    kb.memset(Ss[d][:, 0, :], 0.0)
                    for n_ in range(33):
                        kb.stt(Ss[d][:, n_ + 1, :], Ss[d][:, n_, :], dec[:, n_:n_ + 1], Ss[d][:, n_ + 1, :], ALU.mult, ALU.add)
                else:
                    kb.memset(Ss[d][:, 33, :], 0.0)
                    for n_ in range(32, -1, -1):
                        kb.stt(Ss[d][:, n_, :], Ss[d][:, n_ + 1, :], dec[:, n_:n_ + 1], Ss[d][:, n_, :], ALU.mult, ALU.add)
        kb.barrier()
        yc = kb.sb(st, "D_yc", [128, 2, L], BF16)
        att = [[kb.sb(st, f"D_att{i}_{d}", [64, 4, 64], BF16) for d in range(2)] for i in range(2)]
        osb = [kb.sb(st, f"D_osb{i}", [64, 256], F32) for i in range(2)]
        sq = kb.sb(st, "D_sq", [64, 256], F32)
        ssq = [kb.sb(st, f"D_ssq{i}", [64, 4], F32) for i in range(2)]
        ycb = [kb.sb(st, f"D_ycb{i}", [64, 256], BF16) for i in range(2)]
        for n_, (c0, c1) in enumerate(CH):
            C = c1 - c0
            at = att[n_ % 2]
            for d in range(2):
                ps = nps()
                for h in range(4):
                    kb.mm(ps[0:C, h * 64:h * 64 + C], kim[d][h][:, c0:c1], qd[d][:, c0:c1])
                kb.tt(at[d][0:C, :, 0:C], ps[0:C, 0:256].rearrange("p (h c) -> p h c", h=4)[:, :, 0:C],
                      amask[0:C, d, :].rearrange("p (h c) -> p h c", h=4)[:, :, 0:C], ALU.mult)
            po = nps()
            for d in range(2):
                hin = n_ if d == 0 else n_ + 1
                kb.mm(po[0:C, 0:256], qd[d][:, c0:c1], Ss[d][:, hin, :], start=(d == 0), stop=False)
                for h in range(4):
                    kb.mm(po[0:C, h * 64:(h + 1) * 64], at[d][0:C, h, 0:C], v_tm[0:C, n_, h * 64:(h + 1) * 64],
                          start=False, stop=(d == 1 and h == 3))
            o_ = osb[n_ % 2]
            s_ = ssq[n_ % 2]
            y_ = ycb[n_ % 2]
            kb.copy(o_[0:C, :], po[0:C, 0:256], eng="act")
            kb.tt(sq[0:C, :], o_[0:C, :], o_[0:C, :], ALU.mult)
            kb.reduce(s_[0:C, :], sq[0:C, :].rearrange("p (h d) -> p h d", h=4), ALU.add)
            kb.rsqrt(s_[0:C, :], s_[0:C, :], 1.0 / 64, EPS)
            for h in range(4):
                hs = slice(h * 64, (h + 1) * 64)
                kb.stt(o_[0:C, hs], o_[0:C, hs], s_[0:C, h:h + 1], ng[0:C, hs], ALU.mult, ALU.mult)
            kb.tt(y_[0:C, :], o_[0:C, :], sg_tm[0:C, n_, :], ALU.mult)
            tb_ = nps().bitcast(BF16)
            for j in range(2):
                kb.tr(tb_[:, j * 64:j * 64 + C], y_[0:C, j * 128:(j + 1) * 128], self.ident_bf[0:C, 0:C])
            kb.copy(yc[:, :, c0:c1], tb_[:, 0:128].rearrange("p (j c) -> p j c", j=2)[:, :, 0:C], eng="act")
        kb.dma("sp", self.mixT[:, 6:8, :], yc[:, :, :])
    kb.barrier()


Model.phase_gla = phase_gla


def _moe_consts():
    c = {}
    c["c_iota_c"] = np.ascontiguousarray(np.broadcast_to(np.arange(CAP, dtype=np.float32)[None, :], (128, CAP)))
    ip = np.zeros((128, 3), np.float32)
    for cc in range(3):
        ip[:, cc] = np.arange(128) + 128 * cc
    c["c_iota_p"] = ip
    oh = np.zeros((16, 16, 128), np.float32)
    for e in range(16):
        oh[e, e, :] = 1.0
    c["c_onehot"] = oh
    return c


def phase_moe(self, l):
    kb = self.kb
    kb.barrier()
    psb = self.psb
    pi = [0]

    def nps():
        pi[0] = (pi[0] + 1) % 8
        return psb[pi[0]]

    with contextlib.ExitStack() as st:
        hacc = kb.sb(st, "G_hacc", [128, 17, D], F32)
        u2 = kb.sb(st, "G_u2", [128, 17, D], BF16)
        posm_tok = kb.sb(st, "G_posm", [128, 17, 16], F32)
        gate_tok = kb.sb(st, "G_gate", [128, 17, 16], F32)
        iota_c = kb.sb(st, "G_iotac", [128, CAP], F32)
        kb.dma("sp", iota_c[:], self.c_iota_c[:, :])
        iota_p = kb.sb(st, "G_iotap", [128, 3], F32)
        kb.dma("sp", iota_p[:], self.c_iota_p[:, :])
        with contextlib.ExitStack() as s1:
            g_bc = kb.sb(s1, "F_g", [128, D], F32)
            kb.dma("sp", g_bc[:], self.norm2_bc[l, :, :])
            rt = kb.sb(s1, "F_rt", [128, 8, 16], F32)
            kb.dma("sp", rt[:], self.router[l].rearrange("(c p) e -> p c e", p=128))
            un = [kb.sb(s1, f"F_un{i}", [128, D], F32) for i in range(2)]
            uTf = [kb.sb(s1, f"F_uTf{i}", [128, 8, 128], F32) for i in range(2)]
            junk = kb.sb(s1, "F_junk", [128, D], BF16)
            ssq = [kb.sb(s1, f"F_ssq{i}", [128, 1], F32) for i in range(2)]
            ex = [kb.sb(s1, f"F_ex{i}", [128, 16], F32) for i in range(2)]
            esum = [kb.sb(s1, f"F_es{i}", [128, 1], F32) for i in range(2)]
            aff = kb.sb(s1, "F_aff", [128, 17, 16], F32)
            affT = kb.sb(s1, "F_affT", [16, L], F32)
            posmT = kb.sb(s1, "F_posmT", [16, L], F32)
            for i, (t0, t1) in enumerate(TT):
                R = t1 - t0
                kb.dma("sp", hacc[0:R, i, :], self.h[t0:t1, :])
                u_ = un[i % 2]
                s_ = ssq[i % 2]
                kb.act(junk[0:R, :], hacc[0:R, i, :], AF.Square, accum=s_[0:R, :])
                kb.rsqrt(s_[0:R, :], s_[0:R, :], 1.0 / D, EPS)
                kb.stt(u_[0:R, :], hacc[0:R, i, :], s_[0:R, 0:1], g_bc[0:R, :], ALU.mult, ALU.mult)
                kb.copy(u2[0:R, i, :], u_[0:R, :], eng="act")
                pa, pb = nps(), nps()
                for c in range(8):
                    pp = pa if c < 4 else pb
                    kb.tr(pp[:, (c % 4) * 128:(c % 4) * 128 + R], u_[0:R, c * 128:(c + 1) * 128], self.ident_f[0:R, 0:R])
                tf_ = uTf[i % 2]
                kb.copy(tf_[:, 0:4, 0:R], pa[:, :].rearrange("p (c t) -> p c t", c=4)[:, :, 0:R], eng="dve")
                kb.copy(tf_[:, 4:8, 0:R], pb[:, :].rearrange("p (c t) -> p c t", c=4)[:, :, 0:R], eng="act")
                pl = nps()
                for c in range(8):
                    kb.mm(pl[0:R, 0:16], tf_[:, c, 0:R], rt[:, c, :], start=(c == 0), stop=(c == 7))
                e_ = ex[i % 2]
                es_ = esum[i % 2]
                kb.act(e_[0:R, :], pl[0:R, 0:16], AF.Exp, accum=es_[0:R, :])
                kb.op("dve", lambda e, es_=es_, R=R: e.reciprocal(out=es_[0:R, :], in_=es_[0:R, :]), [es_], [es_])
                kb.ts(aff[0:R, i, :], e_[0:R, :], es_[0:R, 0:1], ALU.mult)
                pt = nps()
                kb.tr(pt[0:16, 0:R], aff[0:R, i, :], self.ident_f[0:R, 0:R])
                kb.copy(affT[:, t0:t1], pt[0:16, 0:R], eng="act")
            work = kb.sb(s1, "F_work", [16, L], F32)
            m8 = kb.sb(s1, "F_m8", [16, 8], F32)
            kb.copy(work[:], affT[:], eng="dve")
            nit = (CAP + 7) // 8
            for it in range(nit):
                kb.op("dve", lambda e: e.max(out=m8[:, :], in_=work[:, :]), [work], [m8])
                rem = CAP - it * 8
                if rem < 8:
                    kb.memset(m8[:, rem:8], 0.0)
                kb.op("dve", lambda e: e.match_replace(out=work[:, :], in_to_replace=m8[:, :], in_values=work[:, :], imm_value=0.0), [m8, work], [work])
            gatesT = kb.sb(s1, "F_gatesT", [16, L], F32)
            maskT = kb.sb(s1, "F_maskT", [16, L], F32)
            ones = kb.sb(s1, "F_ones", [16, L], F32)
            kb.memset(ones[:], 1.0)
            kb.tt(gatesT[:], affT[:], work[:], ALU.subtract)
            kb.ts(maskT[:], gatesT[:], 0.0, ALU.is_gt)
            kb.scan(posmT[:], ones[:], maskT[:], 0.0, ALU.mult, ALU.add)
            kb.tt(posmT[:], posmT[:], maskT[:], ALU.mult)
            kb.ts(posmT[:], posmT[:], -1.0, ALU.add)
            for i, (t0, t1) in enumerate(TT):
                R = t1 - t0
                pt = nps()
                kb.tr(pt[0:R, 0:16], posmT[:, t0:t1], self.ident_f[0:16, 0:16])
                kb.tr(pt[0:R, 16:32], gatesT[:, t0:t1], self.ident_f[0:16, 0:16])
                kb.copy(posm_tok[0:R, i, :], pt[0:R, 0:16], eng="act")
                kb.copy(gate_tok[0:R, i, :], pt[0:R, 16:32], eng="dve")
        kb.barrier()
        if "route" in self.debug:
            kb.dma("sp", self.dbg_out["route"][0, :, :, :], posm_tok[:, :, :])
            kb.dma("sp", self.dbg_out["route"][1, :, :, :], gate_tok[:, :, :])
            kb.barrier()
        w1 = kb.sb(st, "G_w1", [128, 8, FF], BF16)
        w3 = kb.sb(st, "G_w3", [128, 8, FF], BF16)
        w2 = kb.sb(st, "G_w2", [128, 11, D], BF16)
        Sel = kb.sb(st, "G_Sel", [128, 17, CAP], BF16)
        SelTg = kb.sb(st, "G_SelTg", [128, 3, 1024], BF16)
        xy = kb.sb(st, "G_xy", [128, 3 * D], BF16)
        xT = xy[:, 0:8 * CAP].rearrange("p (c n) -> p c n", c=8)
        yb = xy[:, :].rearrange("p (c d) -> p c d", c=3)
        hT = kb.sb(st, "G_hT", [128, 11, CAP], BF16)
        sil = [kb.sb(st, f"G_sil{i}", [128, CAP], F32) for i in range(1)]
        CC = [(0, 86), (86, 172), (172, 258)]
        ne = self.n_experts
        for e in range(ne):
            for c in range(8):
                kb.load_cast(w1[:, c, :], self.e_w1[l, e, c * 128:(c + 1) * 128, :])
                kb.load_cast(w3[:, c, :], self.e_w3[l, e, c * 128:(c + 1) * 128, :])
            for fc in range(11):
                kb.load_cast(w2[:, fc, :], self.e_w2[l, e, fc * 128:(fc + 1) * 128, :])
            for i, (t0, t1) in enumerate(TT):
                R = t1 - t0
                kb.ts(Sel[0:R, i, :], iota_c[0:R, :], posm_tok[0:R, i, e:e + 1], ALU.is_equal, eng="pool")
            for c in range(8):
                ps = nps()
                for i, (t0, t1) in enumerate(TT):
                    R = t1 - t0
                    kb.mm(ps[:, 0:CAP], u2[0:R, i, c * 128:(c + 1) * 128], Sel[0:R, i, :], start=(i == 0), stop=(i == 16))
                kb.copy(xT[:, c, :], ps[:, 0:CAP], eng="act")
            for fc in range(11):
                p1, p3 = nps(), nps()
                for c in range(8):
                    kb.mm(p1[:, 0:CAP], w1[:, c, fc * 128:(fc + 1) * 128], xT[:, c, :], start=(c == 0), stop=(c == 7))
                for c in range(8):
                    kb.mm(p3[:, 0:CAP], w3[:, c, fc * 128:(fc + 1) * 128], xT[:, c, :], start=(c == 0), stop=(c == 7))
                s_ = sil[0]
                kb.act(s_[:, :], p1[:, 0:CAP], AF.Silu)
                kb.tt(hT[:, fc, :], s_[:, :], p3[:, 0:CAP], ALU.mult)
            for cc, (a0, a1) in enumerate(CC):
                m = a1 - a0
                for half in range(2):
                    ps = nps()
                    for fc in range(11):
                        kb.mm(ps[0:m, :], hT[:, fc, a0:a1], w2[:, fc, half * 512:(half + 1) * 512], start=(fc == 0), stop=(fc == 10))
                    kb.copy(yb[0:m, cc, half * 512:(half + 1) * 512], ps[0:m, :], eng="act")
            for g0 in range(0, 17, 8):
                tiles = list(range(g0, min(g0 + 8, 17)))
                for cc, (a0, a1) in enumerate(CC):
                    m = a1 - a0
                    tb_ = nps().bitcast(BF16)
                    for k_, i in enumerate(tiles):
                        t0, t1 = TT[i]
                        R = t1 - t0
                        kb.tr(tb_[0:m, k_ * 128:k_ * 128 + R], Sel[0:R, i, a0:a1], self.ident_bf[0:R, 0:R])
                    eg = "act" if cc % 2 else "dve"
                    if g0 == 0:
                        kb.copy(SelTg[0:m, cc, 0:16], tb_[0:m, 0:16], eng=eg)
                        kb.copy(SelTg[0:m, cc, 128:1024], tb_[0:m, 128:1024], eng=eg)
                    else:
                        kb.copy(SelTg[0:m, cc, 0:len(tiles) * 128], tb_[0:m, 0:len(tiles) * 128], eng=eg)
                for k_, i in enumerate(tiles):
                    t0, t1 = TT[i]
                    R = t1 - t0
                    for half in range(2):
                        ps = nps()
                        for cc, (a0, a1) in enumerate(CC):
                            m = a1 - a0
                            kb.mm(ps[0:R, :], SelTg[0:m, cc, k_ * 128:k_ * 128 + R], yb[0:m, cc, half * 512:(half + 1) * 512], start=(cc == 0), stop=(cc == 2))
                        hv = hacc[0:R, i, half * 512:(half + 1) * 512]
                        kb.stt(hv, ps[0:R, :], gate_tok[0:R, i, e:e + 1], hv, ALU.mult, ALU.add)
        for i, (t0, t1) in enumerate(TT):
            R = t1 - t0
            kb.dma("sp", self.h[t0:t1, :], hacc[0:R, i, :])
    kb.barrier()


Model.phase_moe = phase_moe


def phase_final(self):
    kb = self.kb
    kb.barrier()
    with contextlib.ExitStack() as st:
        g_bc = kb.sb(st, "Z_g", [128, D], F32)
        kb.dma("sp", g_bc[:], self.final_bc[:, :])
        hts = [kb.sb(st, f"Z_h{i}", [128, D], F32) for i in range(3)]
        junk = kb.sb(st, "Z_junk", [128, D], BF16)
        ssqs = [kb.sb(st, f"Z_ssq{i}", [128, 1], F32) for i in range(3)]
        for i, (t0, t1) in enumerate(TT):
            if i == 0:
                continue
            ht, ssq = hts[i % 3], ssqs[i % 3]
            kb.dma("sp", ht[:, :], self.h[t0:t1, :])
            kb.act(junk[:, :], ht[:, :], AF.Square, accum=ssq[:, :])
            kb.rsqrt(ssq[:, :], ssq[:, :], 1.0 / D, EPS)
            kb.stt(ht[:, :], ht[:, :], ssq[:, 0:1], g_bc[:, :], ALU.mult, ALU.mult)
            kb.dma("sp", self.out[t0 - NM:t1 - NM, :], ht[:, :])
    kb.barrier()


Model.phase_final = phase_final


_CACHE = {}


def kernel(**inputs):
    inputs = {k: np.asarray(v) for k, v in inputs.items()}
    if "model" not in _CACHE:
        _ph = _os.environ.get("K_PHASES", "")
        _CACHE["model"] = Model(phases=_ph) if _ph else Model()
    m = _CACHE["model"]
    shared = _prep_shared(inputs)
    B = inputs["x"].shape[0]
    in_maps = []
    for b in range(B):
        im = _prep_inputs(inputs, b, shared)
        in_maps.append({k: v for k, v in im.items() if k in m.kb.inputs})
    res = run_bass_kernel_spmd(m.kb.nc, in_maps, core_ids=list(range(B)))
    out = np.stack([np.asarray(r["out"]) for r in res.results], axis=0)
    return out.astype(np.float32)
```

```python
import contextlib
import math
import numpy as np
import concourse.bass as bass
import concourse.mybir as mybir
from concourse.bass_utils import run_bass_kernel_spmd

F32 = mybir.dt.float32
BF16 = mybir.dt.bfloat16
AF = mybir.ActivationFunctionType
ALU = mybir.AluOpType
AX = mybir.AxisListType

import os as _os
EPOCH = int(_os.environ.get("K_EPOCH", "160"))
N_DMA_SEM = 24


class Buf:
    __slots__ = ("name", "w", "r")

    def __init__(self, name):
        self.name = name
        self.w = None
        self.r = {}


class KB:
    MAXEP = 16

    def __init__(self):
        self.nc = bass.Bass("TRN2", target_bir_lowering=False)
        nc = self.nc
        self.es = contextlib.ExitStack()
        self.eng = {"pe": nc.tensor, "act": nc.scalar, "dve": nc.vector, "pool": nc.gpsimd, "sp": nc.sync}
        self.cnt = {k: 0 for k in self.eng}
        self.sems = {}
        for e in ("pe", "act", "dve", "pool"):
            for ep in range(self.MAXEP):
                self.sems[(e, ep)] = self.es.enter_context(nc.semaphore(f"s_{e}_{ep}"))
        self.waited = {}
        self.bufs = {}
        self.dma_sems = [self.es.enter_context(nc.semaphore(f"dq{i}")) for i in range(N_DMA_SEM)]
        self.dma_tgt = [0] * N_DMA_SEM
        self.dma_rr = 0
        self.bar = [[self.es.enter_context(nc.semaphore(f"bar{p}{q}")) for q in range(2)] for p in range(2)]
        self.bgen = 0
        self.n_rb = 0
        self.inputs = {}
        self.outputs = {}
        self.same_engine_sync = True
        self.n_ins = 0
        self.pe_open = False

    def inp(self, name, shape, dt=F32):
        t = self.nc.dram_tensor(name, list(shape), dt, kind="ExternalInput")
        self.inputs[name] = (tuple(shape), dt)
        return t.ap()

    def outp(self, name, shape, dt=F32):
        t = self.nc.dram_tensor(name, list(shape), dt, kind="ExternalOutput")
        self.outputs[name] = (tuple(shape), dt)
        return t.ap()

    def dram(self, name, shape, dt=F32):
        return self.nc.dram_tensor(name, list(shape), dt, kind="Internal").ap()

    def sb(self, st, name, shape, dt=F32):
        self.uid = getattr(self, "uid", 0) + 1
        return st.enter_context(self.nc.sbuf_tensor(f"{name}_{self.uid}", list(shape), dt))

    def ps(self, st, name, shape, dt=F32):
        self.uid = getattr(self, "uid", 0) + 1
        return st.enter_context(self.nc.psum_tensor(f"{name}_{self.uid}", list(shape), dt))

    def _sem(self, e, ep):
        return self.sems[(e, ep)]

    def _buf(self, ap):
        name = ap if isinstance(ap, str) else getattr(ap, "tensor", ap).name
        b = self.bufs.get(name)
        if b is None:
            b = self.bufs[name] = Buf(name)
        return b

    def _wait(self, e, ev, force=False):
        kind = ev[0]
        if kind == "E":
            _, pe_, ep, val = ev
            if not force and pe_ == e and (e == "pe" or not self.same_engine_sync):
                return
            key = (e, "E", pe_, ep)
            if self.waited.get(key, 0) >= val:
                return
            self.eng[e].wait_ge(self._sem(pe_, ep), val)
            self.waited[key] = val
        else:
            _, slot, val = ev
            key = (e, "D", slot)
            if self.waited.get(key, 0) >= val:
                return
            self.eng[e].wait_ge(self.dma_sems[slot], val)
            self.waited[key] = val

    def _deps(self, e, reads, writes):
        for b in reads:
            if b.w is not None:
                self._wait(e, b.w)
            if b.name.startswith("psb"):
                for k_, ev in b.r.items():
                    if not (ev[0] == "E" and ev[1] == e):
                        self._wait(e, ev)
        for b in writes:
            if b.w is not None:
                self._wait(e, b.w)
            for ev in b.r.values():
                self._wait(e, ev)

    def _commit(self, ev, rkey, reads, writes):
        for b in reads:
            b.r[rkey] = ev
        for b in writes:
            b.w = ev
            b.r = {}

    def _maybe_reset(self, e=None):
        lim = EPOCH * (self.MAXEP - 1)
        if any(c >= lim for c in self.cnt.values()) or max(self.dma_tgt) >= 208:
            self.barrier(reset=True)

    def op(self, e, fn, reads, writes):
        self._maybe_reset()
        reads = [self._buf(x) for x in reads]
        writes = [self._buf(x) for x in writes]
        self._deps(e, reads, writes)
        ins = fn(self.eng[e])
        c = self.cnt[e]
        ep, val = c // EPOCH, c % EPOCH + 1
        ins.then_inc(self._sem(e, ep), 1)
        self.cnt[e] = c + 1
        self.n_ins += 1
        ev = ("E", e, ep, val)
        self._commit(ev, ("E", e), reads, writes)
        return ev

    def dma(self, q, out, in_, extra_r=(), extra_w=()):
        self._maybe_reset()
        reads = [self._buf(in_)] + [self._buf(x) for x in extra_r]
        writes = [self._buf(out)] + [self._buf(x) for x in extra_w]
        self._deps(q, reads, writes)
        slot = self.dma_rr
        self.dma_rr = (self.dma_rr + 1) % N_DMA_SEM
        if self.dma_tgt[slot] > 0:
            self._wait(q, ("D", slot, self.dma_tgt[slot]))
        ins = self.eng[q].dma_start(out=out, in_=in_)
        self.dma_tgt[slot] += 16
        ins.then_inc(self.dma_sems[slot], 16)
        self.n_ins += 1
        ev = ("D", slot, self.dma_tgt[slot])
        self._commit(ev, ("D", slot), reads, writes)
        return ev

    def init_staging(self, st, n=4, width=512):
        self.stg = [self.sb(st, f"stg{i}", [128, width], F32) for i in range(n)]
        self.stg_w = width
        self.stg_i = 0

    def load_cast(self, dst, src, parts=128):
        n = dst.shape[-1]
        assert len(dst.shape) == 2 and len(src.shape) == 2
        for c0 in range(0, n, self.stg_w):
            c1 = min(n, c0 + self.stg_w)
            sg = self.stg[self.stg_i % len(self.stg)]
            self.stg_i += 1
            self.dma("sp", sg[0:parts, 0:c1 - c0], src[:, c0:c1])
            self.copy(dst[:, c0:c1], sg[0:parts, 0:c1 - c0], eng=("dve" if self.stg_i % 2 else "act"))

    def barrier(self, reset=False):
        evs = []
        for e, c in self.cnt.items():
            if c > 0:
                cc = c - 1
                evs.append(("E", e, cc // EPOCH, cc % EPOCH + 1))
        for s, t in enumerate(self.dma_tgt):
            if t > 0:
                evs.append(("D", s, t))
        for e in self.eng:
            for ev in evs:
                self._wait(e, ev, force=True)
        for b in self.bufs.values():
            b.w = None
            b.r = {}
        if not reset:
            return
        self.n_rb += 1
        p = self.bgen % 2
        self.bgen += 1
        A, R = self.bar[p]
        A2, R2 = self.bar[1 - p]
        master = "sp"
        others = [e for e in self.eng if e != master]
        for e in others:
            self.eng[e].sem_inc(A, 1)
        m = self.eng[master]
        m.wait_ge(A, len(others))
        for sem in list(self.sems.values()) + self.dma_sems + [A2, R2]:
            m.sem_clear(sem)
        m.sem_inc(R, 1)
        for e in others:
            self.eng[e].wait_ge(R, 1)
        self.cnt = {k: 0 for k in self.eng}
        self.dma_tgt = [0] * N_DMA_SEM
        self.waited = {}

    def pe_fence(self):
        c = self.cnt["pe"]
        if c > 0:
            cc = c - 1
            self._wait("pe", ("E", "pe", cc // EPOCH, cc % EPOCH + 1), force=True)

    def finish(self):
        self.barrier()

    def mm(self, out, lhsT, rhs, start=True, stop=True, **kw):
        return self.op("pe", lambda e: e.matmul(out, lhsT, rhs, start=start, stop=stop, **kw), [lhsT, rhs], [out])

    def tr(self, out, in_, ident):
        return self.op("pe", lambda e: e.transpose(out, in_, ident), [in_, ident], [out])

    def act(self, out, in_, func, bias=None, scale=None, accum=None, eng="act"):
        kw = {}
        rd = [in_]
        if bias is not None:
            kw["bias"] = bias
            if not isinstance(bias, (int, float)):
                rd.append(bias)
        if scale is not None:
            kw["scale"] = scale
            if not isinstance(scale, (int, float)):
                rd.append(scale)
        wr = [out]
        if accum is not None:
            kw["accum_out"] = accum
            wr.append(accum)
        return self.op(eng, lambda e: e.activation(out=out, in_=in_, func=func, **kw), rd, wr)

    def tt(self, out, a, b, op, eng="dve"):
        return self.op(eng, lambda e: e.tensor_tensor(out=out, in0=a, in1=b, op=op), [a, b], [out])

    def ts(self, out, a, s1, op0, s2=None, op1=None, eng="dve"):
        rd = [a] + [s for s in (s1, s2) if s is not None and not isinstance(s, (int, float))]
        if op1 is None:
            return self.op(eng, lambda e: e.tensor_scalar(out=out, in0=a, scalar1=s1, scalar2=None, op0=op0), rd, [out])
        return self.op(eng, lambda e: e.tensor_scalar(out=out, in0=a, scalar1=s1, scalar2=s2, op0=op0, op1=op1), rd, [out])

    def stt(self, out, a, s, b, op0, op1, eng="dve"):
        rd = [a, b] + ([] if isinstance(s, (int, float)) else [s])
        return self.op(eng, lambda e: e.scalar_tensor_tensor(out=out, in0=a, scalar=s, in1=b, op0=op0, op1=op1), rd, [out])

    def copy(self, out, in_, eng="dve"):
        if eng == "act":
            return self.op("act", lambda e: e.copy(out=out, in_=in_), [in_], [out])
        return self.op(eng, lambda e: e.tensor_copy(out=out, in_=in_), [in_], [out])

    def rsqrt(self, out, in_, scale, bias):
        self.act(out, in_, AF.Sqrt, bias=bias, scale=scale)
        return self.op("dve", lambda e: e.reciprocal(out=out, in_=out), [out], [out])

    def rsqrt_act(self, out, in_, scale, bias):
        self.act(out, in_, AF.Ln, bias=bias, scale=scale)
        return self.act(out, out, AF.Exp, scale=-0.5)

    def scan(self, out, d0, d1, init, op0, op1):
        rd = [d0, d1] + ([] if isinstance(init, (int, float)) else [init])
        return self.op("dve", lambda e: e.tensor_tensor_scan(out=out, data0=d0, data1=d1, initial=init, op0=op0, op1=op1), rd, [out])

    def memset(self, ap, val, eng="dve"):
        return self.op(eng, lambda e: e.memset(ap, val), [], [ap])

    def reduce(self, out, in_, op, axis=AX.X, eng="dve"):
        return self.op(eng, lambda e: e.tensor_reduce(out=out, in_=in_, axis=axis, op=op), [in_], [out])


D = 1024
SEQ = 2048
NM = 16
L = SEQ + NM
DEPTH = 2
A_TOT, B_TOT, C_TOT = 1152, 1536, 800
IN_COLS = 3488
NE, CAP, FF = 16, 258, 1408
TT = [(0, 16)] + [(16 + 128 * j, 144 + 128 * j) for j in range(16)]
CH = [(0, 16)] + [(16 + 64 * i, 80 + 64 * i) for i in range(32)]
NB = [(0, 512), (512, 1024), (1024, 1536), (1536, 2048), (2048, 2064)]
EPS = 1e-6


def _consts():
    c = {}
    c["ident"] = np.eye(128, dtype=np.float32)
    return c


class Model:
    def __init__(self, debug=None, layers=(0, 1), phases="ABCDEGZ", bstop=0, bdirs=2, n_experts=NE, h_init=None):
        self.n_experts = n_experts
        self.h_init = h_init
        self.kb = KB()
        self.bstop = bstop
        self.bdirs = bdirs
        self.debug = debug or ()
        self.layers = layers
        self.phases = phases
        self.build()

    def build(self):
        kb = self.kb
        self.top = contextlib.ExitStack()
        top = self.top
        ph = self.phases
        self.x = kb.inp("x", [SEQ, D])
        self.meta = kb.inp("meta", [NM, D])
        self.c_ident = kb.inp("c_ident", [128, 128])
        self.norm1_bc = kb.inp("norm1_bc", [DEPTH, 128, D])
        if any(p in ph for p in "BCD"):
            self.w_in = kb.inp("w_in", [DEPTH, D, IN_COLS])
        if "B" in ph:
            self.rw_cols = kb.inp("rw_cols", [DEPTH, 128, C_NCOL])
            self.rw_wup = kb.inp("rw_wup", [DEPTH, 128, 256])
            self.rw_aup = kb.inp("rw_aup", [DEPTH, 128, 256])
            self.rw_gup = kb.inp("rw_gup", [DEPTH, 128, 256])
            self.c_blockones = kb.inp("c_blockones", [128, 128])
            self.c_masks = kb.inp("c_masks", [64, 2, 6, 256])
            self.c_rm = kb.inp("c_rm", [128, 512])
        if "C" in ph:
            self.c_cos = kb.inp("c_cos", [128, L])
            self.c_sin = kb.inp("c_sin", [128, L])
            self.c_rotperm = kb.inp("c_rotperm", [128, 128])
            self.df_lam_bc = kb.inp("df_lam_bc", [DEPTH, 128, 256])
            self.df_subln_bc = kb.inp("df_subln_bc", [DEPTH, 128, 128])
        if "D" in ph:
            if "B" not in ph:
                self.c_masks = kb.inp("c_masks", [64, 2, 6, 256])
            self.c_kvmask = kb.inp("c_kvmask", [128, 256])
            self.c_headmask = kb.inp("c_headmask", [128, 4])
            self.c_rmL = kb.inp("c_rmL", [128, L])
            self.gl_cols = kb.inp("gl_cols", [DEPTH, 128, 2])
            self.gl_norm_bc = kb.inp("gl_norm_bc", [DEPTH, 64, 256])
            self.gl_gate_up = kb.inp("gl_gate_up", [DEPTH, 2, 16, 128])
        if "E" in ph:
            self.w_out = kb.inp("w_out", [DEPTH, D, D])
        if "G" in ph:
            self.norm2_bc = kb.inp("norm2_bc", [DEPTH, 128, D])
            self.router = kb.inp("router", [DEPTH, D, NE])
            self.e_w1 = kb.inp("e_w1", [DEPTH, NE, D, FF])
            self.e_w3 = kb.inp("e_w3", [DEPTH, NE, D, FF])
            self.e_w2 = kb.inp("e_w2", [DEPTH, NE, FF, D])
            self.c_iota_c = kb.inp("c_iota_c", [128, CAP])
            self.c_iota_p = kb.inp("c_iota_p", [128, 3])
        if "Z" in ph:
            self.final_bc = kb.inp("final_bc", [128, D])
            self.out = kb.outp("out", [SEQ, D])
        self.dbg_out = {}
        for nm, shp, dt in (("route", [2, 128, 17, 16], F32), ("h", [L, D], F32), ("pT", [128, 9, L], BF16), ("ofm", [128, 2, L], F32), ("uT", [128, 8, L], BF16), ("mixT", [128, 8, L], BF16)):
            if nm in self.debug:
                self.dbg_out[nm] = kb.outp("dbg_" + nm, shp, dt)
        self.h = kb.dram("h_scr", [L, D])
        self.uT = kb.dram("uT_scr", [128, 8, L], BF16)
        self.mixT = kb.dram("mixT_scr", [128, 8, L], BF16)
        self.ident_bf = kb.sb(top, "ident_bf", [128, 128], BF16)
        self.ident_f = kb.sb(top, "ident_f", [128, 128], F32)
        kb.init_staging(top)
        kb.load_cast(self.ident_bf[:, :], self.c_ident[:, :])
        kb.dma("sp", self.ident_f[:], self.c_ident[:, :])
        self.psb = [kb.ps(top, f"psb{i}", [128, 512], F32) for i in range(8)]
        if self.h_init:
            self.h_in = kb.inp("h_in", [L, D])
            kb.dma("sp", self.h[:, :], self.h_in[:, :])
        else:
            kb.dma("sp", self.h[0:NM, :], self.meta[:, :])
            kb.dma("sp", self.h[NM:L, :], self.x[:, :])
        for l in self.layers:
            if "A" in ph:
                self.phase_norm(l, self.norm1_bc, self.uT)
            if "B" in ph:
                self.phase_rwkv(l)
            if "C" in ph:
                self.phase_attn(l)
            if "D" in ph:
                self.phase_gla(l)
            if "E" in ph:
                self.phase_outproj(l)
            if "G" in ph:
                self.phase_moe(l)
        if "Z" in ph:
            self.phase_final()
        kb.barrier()
        if "h" in self.debug:
            kb.dma("sp", self.dbg_out["h"][:, :], self.h[:, :])
        if "uT" in self.debug:
            kb.dma("sp", self.dbg_out["uT"][:, :, :], self.uT[:, :, :])
        if "mixT" in self.debug:
            for (c0, c1, pp) in ((0, 2, "B"), (2, 6, "C"), (6, 8, "D")):
                if pp in ph:
                    kb.dma("sp", self.dbg_out["mixT"][:, c0:c1, :], self.mixT[:, c0:c1, :])
        kb.finish()

    def phase_norm(self, l, g_bc_in, uT_out):
        kb = self.kb
        kb.barrier()
        with contextlib.ExitStack() as st:
            g_bc = kb.sb(st, "A_g", [128, D], F32)
            kb.dma("sp", g_bc[:], g_bc_in[l, :, :])
            uT_sb = kb.sb(st, "A_uT", [128, 8, L], BF16)
            hts = [kb.sb(st, f"A_h{i}", [128, D], F32) for i in range(3)]
            uns = [kb.sb(st, f"A_un{i}", [128, D], BF16) for i in range(2)]
            junk = kb.sb(st, "A_junk", [128, D], BF16)
            ssqs = [kb.sb(st, f"A_ssq{i}", [128, 1], F32) for i in range(3)]
            pst = [kb.ps(st, f"A_pst{i}", [128, 8, 128], BF16) for i in range(2)] if False else None
            for i, (t0, t1) in enumerate(TT):
                R = t1 - t0
                ht = hts[i % 3]
                un = uns[i % 2]
                ssq = ssqs[i % 3]
                kb.dma("sp", ht[:R, :], self.h[t0:t1, :])
                kb.act(junk[:R, :], ht[:R, :], AF.Square, accum=ssq[:R, :])
                kb.rsqrt(ssq[:R, :], ssq[:R, :], 1.0 / D, EPS)
                kb.stt(un[:R, :], ht[:R, :], ssq[:R, 0:1], g_bc[:R, :], ALU.mult, ALU.mult)
                ps = self.psb[i % 2].bitcast(BF16)
                for c in range(8):
                    kb.tr(ps[:, c * 128:c * 128 + R], un[:R, c * 128:(c + 1) * 128], self.ident_bf[:R, :R])
                psv = ps.rearrange("p (c t) -> p c t", c=8)
                kb.copy(uT_sb[:, :, t0:t1], psv[:, :, 0:R], eng="act" if i % 2 else "dve")
            kb.dma("sp", uT_out[:, :, :], uT_sb[:, :, :])
        kb.barrier()


def _chunkcols(v):
    v = np.asarray(v, np.float32)
    n = v.shape[-1] // 128
    return np.moveaxis(v.reshape(v.shape[:-1] + (n, 128)), -1, 0)


def _prep_shared(inputs):
    m = {}
    m["meta"] = np.ascontiguousarray(inputs["meta"])
    m["c_ident"] = np.eye(128, dtype=np.float32)
    m["norm1_bc"] = np.ascontiguousarray(np.broadcast_to(inputs["norm1_g"][:, None, :], (DEPTH, 128, D)))
    m["w_in"] = np.ascontiguousarray(inputs["w_in"])
    cols = np.zeros((DEPTH, 128, C_NCOL), np.float32)
    for l in range(DEPTH):
        sh = _chunkcols(inputs["rw_shift"][l])
        cols[l, :, C_MU0:C_MU0 + 9] = sh[:, 0]
        cols[l, :, C_MU1:C_MU1 + 9] = sh[:, 1]
        cols[l, :, C_W0:C_W0 + 4] = _chunkcols(inputs["rw_w0"][l]).reshape(128, 4)
        cols[l, :, C_A0:C_A0 + 4] = _chunkcols(inputs["rw_a0"][l]).reshape(128, 4)
        for nm, ci in (("rw_k_k", C_KK), ("rw_k_a", C_KA), ("rw_r_k", C_RK), ("rw_gn_g", C_GNG), ("rw_gn_b", C_GNB)):
            cols[l, :, ci:ci + 2] = _chunkcols(inputs[nm][l])
    m["rw_cols"] = cols
    m["rw_wup"] = np.ascontiguousarray(inputs["rw_w_up"].reshape(DEPTH, 128, 256))
    m["rw_aup"] = np.ascontiguousarray(inputs["rw_a_up"].reshape(DEPTH, 128, 256))
    m["rw_gup"] = np.ascontiguousarray(inputs["rw_g_up"])
    m.update(_rwkv_consts())
    m.update(_attn_consts())
    m["df_lam_bc"] = np.ascontiguousarray(np.broadcast_to(inputs["df_lam"].reshape(DEPTH, 1, 256), (DEPTH, 128, 256)))
    m["df_subln_bc"] = np.ascontiguousarray(np.broadcast_to(inputs["df_subln_g"][:, None, :], (DEPTH, 128, 128)))
    m["w_out"] = np.ascontiguousarray(inputs["w_out"])
    m.update(_gla_consts())
    m.update(_moe_consts())
    m["norm2_bc"] = np.ascontiguousarray(np.broadcast_to(inputs["norm2_g"][:, None, :], (DEPTH, 128, D)))
    m["final_bc"] = np.ascontiguousarray(np.broadcast_to(inputs["final_g"][None, :], (128, D)))
    for k_ in ("router", "e_w1", "e_w3", "e_w2"):
        if k_ in inputs:
            m[k_] = np.ascontiguousarray(inputs[k_])
    m["gl_cols"] = np.ascontiguousarray(np.transpose(inputs["gl_gate_b"], (0, 2, 1)))
    m["gl_norm_bc"] = np.ascontiguousarray(np.broadcast_to(np.tile(inputs["gl_norm_g"], (1, 4))[:, None, :], (DEPTH, 64, 256)))
    m["gl_gate_up"] = np.ascontiguousarray(inputs["gl_gate_up"])
    return m


def _prep_inputs(inputs, b, shared=None):
    m = dict(shared if shared is not None else _prep_shared(inputs))
    m["x"] = np.ascontiguousarray(inputs["x"][b])
    return m


C_MU0, C_MU1, C_W0, C_A0, C_KK, C_KA, C_RK, C_GNG, C_GNB, C_NCOL = 0, 9, 18, 22, 26, 28, 30, 32, 34, 36
A_DECAY_SCALE = 0.6065306597126334
GROUPS = [[0]] + [list(range(1 + 4 * g, 5 + 4 * g)) for g in range(8)]
PBLK = [(0, 16)] + [(16 + 512 * b, 16 + 512 * (b + 1)) for b in range(4)]


def _rwkv_consts():
    c = {}
    bo = np.zeros((128, 128), np.float32)
    bo[:64, :64] = 1
    bo[64:, 64:] = 1
    c["c_blockones"] = bo
    i = np.arange(64)
    U = (i[:, None] < i[None, :]).astype(np.float32)
    Lo = (i[:, None] > i[None, :]).astype(np.float32)
    Ui = (i[:, None] <= i[None, :]).astype(np.float32)
    Li = (i[:, None] >= i[None, :]).astype(np.float32)
    I = np.eye(64, dtype=np.float32)
    t4 = lambda m: np.tile(m, (1, 4))
    m = np.zeros((64, 2, 6, 256), np.float32)
    m[:, 0, 0], m[:, 0, 1], m[:, 0, 2], m[:, 0, 3], m[:, 0, 4], m[:, 0, 5] = t4(-U), t4(-Lo), t4(U), t4(Ui), t4(Ui), t4(I)
    m[:, 1, 0], m[:, 1, 1], m[:, 1, 2], m[:, 1, 3], m[:, 1, 4], m[:, 1, 5] = t4(-Lo), t4(-U), t4(Lo), t4(Li), t4(Li), t4(I)
    c["c_masks"] = m
    rm = np.ones((128, 512), np.float32)
    rm[:, ::64] = 0.0
    c["c_rm"] = rm
    return c


def phase_rwkv(self, l):
    kb = self.kb
    kb.barrier()
    psb = self.psb
    pi = [0]

    def nps():
        pi[0] = (pi[0] + 1) % 8
        return psb[pi[0]]

    with contextlib.ExitStack() as st:
        cols = kb.sb(st, "B_cols", [128, C_NCOL], F32)
        kb.dma("sp", cols[:], self.rw_cols[l, :, :])
        muc = kb.sb(st, "B_muc", [128, 9], F32)
        kb.ts(muc[:], cols[:, C_MU0:C_MU0 + 9], -1.0, ALU.mult, 1.0, ALU.add)
        kb.tt(muc[:], muc[:], cols[:, C_MU1:C_MU1 + 9], ALU.subtract)
        omka = kb.sb(st, "B_omka", [128, 2], F32)
        kb.ts(omka[:], cols[:, C_KA:C_KA + 2], -1.0, ALU.mult, 1.0, ALU.add)
        wup = kb.sb(st, "B_wup", [128, 256], BF16)
        aup = kb.sb(st, "B_aup", [128, 256], BF16)
        gup = kb.sb(st, "B_gup", [128, 256], BF16)
        kb.load_cast(wup[:, :], self.rw_wup[l, :, :])
        kb.load_cast(aup[:, :], self.rw_aup[l, :, :])
        kb.load_cast(gup[:, :], self.rw_gup[l, :, :])
        bo = kb.sb(st, "B_bo", [128, 128], BF16)
        kb.load_cast(bo[:, :], self.c_blockones[:, :])
        masks = kb.sb(st, "B_masks", [64, 2, 6, 256], BF16)
        for dd in range(2):
            kb.load_cast(masks[:, dd, :, :].rearrange("p a b -> p (a b)"), self.c_masks[:, dd, :, :].rearrange("p a b -> p (a b)"), parts=64)
        rm = kb.sb(st, "B_rm", [128, 512], F32)
        kb.dma("sp", rm[:], self.c_rm[:, :])

        pT = kb.sb(st, "B_pT", [128, 9, L], BF16)
        with contextlib.ExitStack() as s1:
            uT = kb.sb(s1, "B_uT", [128, 8, L], BF16)
            kb.dma("sp", uT[:], self.uT[:, :, :])
            wa = kb.sb(s1, "B_wa", [128, 8, A_TOT], BF16)
            for c in range(8):
                kb.load_cast(wa[:, c, :], self.w_in[l, c * 128:(c + 1) * 128, 0:A_TOT])
            tmp = [kb.sb(s1, f"B_tmp{i}", [128, 512], F32) for i in range(2)]
            k = 0
            import os
            BIS = os.environ.get("K_BISECT", "")
            for j in range(9 if not BIS else int(BIS)):
                mu0 = cols[:, C_MU0 + j:C_MU0 + j + 1]
                mu1 = cols[:, C_MU1 + j:C_MU1 + j + 1]
                for b in range(5):
                    o0 = 510 * b
                    o1 = min(o0 + 510, L)
                    lo, hi = max(o0 - 1, 0), min(o1 + 1, L)
                    n, no, off = hi - lo, o1 - o0, o0 - lo
                    ps = nps()
                    for c in range(8):
                        kb.mm(ps[:, 0:n], wa[:, c, j * 128:(j + 1) * 128], uT[:, c, lo:hi], start=(c == 0), stop=(c == 7))
                    t = tmp[k % 2]
                    k += 1
                    kb.ts(t[:, 0:no], ps[:, off:off + no], muc[:, j:j + 1], ALU.mult)
                    a = 1 if o0 == 0 else 0
                    kb.stt(t[:, a:no], ps[:, off - 1 + a:off - 1 + no], mu0, t[:, a:no], ALU.mult, ALU.add)
                    nn = no - 1 if o1 == L else no
                    kb.stt(pT[:, j, o0:o0 + nn], ps[:, off + 1:off + 1 + nn], mu1, t[:, 0:nn], ALU.mult, ALU.add)
                    if nn < no:
                        kb.copy(pT[:, j, L - 1:L], t[:, no - 1:no])
        kb.barrier()
        if "pT" in self.debug:
            kb.dma("sp", self.dbg_out["pT"][:, :, :], pT[:, :, :])
            kb.barrier()
        if self.bstop == 1:
            return

        tw = kb.sb(st, "B_tw", [128, L], BF16)
        sg = kb.sb(st, "B_sg", [128, L], BF16)
        kb.act(tw[:], pT[:, 6, :], AF.Tanh)
        kb.act(sg[:], pT[:, 8, :], AF.Sigmoid)
        kk = kb.sb(st, "B_kk", [128, 2, L], BF16)
        gate = kb.sb(st, "B_gate", [128, 2, L], BF16)
        kdsum = kb.sb(st, "B_kdsum", [128, 2, L], BF16)
        o_fm = kb.sb(st, "B_ofm", [128, 2, L], F32)
        kb.memset(o_fm[:], 0.0)
        with contextlib.ExitStack() as s2:
            kkr = kb.sb(s2, "B_kkr", [128, L], BF16)
            sq = kb.sb(s2, "B_sq", [128, L], BF16)
            rn = [kb.sb(s2, f"B_rn{i}", [128, 512], F32) for i in range(2)]
            k = 0
            for j in range(2):
                kb.ts(kkr[:], pT[:, 2 + j, :], cols[:, C_KK + j:C_KK + j + 1], ALU.mult)
                kb.tt(sq[:], kkr[:], kkr[:], ALU.mult)
                for (b0, b1) in NB:
                    n = b1 - b0
                    ps = nps()
                    kb.mm(ps[:, 0:n], bo[:], sq[:, b0:b1])
                    r_ = rn[k % 2]
                    k += 1
                    kb.rsqrt_act(r_[:, 0:n], ps[:, 0:n], 1.0, 1e-30)
                    kb.tt(kk[:, j, b0:b1], kkr[:, b0:b1], r_[:, 0:n], ALU.mult)
                for (b0, b1) in NB:
                    n = b1 - b0
                    ps = nps()
                    kb.mm(ps[:, 0:n], gup[:, j * 128:(j + 1) * 128], sg[:, b0:b1])
                    kb.copy(gate[:, j, b0:b1], ps[:, 0:n], eng="act")
        kb.barrier()
        if self.bstop == 2:
            return

        for d in range(self.bdirs):
            with contextlib.ExitStack() as sd:
                PhiT = kb.sb(sd, "B_PhiT", [128, 33, 2, 128], BF16)
                Hs = kb.sb(sd, "B_Hs", [128, 34, 2, 128], BF16)
                QT = kb.sb(sd, "B_QT", [128, 2, L], BF16)
                WCc = kb.sb(sd, "B_WCc", [128, 2, 33], F32)
                fmn = ["ApT", "BpT", "CpT", "RpT", "BnT"]
                fm = {nm: kb.sb(sd, f"B_{nm}", [128, 2, 512], BF16) for nm in fmn}
                tf = {nm: kb.sb(sd, f"B_t_{nm}", [128, 512], F32) for nm in ["lw", "pre", "xp", "TP", "TX", "ex"]}
                tb = {nm: kb.sb(sd, f"B_t_{nm}", [128, 512], BF16) for nm in ["icl", "kd", "ka", "tb"]}
                NS = 3
                tm_all = [kb.sb(sd, f"B_tm{s}", [64, 4, 256], BF16) for s in range(NS)]
                Vpad = [kb.sb(sd, f"B_Vpad{s}", [64, 4, 128], BF16) for s in range(NS)]
                PP = [[kb.sb(sd, f"B_PP{s}_{q}", [64, 2, 256], BF16) for q in range(2)] for s in range(NS)]
                XX = [[kb.sb(sd, f"B_XX{s}_{q}", [64, 256], BF16) for q in range(2)] for s in range(NS)]
                MP = [kb.sb(sd, f"B_MP{s}", [64, 3, 256], BF16) for s in range(NS)]
                M2Vn = [kb.sb(sd, f"B_M2Vn{s}", [64, 256], BF16) for s in range(NS)]
                GU = [kb.sb(sd, f"B_GU{s}", [64, 2, 256], BF16) for s in range(NS)]
                GUpad = [kb.sb(sd, f"B_GUpad{s}", [64, 2, 4, 128], BF16) for s in range(NS)]
                for s in range(NS):
                    kb.memset(Vpad[s][:], 0.0)
                    kb.memset(GUpad[s][:], 0.0)
                kb.memset(Hs[:, 0 if d == 0 else 33, :, :], 0.0)

                for bi, (b0, b1) in enumerate(PBLK):
                    n = b1 - b0
                    chunks = [0] if bi == 0 else list(range(1 + 8 * (bi - 1), 9 + 8 * (bi - 1)))
                    for j in range(2):
                        lw, pre, xp, TP, TX, ex = (tf[x] for x in ["lw", "pre", "xp", "TP", "TX", "ex"])
                        icl, kd, ka, tbb = (tb[x] for x in ["icl", "kd", "ka", "tb"])
                        ps = nps()
                        kb.mm(ps[:, 0:n], wup[d * 64:(d + 1) * 64, j * 128:(j + 1) * 128], tw[d * 64:(d + 1) * 64, b0:b1])
                        kb.act(lw[:, 0:n], ps[:, 0:n], AF.Sigmoid, bias=cols[:, C_W0 + d * 2 + j:C_W0 + d * 2 + j + 1])
                        kb.ts(lw[:, 0:n], lw[:, 0:n], -A_DECAY_SCALE, ALU.mult)
                        ps = nps()
                        kb.mm(ps[:, 0:n], aup[d * 64:(d + 1) * 64, j * 128:(j + 1) * 128], pT[d * 64:(d + 1) * 64, 7, b0:b1])
                        kb.act(icl[:, 0:n], ps[:, 0:n], AF.Sigmoid, bias=cols[:, C_A0 + d * 2 + j:C_A0 + d * 2 + j + 1])
                        kb.ts(tbb[:, 0:n], icl[:, 0:n], cols[:, C_KA + j:C_KA + j + 1], ALU.mult, omka[:, j:j + 1], ALU.add)
                        kb.tt(kd[:, 0:n], tbb[:, 0:n], pT[:, 2 + j, b0:b1], ALU.mult)
                        kb.tt(ka[:, 0:n], kk[:, j, b0:b1], icl[:, 0:n], ALU.mult)
                        if d == 0:
                            kb.copy(kdsum[:, j, b0:b1], kd[:, 0:n])
                        else:
                            kb.tt(kdsum[:, j, b0:b1], kdsum[:, j, b0:b1], kd[:, 0:n], ALU.add)
                        kb.scan(pre[:, 0:n], rm[:, 0:n], lw[:, 0:n], 0.0, ALU.mult, ALU.add)
                        kb.tt(xp[:, 0:n], pre[:, 0:n], lw[:, 0:n], ALU.subtract)
                        if bi == 0:
                            tot = pre[:, n - 1:n].to_broadcast([128, n])
                            kb.tt(TP[:, 0:n], tot, pre[:, 0:n], ALU.subtract)
                            kb.tt(TX[:, 0:n], tot, xp[:, 0:n], ALU.subtract)
                            kb.act(WCc[:, j, 0:1], pre[:, n - 1:n], AF.Exp)
                        else:
                            v3 = lambda t: t[:, 0:512].rearrange("p (n c) -> p n c", c=64)
                            tot = v3(pre)[:, :, 63:64].to_broadcast([128, 8, 64])
                            kb.tt(v3(TP), tot, v3(pre), ALU.subtract)
                            kb.tt(v3(TX), tot, v3(xp), ALU.subtract)
                            kb.act(WCc[:, j, chunks[0]:chunks[0] + 8], v3(pre)[:, :, 63], AF.Exp)
                        r_ = pT[:, 0 + j, b0:b1]
                        kkj = kk[:, j, b0:b1]
                        if d == 0:
                            srcs = dict(E1=(TP, 1.0), E2=(TP, -1.0), E2w=(TX, -1.0), E3=(pre, 1.0), E3w=(xp, 1.0))
                        else:
                            srcs = dict(E1=(xp, 1.0), E2=(xp, -1.0), E2w=(pre, -1.0), E3=(TX, 1.0), E3w=(TP, 1.0))
                        kb.act(ex[:, 0:n], srcs["E1"][0][:, 0:n], AF.Exp, scale=srcs["E1"][1])
                        kb.tt(fm["ApT"][:, j, 0:n], ka[:, 0:n], ex[:, 0:n], ALU.mult)
                        kb.tt(fm["CpT"][:, j, 0:n], kd[:, 0:n], ex[:, 0:n], ALU.mult)
                        kb.act(ex[:, 0:n], srcs["E2"][0][:, 0:n], AF.Exp, scale=srcs["E2"][1])
                        kb.tt(fm["RpT"][:, j, 0:n], r_, ex[:, 0:n], ALU.mult)
                        kb.act(ex[:, 0:n], srcs["E2w"][0][:, 0:n], AF.Exp, scale=srcs["E2w"][1])
                        kb.tt(fm["BpT"][:, j, 0:n], kkj, ex[:, 0:n], ALU.mult)
                        kb.act(ex[:, 0:n], srcs["E3"][0][:, 0:n], AF.Exp, scale=srcs["E3"][1])
                        kb.tt(QT[:, j, b0:b1], r_, ex[:, 0:n], ALU.mult)
                        kb.act(ex[:, 0:n], srcs["E3w"][0][:, 0:n], AF.Exp, scale=srcs["E3w"][1])
                        kb.stt(fm["BnT"][:, j, 0:n], kkj, -1.0, ex[:, 0:n], ALU.mult, ALU.mult)

                    for g0 in range(0, len(chunks), NS):
                        grp = chunks[g0:g0 + NS]
                        info = []
                        for s, cn in enumerate(grp):
                            c0, c1 = CH[cn]
                            info.append((s, cn, c1 - c0, c0 - b0))
                        hv = lambda ap, C: ap.rearrange("p (h c) -> p h c", h=4)[:, :, 0:C]
                        for (s, cn, C, o) in info:
                            ps = nps().bitcast(BF16)
                            for ti, nm in enumerate(["BnT", "ApT", "CpT", None]):
                                for j in range(2):
                                    src = fm[nm][:, j, o:o + C] if nm else pT[:, 4 + j, b0 + o:b0 + o + C]
                                    kb.tr(ps[0:C, ti * 256 + j * 128:ti * 256 + (j + 1) * 128], src, self.ident_bf[:, :])
                            kb.copy(tm_all[s][0:C, :, :], ps[0:C, :].rearrange("p (a b) -> p a b", a=4), eng="act")
                            for hp in range(2):
                                vsrc = tm_all[s][0:C, 3, :].rearrange("p (j q k) -> p j q k", j=2, q=2)[:, :, hp, :]
                                vdst = Vpad[s][0:C, :, :].rearrange("p (j q) m -> p j q m", q=2)[:, :, hp, hp * 64:(hp + 1) * 64]
                                kb.copy(vdst, vsrc, eng="dve")
                        for (s, cn, C, o) in info:
                            pa, pb, pc = nps(), nps(), nps()
                            for h in (0, 2, 1, 3):
                                j, q = h // 2, (h % 2) * 64
                                if h == 1:
                                    kb.pe_fence()
                                A_ = fm["ApT"][q:q + 64, j, o:o + C]
                                B_ = fm["BpT"][q:q + 64, j, o:o + C]
                                C_ = fm["CpT"][q:q + 64, j, o:o + C]
                                R_ = fm["RpT"][q:q + 64, j, o:o + C]
                                kb.mm(pa[0:C, h * 64:h * 64 + C], A_, B_)
                                kb.mm(pa[0:C, 256 + h * 64:256 + h * 64 + C], B_, A_)
                                kb.mm(pb[0:C, h * 64:h * 64 + C], C_, B_)
                                kb.mm(pb[0:C, 256 + h * 64:256 + h * 64 + C], A_, R_)
                                kb.mm(pc[0:C, h * 64:h * 64 + C], C_, R_)
                            P0 = PP[s][0]
                            kb.tt(hv(P0[0:C, 0, :], C), hv(pa[0:C, 0:256], C), hv(masks[0:C, d, 0, :], C), ALU.mult)
                            kb.tt(hv(P0[0:C, 1, :], C), hv(pa[0:C, 256:512], C), hv(masks[0:C, d, 1, :], C), ALU.mult)
                            kb.tt(hv(XX[s][0][0:C, :], C), hv(P0[0:C, 0, :], C), hv(masks[0:C, d, 5, :], C), ALU.add)
                            kb.tt(hv(MP[s][0:C, 0, :], C), hv(pb[0:C, 0:256], C), hv(masks[0:C, d, 2, :], C), ALU.mult)
                            kb.tt(hv(MP[s][0:C, 1, :], C), hv(pb[0:C, 256:512], C), hv(masks[0:C, d, 3, :], C), ALU.mult)
                            kb.tt(hv(MP[s][0:C, 2, :], C), hv(pc[0:C, 0:256], C), hv(masks[0:C, d, 4, :], C), ALU.mult)
                        nlev = 5 if len(grp) > 1 or grp[0] != 0 else 3
                        for lev in range(nlev):
                            cur, nxt = lev % 2, (lev + 1) % 2
                            last = lev == nlev - 1
                            for (s, cn, C, o) in info:
                                ps = nps()
                                Pc, Pn = PP[s][cur], PP[s][nxt]
                                for h in range(4):
                                    P_ = Pc[0:C, 0, h * 64:h * 64 + C]
                                    PT_ = Pc[0:C, 1, h * 64:h * 64 + C]
                                    kb.mm(ps[0:C, 256 + h * 64:256 + h * 64 + C], P_, PT_)
                                    if not last:
                                        kb.mm(ps[0:C, h * 64:h * 64 + C], PT_, P_)
                                if last:
                                    kb.copy(hv(Pn[0:C, 1, :], C), hv(ps[0:C, 256:512], C), eng="act")
                                else:
                                    kb.copy(Pn[0:C, :, :].rearrange("p a (h c) -> p a h c", h=4)[:, :, :, 0:C],
                                            ps[0:C, :].rearrange("p (a h c) -> p a h c", a=2, h=4)[:, :, :, 0:C], eng="act")
                            for (s, cn, C, o) in info:
                                ps = nps()
                                Pn = PP[s][nxt]
                                Xc, Xn = XX[s][cur], XX[s][nxt]
                                for h in range(4):
                                    sl = slice(h * 64, h * 64 + C)
                                    kb.mm(ps[0:C, sl], self.ident_bf[0:C, 0:C], Xc[0:C, sl], start=True, stop=False)
                                    kb.mm(ps[0:C, sl], Pn[0:C, 1, sl], Xc[0:C, sl], start=False, stop=True)
                                kb.copy(hv(Xn[0:C, :], C), hv(ps[0:C, 0:256], C), eng="dve")
                        xf = nlev % 2
                        for (s, cn, C, o) in info:
                            ps = nps()
                            for h in range(4):
                                sl = slice(h * 64, h * 64 + C)
                                kb.mm(ps[0:C, h * 64:(h + 1) * 64], MP[s][0:C, 0, sl], tm_all[s][0:C, 3, h * 64:(h + 1) * 64])
                            kb.act(M2Vn[s][0:C, :], ps[0:C, 0:256], AF.Copy, scale=-1.0)
                        for (s, cn, C, o) in info:
                            ps = nps()
                            TT_ = XX[s][xf]
                            for h in range(4):
                                sl = slice(h * 64, h * 64 + C)
                                kb.mm(ps[0:C, h * 64:(h + 1) * 64], TT_[0:C, sl], tm_all[s][0:C, 0, h * 64:(h + 1) * 64])
                                kb.mm(ps[0:C, 256 + h * 64:256 + (h + 1) * 64], TT_[0:C, sl], M2Vn[s][0:C, h * 64:(h + 1) * 64])
                            kb.copy(GU[s][0:C, :, :], ps[0:C, :].rearrange("p (a b) -> p a b", a=2), eng="act")
                            for hp in range(2):
                                src = GU[s][0:C, :, :].rearrange("p a (j q k) -> p a j q k", j=2, q=2)[:, :, :, hp, :]
                                dst = GUpad[s][0:C, :, :, :].rearrange("p a (j q) m -> p a j q m", q=2)[:, :, :, hp, hp * 64:(hp + 1) * 64]
                                kb.copy(dst, src, eng="dve")
                        for (s, cn, C, o) in info:
                            ps = nps()
                            for j in range(2):
                                pr = slice(j * 128, (j + 1) * 128)
                                kb.mm(ps[:, j * 128:(j + 1) * 128], GU[s][0:C, 0, pr], tm_all[s][0:C, 1, pr])
                                kb.mm(ps[:, 256 + j * 128:256 + (j + 1) * 128], tm_all[s][0:C, 1, pr], GU[s][0:C, 1, pr], start=True, stop=False)
                                kb.mm(ps[:, 256 + j * 128:256 + (j + 1) * 128], tm_all[s][0:C, 2, pr], tm_all[s][0:C, 3, pr], start=False, stop=True)
                            bo2 = bo[:, :].to_broadcast([128, 128]) if False else None
                            hslot = cn + 1 if d == 0 else cn
                            for j in range(2):
                                kb.tt(PhiT[:, cn, j, :], ps[:, j * 128:(j + 1) * 128], bo[:, :], ALU.mult)
                                kb.stt(PhiT[:, cn, j, :], self.ident_bf[:, :], WCc[:, j, cn:cn + 1], PhiT[:, cn, j, :], ALU.mult, ALU.add)
                                kb.tt(Hs[:, hslot, j, :], ps[:, 256 + j * 128:256 + (j + 1) * 128], bo[:, :], ALU.mult)
                        for (s, cn, C, o) in info:
                            ps = nps()
                            for j in range(2):
                                for q in range(2):
                                    h = 2 * j + q
                                    kb.mm(ps[:, j * 64:j * 64 + C], GUpad[s][0:C, 0, h, :], MP[s][0:C, 1, h * 64:h * 64 + C],
                                          start=(q == 0), stop=(q == 1))
                            c0 = CH[cn][0]
                            qv = QT[:, :, c0:c0 + C]
                            kb.tt(qv, qv, ps[:, 0:128].rearrange("p (j c) -> p j c", j=2)[:, :, 0:C], ALU.add)
                        for (s, cn, C, o) in info:
                            ps = nps()
                            for j in range(2):
                                for q in range(2):
                                    h = 2 * j + q
                                    kb.mm(ps[:, j * 64:j * 64 + C], GUpad[s][0:C, 1, h, :], MP[s][0:C, 1, h * 64:h * 64 + C],
                                          start=(q == 0), stop=False)
                                    kb.mm(ps[:, j * 64:j * 64 + C], Vpad[s][0:C, h, :], MP[s][0:C, 2, h * 64:h * 64 + C],
                                          start=False, stop=(q == 1))
                            c0 = CH[cn][0]
                            ov = o_fm[:, :, c0:c0 + C]
                            kb.tt(ov, ov, ps[:, 0:128].rearrange("p (j c) -> p j c", j=2)[:, :, 0:C], ALU.add)

                order = list(range(33)) if d == 0 else list(range(32, -1, -1))
                for cn in order:
                    hin = cn if d == 0 else cn + 1
                    hout = cn + 1 if d == 0 else cn
                    ps = nps()
                    for j in range(2):
                        kb.mm(ps[:, j * 128:(j + 1) * 128], PhiT[:, cn, j, :], Hs[:, hin, j, :])
                    hv_ = Hs[:, hout, :, :]
                    kb.tt(hv_, hv_, ps[:, 0:256].rearrange("p (j m) -> p j m", j=2), ALU.add)
                for cn in range(33):
                    c0, c1 = CH[cn]
                    C = c1 - c0
                    hin = cn if d == 0 else cn + 1
                    ps = nps()
                    for j in range(2):
                        kb.mm(ps[:, j * 64:j * 64 + C], Hs[:, hin, j, :], QT[:, j, c0:c1])
                    ov = o_fm[:, :, c0:c1]
                    kb.tt(ov, ov, ps[:, 0:128].rearrange("p (j c) -> p j c", j=2)[:, :, 0:C], ALU.add)
            kb.barrier()
        if "ofm" in self.debug:
            kb.dma("sp", self.dbg_out["ofm"][:, :, :], o_fm[:, :, :])
            kb.barrier()
        if self.bstop == 3:
            return

        with contextlib.ExitStack() as s3:
            ob = kb.sb(s3, "B_ob", [128, L], BF16)
            cen = kb.sb(s3, "B_cen", [128, L], F32)
            sqb = kb.sb(s3, "B_sqb", [128, L], BF16)
            rstd = kb.sb(s3, "B_rstd", [128, L], F32)
            prod = kb.sb(s3, "B_prod", [128, L], BF16)
            ya = kb.sb(s3, "B_ya", [128, 2, L], BF16)
            for j in range(2):
                kb.copy(ob[:], o_fm[:, j, :], eng="act")
                for (b0, b1) in NB:
                    n = b1 - b0
                    ps = nps()
                    kb.mm(ps[:, 0:n], bo[:], ob[:, b0:b1])
                    kb.stt(cen[:, b0:b1], ps[:, 0:n], -1.0 / 64, o_fm[:, j, b0:b1], ALU.mult, ALU.add)
                kb.tt(sqb[:], cen[:], cen[:], ALU.mult)
                for (b0, b1) in NB:
                    n = b1 - b0
                    ps = nps()
                    kb.mm(ps[:, 0:n], bo[:], sqb[:, b0:b1])
                    kb.rsqrt_act(rstd[:, b0:b1], ps[:, 0:n], 1.0 / 64, 64e-5)
                kb.tt(cen[:], cen[:], rstd[:], ALU.mult)
                kb.ts(cen[:], cen[:], cols[:, C_GNG + j:C_GNG + j + 1], ALU.mult, cols[:, C_GNB + j:C_GNB + j + 1], ALU.add)
                kb.stt(prod[:], pT[:, 0 + j, :], cols[:, C_RK + j:C_RK + j + 1], kdsum[:, j, :], ALU.mult, ALU.mult)
                for (b0, b1) in NB:
                    n = b1 - b0
                    ps = nps()
                    kb.mm(ps[:, 0:n], bo[:], prod[:, b0:b1])
                    kb.tt(rstd[:, b0:b1], ps[:, 0:n], pT[:, 4 + j, b0:b1], ALU.mult)
                kb.tt(cen[:], cen[:], rstd[:], ALU.add)
                kb.tt(ya[:, j, :], cen[:], gate[:, j, :], ALU.mult)
            kb.dma("sp", self.mixT[:, 0:2, :], ya[:, :, :])
    kb.barrier()


Model.phase_rwkv = phase_rwkv


ROPE_THETA = 10000.0


def _attn_consts():
    c = {}
    pos = np.arange(L, dtype=np.float32)
    inv = (ROPE_THETA ** (-np.arange(32, dtype=np.float32) / 32)).astype(np.float32)
    ang = pos[None, :] * inv[:, None]
    cos = np.cos(ang).astype(np.float32)
    sin = np.sin(ang).astype(np.float32)
    c["c_cos"] = np.ascontiguousarray(np.tile(cos, (4, 1)))
    c["c_sin"] = np.ascontiguousarray(np.tile(sin, (4, 1)))
    P = np.zeros((128, 128), np.float32)
    for blk in range(2):
        o = blk * 64
        for i in range(32):
            P[o + i, o + 32 + i] = -1.0
            P[o + 32 + i, o + i] = 1.0
    c["c_rotperm"] = np.ascontiguousarray(P.T)
    return c


def phase_attn(self, l):
    kb = self.kb
    kb.barrier()
    psb = self.psb
    lam_init = 0.8 - 0.6 * math.exp(-0.3 * l)
    pi = [0]

    def nps():
        pi[0] = (pi[0] + 1) % 8
        return psb[pi[0]]

    with contextlib.ExitStack() as st:
        lamb = kb.sb(st, "C_lamb", [128, 256], F32)
        kb.dma("sp", lamb[:], self.df_lam_bc[l, :, :])
        sub_g = kb.sb(st, "C_subg", [128, 128], F32)
        kb.dma("sp", sub_g[:], self.df_subln_bc[l, :, :])
        kb.ts(sub_g[:], sub_g[:], 1.0 - lam_init, ALU.mult)
        lt = kb.sb(st, "C_lt", [128, 128], F32)
        ee = kb.sb(st, "C_ee", [128, 2], F32)
        kb.tt(lt[:, 0:64], lamb[:, 0:64], lamb[:, 64:128], ALU.mult)
        kb.tt(lt[:, 64:128], lamb[:, 128:192], lamb[:, 192:256], ALU.mult)
        kb.reduce(ee[:, :], lt[:, :].rearrange("p (a b) -> p a b", a=2), ALU.add)
        kb.act(ee[:, :], ee[:, :], AF.Exp)
        nlam = kb.sb(st, "C_nlam", [128, 1], F32)
        kb.tt(nlam[:], ee[:, 1:2], ee[:, 0:1], ALU.subtract)
        kb.ts(nlam[:], nlam[:], -lam_init, ALU.add)
        cos = kb.sb(st, "C_cos", [128, L], F32)
        sin = kb.sb(st, "C_sin", [128, L], F32)
        kb.dma("sp", cos[:], self.c_cos[:, :])
        kb.dma("sp", sin[:], self.c_sin[:, :])
        perm = kb.sb(st, "C_perm", [128, 128], BF16)
        kb.load_cast(perm[:, :], self.c_rotperm[:, :])
        qkT = kb.sb(st, "C_qkT", [128, 8, L], BF16)
        v_aug = kb.sb(st, "C_vaug", [128, 17, 4, 132], BF16)
        kb.memset(v_aug[:, :, :, 128:129], 1.0)
        with contextlib.ExitStack() as s1:
            uT = kb.sb(s1, "C_uT", [128, 8, L], BF16)
            kb.dma("sp", uT[:], self.uT[:, :, :])
            wb = kb.sb(s1, "C_wb", [128, 8, B_TOT], BF16)
            for c in range(8):
                kb.load_cast(wb[:, c, :], self.w_in[l, c * 128:(c + 1) * 128, A_TOT:A_TOT + B_TOT])
            raw = [kb.sb(s1, f"C_raw{i}", [128, 512], BF16) for i in range(2)]
            t1 = [kb.sb(s1, f"C_t1{i}", [128, 512], F32) for i in range(2)]
            t2 = [kb.sb(s1, f"C_t2{i}", [128, 512], F32) for i in range(2)]
            k = 0
            for jj in range(8):
                for (b0, b1) in NB:
                    n = b1 - b0
                    ps = nps()
                    for c in range(8):
                        kb.mm(ps[:, 0:n], wb[:, c, jj * 128:(jj + 1) * 128], uT[:, c, b0:b1], start=(c == 0), stop=(c == 7))
                    r_, a_, b_ = raw[k % 2], t1[k % 2], t2[k % 2]
                    k += 1
                    kb.act(r_[:, 0:n], ps[:, 0:n], AF.Copy, scale=(0.125 if jj < 4 else 1.0))
                    ps2 = nps()
                    kb.mm(ps2[:, 0:n], perm[:, :], r_[:, 0:n])
                    kb.tt(a_[:, 0:n], r_[:, 0:n], cos[:, b0:b1], ALU.mult)
                    kb.tt(b_[:, 0:n], ps2[:, 0:n], sin[:, b0:b1], ALU.mult)
                    kb.tt(qkT[:, jj, b0:b1], a_[:, 0:n], b_[:, 0:n], ALU.add)
            for i, (t0, t1_) in enumerate(TT):
                R = t1_ - t0
                ps = nps()
                for c in range(8):
                    kb.mm(ps[0:R, :], uT[:, c, t0:t1_], wb[:, c, 1024:1536], start=(c == 0), stop=(c == 7))
                kb.copy(v_aug[0:R, i, :, 0:128], ps[0:R, :].rearrange("p (h d) -> p h d", h=4), eng="act")
        kb.barrier()
        yb = kb.sb(st, "C_yb", [128, 4, L], BF16)
        pTs = [kb.sb(st, f"C_pT{i}", [128, 512], BF16) for i in range(3)]
        oc = [[kb.sb(st, f"C_oc{c}_{q}", [128, 132], F32) for q in range(4)] for c in range(2)]
        rr = kb.sb(st, "C_rr", [128, 4], F32)
        od = [kb.sb(st, f"C_od{q}", [128, 128], F32) for q in range(2)]
        ob = [kb.sb(st, f"C_ob{q}", [128, 128], BF16) for q in range(2)]
        junk = kb.sb(st, "C_junk", [128, 128], F32)
        ssq = kb.sb(st, "C_ssq", [128, 2], F32)
        S_banks = [psb[0], psb[1]]
        A_banks = [psb[2], psb[3], psb[4], psb[5]]
        T_banks = [psb[6], psb[7]]
        si = 0
        pk = 0
        dk = 0
        for h in range(4):
            for (b0, b1) in NB:
                nq = b1 - b0
                subs = [(q0, min(q0 + 128, nq)) for q0 in range(0, nq, 128)]
                for c in range(2):
                    kb.pe_fence()
                    for i, (t0, t1_) in enumerate(TT):
                        R = t1_ - t0
                        S = S_banks[si % 2]
                        si += 1
                        kb.mm(S[0:R, 0:nq], qkT[c * 64:(c + 1) * 64, 4 + h, t0:t1_], qkT[c * 64:(c + 1) * 64, h, b0:b1])
                        p_ = pTs[pk % 3]
                        pk += 1
                        kb.act(p_[0:R, 0:nq], S[0:R, 0:nq], AF.Exp)
                        for qi, (q0, q1) in enumerate(subs):
                            kb.mm(A_banks[qi][0:q1 - q0, 0:129], p_[0:R, q0:q1], v_aug[0:R, i, h, 0:129], start=(i == 0), stop=(i == 16))
                    for qi, (q0, q1) in enumerate(subs):
                        kb.copy(oc[c][qi][0:q1 - q0, 0:129], A_banks[qi][0:q1 - q0, 0:129], eng=("act" if qi % 2 else "dve"))
                for qi, (q0, q1) in enumerate(subs):
                    m = q1 - q0
                    o0, o1 = oc[0][qi], oc[1][qi]
                    d_ = od[dk % 2]
                    b_ = ob[dk % 2]
                    dk += 1
                    kb.op("dve", lambda e, o0=o0, m=m: e.reciprocal(out=rr[0:m, 0:1], in_=o0[0:m, 128:129]), [o0], [rr])
                    kb.op("dve", lambda e, o1=o1, m=m: e.reciprocal(out=rr[0:m, 1:2], in_=o1[0:m, 128:129]), [o1], [rr])
                    kb.tt(rr[0:m, 1:2], rr[0:m, 1:2], nlam[0:m, 0:1], ALU.mult)
                    kb.ts(d_[0:m, :], o0[0:m, 0:128], rr[0:m, 0:1], ALU.mult)
                    kb.stt(d_[0:m, :], o1[0:m, 0:128], rr[0:m, 1:2], d_[0:m, :], ALU.mult, ALU.add)
                    kb.act(junk[0:m, :], d_[0:m, :], AF.Square, accum=ssq[0:m, 0:1])
                    kb.rsqrt(ssq[0:m, 0:1], ssq[0:m, 0:1], 1.0 / 128, EPS)
                    kb.stt(b_[0:m, :], d_[0:m, :], ssq[0:m, 0:1], sub_g[0:m, :], ALU.mult, ALU.mult)
                    tb_ = T_banks[dk % 2].bitcast(BF16)
                    kb.tr(tb_[:, 0:m], b_[0:m, :], self.ident_bf[0:m, 0:m])
                    kb.copy(yb[:, h, b0 + q0:b0 + q1], tb_[:, 0:m], eng="act")
        kb.dma("sp", self.mixT[:, 2:6, :], yb[:, :, :])
    kb.barrier()


Model.phase_attn = phase_attn


def phase_outproj(self, l):
    kb = self.kb
    kb.barrier()
    with contextlib.ExitStack() as st:
        mixT = kb.sb(st, "E_mixT", [128, 8, L], BF16)
        kb.dma("sp", mixT[:], self.mixT[:, :, :])
        wo = kb.sb(st, "E_wo", [128, 8, D], BF16)
        for c in range(8):
            kb.load_cast(wo[:, c, :], self.w_out[l, c * 128:(c + 1) * 128, :])
        hts = [kb.sb(st, f"E_h{i}", [128, D], F32) for i in range(3)]
        k = 0
        for i, (t0, t1) in enumerate(TT):
            R = t1 - t0
            ht = hts[i % 3]
            kb.dma("sp", ht[0:R, :], self.h[t0:t1, :])
            for half in range(2):
                ps = self.psb[k % 4]
                k += 1
                for c in range(8):
                    kb.mm(ps[0:R, :], mixT[:, c, t0:t1], wo[:, c, half * 512:(half + 1) * 512], start=(c == 0), stop=(c == 7))
                kb.tt(ht[0:R, half * 512:(half + 1) * 512], ht[0:R, half * 512:(half + 1) * 512], ps[0:R, :], ALU.add)
            kb.dma("sp", self.h[t0:t1, :], ht[0:R, :])
    kb.barrier()


Model.phase_outproj = phase_outproj


def _gla_consts():
    c = {}
    kvm = np.zeros((128, 256), np.float32)
    hm = np.zeros((128, 4), np.float32)
    for h in range(4):
        kvm[h * 32:(h + 1) * 32, h * 64:(h + 1) * 64] = 1.0
        hm[h * 32:(h + 1) * 32, h] = 1.0
    c["c_kvmask"] = kvm
    c["c_headmask"] = hm
    rm = np.ones((128, L), np.float32)
    for (c0, c1) in CH:
        rm[:, c0] = 0.0
    c["c_rmL"] = rm
    return c


def phase_gla(self, l):
    kb = self.kb
    kb.barrier()
    psb = self.psb
    pi = [0]

    def nps():
        pi[0] = (pi[0] + 1) % 8
        return psb[pi[0]]

    with contextlib.ExitStack() as st:
        gcols = kb.sb(st, "D_gcols", [128, 2], F32)
        kb.dma("sp", gcols[:], self.gl_cols[l, :, :])
        kb.ts(gcols[:], gcols[:], -1.0, ALU.mult)
        ng = kb.sb(st, "D_ng", [64, 256], F32)
        kb.dma("sp", ng[:], self.gl_norm_bc[l, :, :])
        kvm = kb.sb(st, "D_kvm", [128, 256], F32)
        kb.dma("sp", kvm[:], self.c_kvmask[:, :])
        hm = kb.sb(st, "D_hm", [128, 4], F32)
        kb.dma("sp", hm[:], self.c_headmask[:, :])
        rmL = kb.sb(st, "D_rmL", [128, L], F32)
        kb.dma("sp", rmL[:], self.c_rmL[:, :])
        amask = kb.sb(st, "D_amask", [64, 2, 256], BF16)
        for d in range(2):
            kb.load_cast(amask[:, d, :], self.c_masks[:, d, 3, :], parts=64)
        gu = [kb.sb(st, f"D_gu{d}", [16, 128], BF16) for d in range(2)]
        for d in range(2):
            kb.load_cast(gu[d][:, :], self.gl_gate_up[l, d, :, :], parts=16)
        qT = kb.sb(st, "D_qT", [128, L], F32)
        kT = kb.sb(st, "D_kT", [128, L], F32)
        adT = [kb.sb(st, f"D_adT{d}", [16, L], BF16) for d in range(2)]
        v_tm = kb.sb(st, "D_vtm", [64, 33, 256], BF16)
        sg_tm = kb.sb(st, "D_sgtm", [64, 33, 256], BF16)
        with contextlib.ExitStack() as s1:
            uT = kb.sb(s1, "D_uT", [128, 8, L], BF16)
            kb.dma("sp", uT[:], self.uT[:, :, :])
            wc = kb.sb(s1, "D_wc", [128, 8, C_TOT], BF16)
            for c in range(8):
                kb.load_cast(wc[:, c, :], self.w_in[l, c * 128:(c + 1) * 128, A_TOT + B_TOT:IN_COLS])
            for (dst, col0, ncol, scale) in ((qT, 0, 128, 32 ** -0.5), (kT, 128, 128, 1.0), (adT[0], 768, 16, 1.0), (adT[1], 784, 16, 1.0)):
                for (b0, b1) in NB:
                    n = b1 - b0
                    ps = nps()
                    for c in range(8):
                        kb.mm(ps[0:ncol, 0:n], wc[:, c, col0:col0 + ncol], uT[:, c, b0:b1], start=(c == 0), stop=(c == 7))
                    kb.act(dst[0:ncol, b0:b1], ps[0:ncol, 0:n], AF.Copy, scale=scale)
            for n_, (c0, c1) in enumerate(CH):
                C = c1 - c0
                ps = nps()
                for c in range(8):
                    kb.mm(ps[0:C, :], uT[:, c, c0:c1], wc[:, c, 256:768], start=(c == 0), stop=(c == 7))
                kb.copy(v_tm[0:C, n_, :], ps[0:C, 0:256], eng="dve")
                kb.act(sg_tm[0:C, n_, :], ps[0:C, 256:512], AF.Silu)
        kb.barrier()
        qd = [kb.sb(st, f"D_qd{d}", [128, L], BF16) for d in range(2)]
        kim = [[kb.sb(st, f"D_kim{d}_{h}", [128, L], BF16) for h in range(4)] for d in range(2)]
        Ss = [kb.sb(st, f"D_Ss{d}", [128, 34, 256], BF16) for d in range(2)]
        with contextlib.ExitStack() as s2:
            lw = kb.sb(s2, "D_lw", [128, L], F32)
            pre = kb.sb(s2, "D_pre", [128, L], F32)
            a1 = kb.sb(s2, "D_a1", [128, L], F32)
            ex = kb.sb(s2, "D_ex", [128, L], F32)
            ki = kb.sb(s2, "D_ki", [128, L], BF16)
            ke = kb.sb(s2, "D_ke", [128, L], BF16)
            dec = kb.sb(s2, "D_dec", [128, 33], F32)
            ketm = [kb.sb(s2, f"D_ketm{i}", [64, 128], BF16) for i in range(2)]
            for d in range(2):
                for (b0, b1) in NB:
                    n = b1 - b0
                    ps = nps()
                    kb.mm(ps[:, 0:n], gu[d][:, :], adT[d][:, b0:b1])
                    kb.act(lw[:, b0:b1], ps[:, 0:n], AF.Exp, scale=-1.0, bias=gcols[:, d:d + 1])
                kb.act(lw[:], lw[:], AF.Ln, bias=1.0)
                kb.ts(lw[:], lw[:], -1.0 / 16.0, ALU.mult)
                kb.scan(pre[:], rmL[:], lw[:], 0.0, ALU.mult, ALU.add)
                v3 = lambda t: t[:, 16:L].rearrange("p (n c) -> p n c", c=64)
                tot3 = v3(pre)[:, :, 63:64].to_broadcast([128, 32, 64])
                tot0 = pre[:, 15:16].to_broadcast([128, 16])
                kb.act(dec[:, 0:1], pre[:, 15:16], AF.Exp)
                kb.act(dec[:, 1:33], v3(pre)[:, :, 63], AF.Exp)
                if d == 0:
                    kb.act(ex[:], pre[:], AF.Exp)
                    kb.tt(qd[d][:], qT[:], ex[:], ALU.mult)
                    kb.act(ex[:], pre[:], AF.Exp, scale=-1.0)
                    kb.tt(ki[:], kT[:], ex[:], ALU.mult)
                    kb.tt(v3(a1), tot3, v3(pre), ALU.subtract)
                    kb.tt(a1[:, 0:16], tot0, pre[:, 0:16], ALU.subtract)
                    kb.act(ex[:], a1[:], AF.Exp)
                    kb.tt(ke[:], kT[:], ex[:], ALU.mult)
                else:
                    kb.tt(pre[:], pre[:], lw[:], ALU.subtract)
                    kb.act(ex[:], pre[:], AF.Exp)
                    kb.tt(ke[:], kT[:], ex[:], ALU.mult)
                    kb.tt(a1[:], pre[:], lw[:], ALU.add)
                    a3 = v3(a1)[:, :, 63:64].to_broadcast([128, 32, 64])
                    a0 = a1[:, 15:16].to_broadcast([128, 16])
                    kb.tt(v3(lw), a3, v3(pre), ALU.subtract)
                    kb.tt(lw[:, 0:16], a0, pre[:, 0:16], ALU.subtract)
                    kb.act(ex[:], lw[:], AF.Exp)
                    kb.tt(qd[d][:], qT[:], ex[:], ALU.mult)
                    kb.act(ex[:], lw[:], AF.Exp, scale=-1.0)
                    kb.tt(ki[:], kT[:], ex[:], ALU.mult)
                for h in range(4):
                    kb.ts(kim[d][h][:], ki[:], hm[:, h:h + 1], ALU.mult)
                for n_, (c0, c1) in enumerate(CH):
                    C = c1 - c0
                    tb_ = nps().bitcast(BF16)
                    kb.tr(tb_[0:C, 0:128], ke[:, c0:c1], self.ident_bf[:, :])
                    kt_ = ketm[n_ % 2]
                    kb.copy(kt_[0:C, :], tb_[0:C, 0:128], eng="act")
                    ps = nps()
                    kb.mm(ps[:, 0:256], kt_[0:C, :], v_tm[0:C, n_, :])
                    kb.tt(Ss[d][:, (n_ + 1 if d == 0 else n_), :], ps[:, 0:256], kvm[:, :], ALU.mult)
                if d == 0:
                    kb.memset(Ss[d][:, 0, :], 0.0)
                    for n_ in range(33):
                        kb.stt(Ss[d][:, n_ + 1, :], Ss[d][:, n_, :], dec[:, n_:n_ + 1], Ss[d][:, n_ + 1, :], ALU.mult, ALU.add)
                else:
                    kb.memset(Ss[d][:, 33, :], 0.0)
                    for n_ in range(32, -1, -1):
                        kb.stt(Ss[d][:, n_, :], Ss[d][:, n_ + 1, :], dec[:, n_:n_ + 1], Ss[d][:, n_, :], ALU.mult, ALU.add)
        kb.barrier()
        yc = kb.sb(st, "D_yc", [128, 2, L], BF16)
        att = [[kb.sb(st, f"D_att{i}_{d}", [64, 4, 64], BF16) for d in range(2)] for i in range(2)]
        osb = [kb.sb(st, f"D_osb{i}", [64, 256], F32) for i in range(2)]
        sq = kb.sb(st, "D_sq", [64, 256], F32)
        ssq = [kb.sb(st, f"D_ssq{i}", [64, 4], F32) for i in range(2)]
        ycb = [kb.sb(st, f"D_ycb{i}", [64, 256], BF16) for i in range(2)]
        for n_, (c0, c1) in enumerate(CH):
            C = c1 - c0
            at = att[n_ % 2]
            for d in range(2):
                ps = nps()
                for h in range(4):
                    kb.mm(ps[0:C, h * 64:h * 64 + C], kim[d][h][:, c0:c1], qd[d][:, c0:c1])
                kb.tt(at[d][0:C, :, 0:C], ps[0:C, 0:256].rearrange("p (h c) -> p h c", h=4)[:, :, 0:C],
                      amask[0:C, d, :].rearrange("p (h c) -> p h c", h=4)[:, :, 0:C], ALU.mult)
            po = nps()
            for d in range(2):
                hin = n_ if d == 0 else n_ + 1
                kb.mm(po[0:C, 0:256], qd[d][:, c0:c1], Ss[d][:, hin, :], start=(d == 0), stop=False)
                for h in range(4):
                    kb.mm(po[0:C, h * 64:(h + 1) * 64], at[d][0:C, h, 0:C], v_tm[0:C, n_, h * 64:(h + 1) * 64],
                          start=False, stop=(d == 1 and h == 3))
            o_ = osb[n_ % 2]
            s_ = ssq[n_ % 2]
            y_ = ycb[n_ % 2]
            kb.copy(o_[0:C, :], po[0:C, 0:256], eng="act")
            kb.tt(sq[0:C, :], o_[0:C, :], o_[0:C, :], ALU.mult)
            kb.reduce(s_[0:C, :], sq[0:C, :].rearrange("p (h d) -> p h d", h=4), ALU.add)
            kb.rsqrt(s_[0:C, :], s_[0:C, :], 1.0 / 64, EPS)
            for h in range(4):
                hs = slice(h * 64, (h + 1) * 64)
                kb.stt(o_[0:C, hs], o_[0:C, hs], s_[0:C, h:h + 1], ng[0:C, hs], ALU.mult, ALU.mult)
            kb.tt(y_[0:C, :], o_[0:C, :], sg_tm[0:C, n_, :], ALU.mult)
            tb_ = nps().bitcast(BF16)
            for j in range(2):
                kb.tr(tb_[:, j * 64:j * 64 + C], y_[0:C, j * 128:(j + 1) * 128], self.ident_bf[0:C, 0:C])
            kb.copy(yc[:, :, c0:c1], tb_[:, 0:128].rearrange("p (j c) -> p j c", j=2)[:, :, 0:C], eng="act")
        kb.dma("sp", self.mixT[:, 6:8, :], yc[:, :, :])
    kb.barrier()


Model.phase_gla = phase_gla


def _moe_consts():
    c = {}
    c["c_iota_c"] = np.ascontiguousarray(np.broadcast_to(np.arange(CAP, dtype=np.float32)[None, :], (128, CAP)))
    ip = np.zeros((128, 3), np.float32)
    for cc in range(3):
        ip[:, cc] = np.arange(128) + 128 * cc
    c["c_iota_p"] = ip
    oh = np.zeros((16, 16, 128), np.float32)
    for e in range(16):
        oh[e, e, :] = 1.0
    c["c_onehot"] = oh
    return c


def phase_moe(self, l):
    kb = self.kb
    kb.barrier()
    psb = self.psb
    pi = [0]

    def nps():
        pi[0] = (pi[0] + 1) % 8
        return psb[pi[0]]

    with contextlib.ExitStack() as st:
        hacc = kb.sb(st, "G_hacc", [128, 17, D], F32)
        u2 = kb.sb(st, "G_u2", [128, 17, D], BF16)
        posm_tok = kb.sb(st, "G_posm", [128, 17, 16], F32)
        gate_tok = kb.sb(st, "G_gate", [128, 17, 16], F32)
        iota_c = kb.sb(st, "G_iotac", [128, CAP], F32)
        kb.dma("sp", iota_c[:], self.c_iota_c[:, :])
        iota_p = kb.sb(st, "G_iotap", [128, 3], F32)
        kb.dma("sp", iota_p[:], self.c_iota_p[:, :])
        with contextlib.ExitStack() as s1:
            g_bc = kb.sb(s1, "F_g", [128, D], F32)
            kb.dma("sp", g_bc[:], self.norm2_bc[l, :, :])
            rt = kb.sb(s1, "F_rt", [128, 8, 16], F32)
            kb.dma("sp", rt[:], self.router[l].rearrange("(c p) e -> p c e", p=128))
            un = [kb.sb(s1, f"F_un{i}", [128, D], F32) for i in range(2)]
            uTf = [kb.sb(s1, f"F_uTf{i}", [128, 8, 128], F32) for i in range(2)]
            junk = kb.sb(s1, "F_junk", [128, D], BF16)
            ssq = [kb.sb(s1, f"F_ssq{i}", [128, 1], F32) for i in range(2)]
            ex = [kb.sb(s1, f"F_ex{i}", [128, 16], F32) for i in range(2)]
            esum = [kb.sb(s1, f"F_es{i}", [128, 1], F32) for i in range(2)]
            aff = kb.sb(s1, "F_aff", [128, 17, 16], F32)
            affT = kb.sb(s1, "F_affT", [16, L], F32)
            posmT = kb.sb(s1, "F_posmT", [16, L], F32)
            for i, (t0, t1) in enumerate(TT):
                R = t1 - t0
                kb.dma("sp", hacc[0:R, i, :], self.h[t0:t1, :])
                u_ = un[i % 2]
                s_ = ssq[i % 2]
                kb.act(junk[0:R, :], hacc[0:R, i, :], AF.Square, accum=s_[0:R, :])
                kb.rsqrt(s_[0:R, :], s_[0:R, :], 1.0 / D, EPS)
                kb.stt(u_[0:R, :], hacc[0:R, i, :], s_[0:R, 0:1], g_bc[0:R, :], ALU.mult, ALU.mult)
                kb.copy(u2[0:R, i, :], u_[0:R, :], eng="act")
                pa, pb = nps(), nps()
                for c in range(8):
                    pp = pa if c < 4 else pb
                    kb.tr(pp[:, (c % 4) * 128:(c % 4) * 128 + R], u_[0:R, c * 128:(c + 1) * 128], self.ident_f[0:R, 0:R])
                tf_ = uTf[i % 2]
                kb.copy(tf_[:, 0:4, 0:R], pa[:, :].rearrange("p (c t) -> p c t", c=4)[:, :, 0:R], eng="dve")
                kb.copy(tf_[:, 4:8, 0:R], pb[:, :].rearrange("p (c t) -> p c t", c=4)[:, :, 0:R], eng="act")
                pl = nps()
                for c in range(8):
                    kb.mm(pl[0:R, 0:16], tf_[:, c, 0:R], rt[:, c, :], start=(c == 0), stop=(c == 7))
                e_ = ex[i % 2]
                es_ = esum[i % 2]
                kb.act(e_[0:R, :], pl[0:R, 0:16], AF.Exp, accum=es_[0:R, :])
                kb.op("dve", lambda e, es_=es_, R=R: e.reciprocal(out=es_[0:R, :], in_=es_[0:R, :]), [es_], [es_])
                kb.ts(aff[0:R, i, :], e_[0:R, :], es_[0:R, 0:1], ALU.mult)
                pt = nps()
                kb.tr(pt[0:16, 0:R], aff[0:R, i, :], self.ident_f[0:R, 0:R])
                kb.copy(affT[:, t0:t1], pt[0:16, 0:R], eng="act")
            work = kb.sb(s1, "F_work", [16, L], F32)
            m8 = kb.sb(s1, "F_m8", [16, 8], F32)
            kb.copy(work[:], affT[:], eng="dve")
            nit = (CAP + 7) // 8
            for it in range(nit):
                kb.op("dve", lambda e: e.max(out=m8[:, :], in_=work[:, :]), [work], [m8])
                rem = CAP - it * 8
                if rem < 8:
                    kb.memset(m8[:, rem:8], 0.0)
                kb.op("dve", lambda e: e.match_replace(out=work[:, :], in_to_replace=m8[:, :], in_values=work[:, :], imm_value=0.0), [m8, work], [work])
            gatesT = kb.sb(s1, "F_gatesT", [16, L], F32)
            maskT = kb.sb(s1, "F_maskT", [16, L], F32)
            ones = kb.sb(s1, "F_ones", [16, L], F32)
            kb.memset(ones[:], 1.0)
            kb.tt(gatesT[:], affT[:], work[:], ALU.subtract)
            kb.ts(maskT[:], gatesT[:], 0.0, ALU.is_gt)
            kb.scan(posmT[:], ones[:], maskT[:], 0.0, ALU.mult, ALU.add)
            kb.tt(posmT[:], posmT[:], maskT[:], ALU.mult)
            kb.ts(posmT[:], posmT[:], -1.0, ALU.add)
            for i, (t0, t1) in enumerate(TT):
                R = t1 - t0
                pt = nps()
                kb.tr(pt[0:R, 0:16], posmT[:, t0:t1], self.ident_f[0:16, 0:16])
                kb.tr(pt[0:R, 16:32], gatesT[:, t0:t1], self.ident_f[0:16, 0:16])
                kb.copy(posm_tok[0:R, i, :], pt[0:R, 0:16], eng="act")
                kb.copy(gate_tok[0:R, i, :], pt[0:R, 16:32], eng="dve")
        kb.barrier()
        if "route" in self.debug:
            kb.dma("sp", self.dbg_out["route"][0, :, :, :], posm_tok[:, :, :])
            kb.dma("sp", self.dbg_out["route"][1, :, :, :], gate_tok[:, :, :])
            kb.barrier()
        w1 = kb.sb(st, "G_w1", [128, 8, FF], BF16)
        w3 = kb.sb(st, "G_w3", [128, 8, FF], BF16)
        w2 = kb.sb(st, "G_w2", [128, 11, D], BF16)
        Sel = kb.sb(st, "G_Sel", [128, 17, CAP], BF16)
        SelTg = kb.sb(st, "G_SelTg", [128, 3, 1024], BF16)
        xy = kb.sb(st, "G_xy", [128, 3 * D], BF16)
        xT = xy[:, 0:8 * CAP].rearrange("p (c n) -> p c n", c=8)
        yb = xy[:, :].rearrange("p (c d) -> p c d", c=3)
        hT = kb.sb(st, "G_hT", [128, 11, CAP], BF16)
        sil = [kb.sb(st, f"G_sil{i}", [128, CAP], F32) for i in range(1)]
        CC = [(0, 86), (86, 172), (172, 258)]
        ne = self.n_experts
        for e in range(ne):
            for c in range(8):
                kb.load_cast(w1[:, c, :], self.e_w1[l, e, c * 128:(c + 1) * 128, :])
                kb.load_cast(w3[:, c, :], self.e_w3[l, e, c * 128:(c + 1) * 128, :])
            for fc in range(11):
                kb.load_cast(w2[:, fc, :], self.e_w2[l, e, fc * 128:(fc + 1) * 128, :])
            for i, (t0, t1) in enumerate(TT):
                R = t1 - t0
                kb.ts(Sel[0:R, i, :], iota_c[0:R, :], posm_tok[0:R, i, e:e + 1], ALU.is_equal)
            for c in range(8):
                ps = nps()
                for i, (t0, t1) in enumerate(TT):
                    R = t1 - t0
                    kb.mm(ps[:, 0:CAP], u2[0:R, i, c * 128:(c + 1) * 128], Sel[0:R, i, :], start=(i == 0), stop=(i == 16))
                kb.copy(xT[:, c, :], ps[:, 0:CAP], eng="act")
            for fc in range(11):
                p1, p3 = nps(), nps()
                for c in range(8):
                    kb.mm(p1[:, 0:CAP], w1[:, c, fc * 128:(fc + 1) * 128], xT[:, c, :], start=(c == 0), stop=(c == 7))
                for c in range(8):
                    kb.mm(p3[:, 0:CAP], w3[:, c, fc * 128:(fc + 1) * 128], xT[:, c, :], start=(c == 0), stop=(c == 7))
                s_ = sil[0]
                kb.act(s_[:, :], p1[:, 0:CAP], AF.Silu)
                kb.tt(hT[:, fc, :], s_[:, :], p3[:, 0:CAP], ALU.mult)
            for cc, (a0, a1) in enumerate(CC):
                m = a1 - a0
                for half in range(2):
                    ps = nps()
                    for fc in range(11):
                        kb.mm(ps[0:m, :], hT[:, fc, a0:a1], w2[:, fc, half * 512:(half + 1) * 512], start=(fc == 0), stop=(fc == 10))
                    kb.copy(yb[0:m, cc, half * 512:(half + 1) * 512], ps[0:m, :], eng="act")
            for g0 in range(0, 17, 8):
                tiles = list(range(g0, min(g0 + 8, 17)))
                for cc, (a0, a1) in enumerate(CC):
                    m = a1 - a0
                    tb_ = nps().bitcast(BF16)
                    for k_, i in enumerate(tiles):
                        t0, t1 = TT[i]
                        R = t1 - t0
                        kb.tr(tb_[0:m, k_ * 128:k_ * 128 + R], Sel[0:R, i, a0:a1], self.ident_bf[0:R, 0:R])
                    eg = "act" if cc % 2 else "dve"
                    if g0 == 0:
                        kb.copy(SelTg[0:m, cc, 0:16], tb_[0:m, 0:16], eng=eg)
                        kb.copy(SelTg[0:m, cc, 128:1024], tb_[0:m, 128:1024], eng=eg)
                    else:
                        kb.copy(SelTg[0:m, cc, 0:len(tiles) * 128], tb_[0:m, 0:len(tiles) * 128], eng=eg)
                for k_, i in enumerate(tiles):
                    t0, t1 = TT[i]
                    R = t1 - t0
                    for half in range(2):
                        ps = nps()
                        for cc, (a0, a1) in enumerate(CC):
                            m = a1 - a0
                            kb.mm(ps[0:R, :], SelTg[0:m, cc, k_ * 128:k_ * 128 + R], yb[0:m, cc, half * 512:(half + 1) * 512], start=(cc == 0), stop=(cc == 2))
                        hv = hacc[0:R, i, half * 512:(half + 1) * 512]
                        kb.stt(hv, ps[0:R, :], gate_tok[0:R, i, e:e + 1], hv, ALU.mult, ALU.add)
        for i, (t0, t1) in enumerate(TT):
            R = t1 - t0
            kb.dma("sp", self.h[t0:t1, :], hacc[0:R, i, :])
    kb.barrier()


Model.phase_moe = phase_moe


def phase_final(self):
    kb = self.kb
    kb.barrier()
    with contextlib.ExitStack() as st:
        g_bc = kb.sb(st, "Z_g", [128, D], F32)
        kb.dma("sp", g_bc[:], self.final_bc[:, :])
        hts = [kb.sb(st, f"Z_h{i}", [128, D], F32) for i in range(3)]
        junk = kb.sb(st, "Z_junk", [128, D], BF16)
        ssqs = [kb.sb(st, f"Z_ssq{i}", [128, 1], F32) for i in range(3)]
        for i, (t0, t1) in enumerate(TT):
            if i == 0:
                continue
            ht, ssq = hts[i % 3], ssqs[i % 3]
            kb.dma("sp", ht[:, :], self.h[t0:t1, :])
            kb.act(junk[:, :], ht[:, :], AF.Square, accum=ssq[:, :])
            kb.rsqrt(ssq[:, :], ssq[:, :], 1.0 / D, EPS)
            kb.stt(ht[:, :], ht[:, :], ssq[:, 0:1], g_bc[:, :], ALU.mult, ALU.mult)
            kb.dma("sp", self.out[t0 - NM:t1 - NM, :], ht[:, :])
    kb.barrier()


Model.phase_final = phase_final


_CACHE = {}


def kernel(**inputs):
    inputs = {k: np.asarray(v) for k, v in inputs.items()}
    if "model" not in _CACHE:
        _ph = _os.environ.get("K_PHASES", "")
        _CACHE["model"] = Model(phases=_ph) if _ph else Model()
    m = _CACHE["model"]
    shared = _prep_shared(inputs)
    B = inputs["x"].shape[0]
    in_maps = []
    for b in range(B):
        im = _prep_inputs(inputs, b, shared)
        in_maps.append({k: v for k, v in im.items() if k in m.kb.inputs})
    res = run_bass_kernel_spmd(m.kb.nc, in_maps, core_ids=list(range(B)))
    out = np.stack([np.asarray(r["out"]) for r in res.results], axis=0)
    return out.astype(np.float32)
```

```python
import contextlib
import math
import numpy as np
import concourse.bass as bass
import concourse.mybir as mybir
from concourse.bass_utils import run_bass_kernel_spmd

F32 = mybir.dt.float32
BF16 = mybir.dt.bfloat16
AF = mybir.ActivationFunctionType
ALU = mybir.AluOpType
AX = mybir.AxisListType

import os as _os
EPOCH = int(_os.environ.get("K_EPOCH", "160"))
N_DMA_SEM = 24


class Buf:
    __slots__ = ("name", "w", "r")

    def __init__(self, name):
        self.name = name
        self.w = None
        self.r = {}


class KB:
    MAXEP = 16

    def __init__(self):
        self.nc = bass.Bass("TRN2", target_bir_lowering=False)
        nc = self.nc
        self.es = contextlib.ExitStack()
        self.eng = {"pe": nc.tensor, "act": nc.scalar, "dve": nc.vector, "pool": nc.gpsimd, "sp": nc.sync}
        self.cnt = {k: 0 for k in self.eng}
        self.sems = {}
        for e in ("pe", "act", "dve", "pool"):
            for ep in range(self.MAXEP):
                self.sems[(e, ep)] = self.es.enter_context(nc.semaphore(f"s_{e}_{ep}"))
        self.waited = {}
        self.bufs = {}
        self.dma_sems = [self.es.enter_context(nc.semaphore(f"dq{i}")) for i in range(N_DMA_SEM)]
        self.dma_tgt = [0] * N_DMA_SEM
        self.dma_rr = 0
        self.bar = [[self.es.enter_context(nc.semaphore(f"bar{p}{q}")) for q in range(2)] for p in range(2)]
        self.bgen = 0
        self.n_rb = 0
        self.inputs = {}
        self.outputs = {}
        self.same_engine_sync = True
        self.n_ins = 0
        self.pe_open = False

    def inp(self, name, shape, dt=F32):
        t = self.nc.dram_tensor(name, list(shape), dt, kind="ExternalInput")
        self.inputs[name] = (tuple(shape), dt)
        return t.ap()

    def outp(self, name, shape, dt=F32):
        t = self.nc.dram_tensor(name, list(shape), dt, kind="ExternalOutput")
        self.outputs[name] = (tuple(shape), dt)
        return t.ap()

    def dram(self, name, shape, dt=F32):
        return self.nc.dram_tensor(name, list(shape), dt, kind="Internal").ap()

    def sb(self, st, name, shape, dt=F32):
        self.uid = getattr(self, "uid", 0) + 1
        return st.enter_context(self.nc.sbuf_tensor(f"{name}_{self.uid}", list(shape), dt))

    def ps(self, st, name, shape, dt=F32):
        self.uid = getattr(self, "uid", 0) + 1
        return st.enter_context(self.nc.psum_tensor(f"{name}_{self.uid}", list(shape), dt))

    def _sem(self, e, ep):
        return self.sems[(e, ep)]

    def _buf(self, ap):
        name = ap if isinstance(ap, str) else getattr(ap, "tensor", ap).name
        b = self.bufs.get(name)
        if b is None:
            b = self.bufs[name] = Buf(name)
        return b

    def _wait(self, e, ev, force=False):
        kind = ev[0]
        if kind == "E":
            _, pe_, ep, val = ev
            if not force and pe_ == e and (e == "pe" or not self.same_engine_sync):
                return
            key = (e, "E", pe_, ep)
            if self.waited.get(key, 0) >= val:
                return
            self.eng[e].wait_ge(self._sem(pe_, ep), val)
            self.waited[key] = val
        else:
            _, slot, val = ev
            key = (e, "D", slot)
            if self.waited.get(key, 0) >= val:
                return
            self.eng[e].wait_ge(self.dma_sems[slot], val)
            self.waited[key] = val

    def _deps(self, e, reads, writes):
        for b in reads:
            if b.w is not None:
                self._wait(e, b.w)
            if b.name.startswith("psb"):
                for k_, ev in b.r.items():
                    if not (ev[0] == "E" and ev[1] == e):
                        self._wait(e, ev)
        for b in writes:
            if b.w is not None:
                self._wait(e, b.w)
            for ev in b.r.values():
                self._wait(e, ev)

    def _commit(self, ev, rkey, reads, writes):
        for b in reads:
            b.r[rkey] = ev
        for b in writes:
            b.w = ev
            b.r = {}

    def _maybe_reset(self, e=None):
        lim = EPOCH * (self.MAXEP - 1)
        if any(c >= lim for c in self.cnt.values()) or max(self.dma_tgt) >= 208:
            self.barrier(reset=True)

    def op(self, e, fn, reads, writes):
        self._maybe_reset()
        reads = [self._buf(x) for x in reads]
        writes = [self._buf(x) for x in writes]
        self._deps(e, reads, writes)
        ins = fn(self.eng[e])
        c = self.cnt[e]
        ep, val = c // EPOCH, c % EPOCH + 1
        ins.then_inc(self._sem(e, ep), 1)
        self.cnt[e] = c + 1
        self.n_ins += 1
        ev = ("E", e, ep, val)
        self._commit(ev, ("E", e), reads, writes)
        return ev

    def dma(self, q, out, in_, extra_r=(), extra_w=()):
        self._maybe_reset()
        reads = [self._buf(in_)] + [self._buf(x) for x in extra_r]
        writes = [self._buf(out)] + [self._buf(x) for x in extra_w]
        self._deps(q, reads, writes)
        slot = self.dma_rr
        self.dma_rr = (self.dma_rr + 1) % N_DMA_SEM
        if self.dma_tgt[slot] > 0:
            self._wait(q, ("D", slot, self.dma_tgt[slot]))
        ins = self.eng[q].dma_start(out=out, in_=in_)
        self.dma_tgt[slot] += 16
        ins.then_inc(self.dma_sems[slot], 16)
        self.n_ins += 1
        ev = ("D", slot, self.dma_tgt[slot])
        self._commit(ev, ("D", slot), reads, writes)
        return ev

    def init_staging(self, st, n=4, width=512):
        self.stg = [self.sb(st, f"stg{i}", [128, width], F32) for i in range(n)]
        self.stg_w = width
        self.stg_i = 0

    def load_cast(self, dst, src, parts=128):
        n = dst.shape[-1]
        assert len(dst.shape) == 2 and len(src.shape) == 2
        for c0 in range(0, n, self.stg_w):
            c1 = min(n, c0 + self.stg_w)
            sg = self.stg[self.stg_i % len(self.stg)]
            self.stg_i += 1
            self.dma("sp", sg[0:parts, 0:c1 - c0], src[:, c0:c1])
            self.copy(dst[:, c0:c1], sg[0:parts, 0:c1 - c0], eng="act")

    def barrier(self, reset=False):
        evs = []
        for e, c in self.cnt.items():
            if c > 0:
                cc = c - 1
                evs.append(("E", e, cc // EPOCH, cc % EPOCH + 1))
        for s, t in enumerate(self.dma_tgt):
            if t > 0:
                evs.append(("D", s, t))
        for e in self.eng:
            for ev in evs:
                self._wait(e, ev, force=True)
        for b in self.bufs.values():
            b.w = None
            b.r = {}
        if not reset:
            return
        self.n_rb += 1
        p = self.bgen % 2
        self.bgen += 1
        A, R = self.bar[p]
        A2, R2 = self.bar[1 - p]
        master = "sp"
        others = [e for e in self.eng if e != master]
        for e in others:
            self.eng[e].sem_inc(A, 1)
        m = self.eng[master]
        m.wait_ge(A, len(others))
        for sem in list(self.sems.values()) + self.dma_sems + [A2, R2]:
            m.sem_clear(sem)
        m.sem_inc(R, 1)
        for e in others:
            self.eng[e].wait_ge(R, 1)
        self.cnt = {k: 0 for k in self.eng}
        self.dma_tgt = [0] * N_DMA_SEM
        self.waited = {}

    def pe_fence(self):
        c = self.cnt["pe"]
        if c > 0:
            cc = c - 1
            self._wait("pe", ("E", "pe", cc // EPOCH, cc % EPOCH + 1), force=True)

    def finish(self):
        self.barrier()

    def mm(self, out, lhsT, rhs, start=True, stop=True, **kw):
        return self.op("pe", lambda e: e.matmul(out, lhsT, rhs, start=start, stop=stop, **kw), [lhsT, rhs], [out])

    def tr(self, out, in_, ident):
        return self.op("pe", lambda e: e.transpose(out, in_, ident), [in_, ident], [out])

    def act(self, out, in_, func, bias=None, scale=None, accum=None, eng="act"):
        kw = {}
        rd = [in_]
        if bias is not None:
            kw["bias"] = bias
            if not isinstance(bias, (int, float)):
                rd.append(bias)
        if scale is not None:
            kw["scale"] = scale
            if not isinstance(scale, (int, float)):
                rd.append(scale)
        wr = [out]
        if accum is not None:
            kw["accum_out"] = accum
            wr.append(accum)
        return self.op(eng, lambda e: e.activation(out=out, in_=in_, func=func, **kw), rd, wr)

    def tt(self, out, a, b, op, eng="dve"):
        return self.op(eng, lambda e: e.tensor_tensor(out=out, in0=a, in1=b, op=op), [a, b], [out])

    def ts(self, out, a, s1, op0, s2=None, op1=None, eng="dve"):
        rd = [a] + [s for s in (s1, s2) if s is not None and not isinstance(s, (int, float))]
        if op1 is None:
            return self.op(eng, lambda e: e.tensor_scalar(out=out, in0=a, scalar1=s1, scalar2=None, op0=op0), rd, [out])
        return self.op(eng, lambda e: e.tensor_scalar(out=out, in0=a, scalar1=s1, scalar2=s2, op0=op0, op1=op1), rd, [out])

    def stt(self, out, a, s, b, op0, op1, eng="dve"):
        rd = [a, b] + ([] if isinstance(s, (int, float)) else [s])
        return self.op(eng, lambda e: e.scalar_tensor_tensor(out=out, in0=a, scalar=s, in1=b, op0=op0, op1=op1), rd, [out])

    def copy(self, out, in_, eng="dve"):
        if eng == "act":
            return self.op("act", lambda e: e.copy(out=out, in_=in_), [in_], [out])
        return self.op(eng, lambda e: e.tensor_copy(out=out, in_=in_), [in_], [out])

    def rsqrt(self, out, in_, scale, bias):
        self.act(out, in_, AF.Sqrt, bias=bias, scale=scale)
        return self.op("dve", lambda e: e.reciprocal(out=out, in_=out), [out], [out])

    def rsqrt_act(self, out, in_, scale, bias):
        self.act(out, in_, AF.Ln, bias=bias, scale=scale)
        return self.act(out, out, AF.Exp, scale=-0.5)

    def scan(self, out, d0, d1, init, op0, op1):
        rd = [d0, d1] + ([] if isinstance(init, (int, float)) else [init])
        return self.op("dve", lambda e: e.tensor_tensor_scan(out=out, data0=d0, data1=d1, initial=init, op0=op0, op1=op1), rd, [out])

    def memset(self, ap, val, eng="dve"):
        return self.op(eng, lambda e: e.memset(ap, val), [], [ap])

    def reduce(self, out, in_, op, axis=AX.X, eng="dve"):
        return self.op(eng, lambda e: e.tensor_reduce(out=out, in_=in_, axis=axis, op=op), [in_], [out])


D = 1024
SEQ = 2048
NM = 16
L = SEQ + NM
DEPTH = 2
A_TOT, B_TOT, C_TOT = 1152, 1536, 800
IN_COLS = 3488
NE, CAP, FF = 16, 258, 1408
TT = [(0, 16)] + [(16 + 128 * j, 144 + 128 * j) for j in range(16)]
CH = [(0, 16)] + [(16 + 64 * i, 80 + 64 * i) for i in range(32)]
NB = [(0, 512), (512, 1024), (1024, 1536), (1536, 2048), (2048, 2064)]
EPS = 1e-6


def _consts():
    c = {}
    c["ident"] = np.eye(128, dtype=np.float32)
    return c


class Model:
    def __init__(self, debug=None, layers=(0, 1), phases="ABCDEGZ", bstop=0, bdirs=2, n_experts=NE, h_init=None):
        self.n_experts = n_experts
        self.h_init = h_init
        self.kb = KB()
        self.bstop = bstop
        self.bdirs = bdirs
        self.debug = debug or ()
        self.layers = layers
        self.phases = phases
        self.build()

    def build(self):
        kb = self.kb
        self.top = contextlib.ExitStack()
        top = self.top
        ph = self.phases
        self.x = kb.inp("x", [SEQ, D])
        self.meta = kb.inp("meta", [NM, D])
        self.c_ident = kb.inp("c_ident", [128, 128])
        self.norm1_bc = kb.inp("norm1_bc", [DEPTH, 128, D])
        if any(p in ph for p in "BCD"):
            self.w_in = kb.inp("w_in", [DEPTH, D, IN_COLS])
        if "B" in ph:
            self.rw_cols = kb.inp("rw_cols", [DEPTH, 128, C_NCOL])
            self.rw_wup = kb.inp("rw_wup", [DEPTH, 128, 256])
            self.rw_aup = kb.inp("rw_aup", [DEPTH, 128, 256])
            self.rw_gup = kb.inp("rw_gup", [DEPTH, 128, 256])
            self.c_blockones = kb.inp("c_blockones", [128, 128])
            self.c_masks = kb.inp("c_masks", [64, 2, 6, 256])
            self.c_rm = kb.inp("c_rm", [128, 512])
        if "C" in ph:
            self.c_cos = kb.inp("c_cos", [128, L])
            self.c_sin = kb.inp("c_sin", [128, L])
            self.c_rotperm = kb.inp("c_rotperm", [128, 128])
            self.df_lam_bc = kb.inp("df_lam_bc", [DEPTH, 128, 256])
            self.df_subln_bc = kb.inp("df_subln_bc", [DEPTH, 128, 128])
        if "D" in ph:
            if "B" not in ph:
                self.c_masks = kb.inp("c_masks", [64, 2, 6, 256])
            self.c_kvmask = kb.inp("c_kvmask", [128, 256])
            self.c_headmask = kb.inp("c_headmask", [128, 4])
            self.c_rmL = kb.inp("c_rmL", [128, L])
            self.gl_cols = kb.inp("gl_cols", [DEPTH, 128, 2])
            self.gl_norm_bc = kb.inp("gl_norm_bc", [DEPTH, 64, 256])
            self.gl_gate_up = kb.inp("gl_gate_up", [DEPTH, 2, 16, 128])
        if "E" in ph:
            self.w_out = kb.inp("w_out", [DEPTH, D, D])
        if "G" in ph:
            self.norm2_bc = kb.inp("norm2_bc", [DEPTH, 128, D])
            self.router = kb.inp("router", [DEPTH, D, NE])
            self.e_w1 = kb.inp("e_w1", [DEPTH, NE, D, FF])
            self.e_w3 = kb.inp("e_w3", [DEPTH, NE, D, FF])
            self.e_w2 = kb.inp("e_w2", [DEPTH, NE, FF, D])
            self.c_iota_c = kb.inp("c_iota_c", [128, CAP])
            self.c_iota_p = kb.inp("c_iota_p", [128, 3])
        if "Z" in ph:
            self.final_bc = kb.inp("final_bc", [128, D])
            self.out = kb.outp("out", [SEQ, D])
        self.dbg_out = {}
        for nm, shp, dt in (("route", [2, 128, 17, 16], F32), ("h", [L, D], F32), ("pT", [128, 9, L], BF16), ("ofm", [128, 2, L], F32), ("uT", [128, 8, L], BF16), ("mixT", [128, 8, L], BF16)):
            if nm in self.debug:
                self.dbg_out[nm] = kb.outp("dbg_" + nm, shp, dt)
        self.h = kb.dram("h_scr", [L, D])
        self.uT = kb.dram("uT_scr", [128, 8, L], BF16)
        self.mixT = kb.dram("mixT_scr", [128, 8, L], BF16)
        self.ident_bf = kb.sb(top, "ident_bf", [128, 128], BF16)
        self.ident_f = kb.sb(top, "ident_f", [128, 128], F32)
        kb.init_staging(top)
        kb.load_cast(self.ident_bf[:, :], self.c_ident[:, :])
        kb.dma("sp", self.ident_f[:], self.c_ident[:, :])
        self.psb = [kb.ps(top, f"psb{i}", [128, 512], F32) for i in range(8)]
        if self.h_init:
            self.h_in = kb.inp("h_in", [L, D])
            kb.dma("sp", self.h[:, :], self.h_in[:, :])
        else:
            kb.dma("sp", self.h[0:NM, :], self.meta[:, :])
            kb.dma("sp", self.h[NM:L, :], self.x[:, :])
        for l in self.layers:
            if "A" in ph:
                self.phase_norm(l, self.norm1_bc, self.uT)
            if "B" in ph:
                self.phase_rwkv(l)
            if "C" in ph:
                self.phase_attn(l)
            if "D" in ph:
                self.phase_gla(l)
            if "E" in ph:
                self.phase_outproj(l)
            if "G" in ph:
                self.phase_moe(l)
        if "Z" in ph:
            self.phase_final()
        kb.barrier()
        if "h" in self.debug:
            kb.dma("sp", self.dbg_out["h"][:, :], self.h[:, :])
        if "uT" in self.debug:
            kb.dma("sp", self.dbg_out["uT"][:, :, :], self.uT[:, :, :])
        if "mixT" in self.debug:
            for (c0, c1, pp) in ((0, 2, "B"), (2, 6, "C"), (6, 8, "D")):
                if pp in ph:
                    kb.dma("sp", self.dbg_out["mixT"][:, c0:c1, :], self.mixT[:, c0:c1, :])
        kb.finish()

    def phase_norm(self, l, g_bc_in, uT_out):
        kb = self.kb
        kb.barrier()
        with contextlib.ExitStack() as st:
            g_bc = kb.sb(st, "A_g", [128, D], F32)
            kb.dma("sp", g_bc[:], g_bc_in[l, :, :])
            uT_sb = kb.sb(st, "A_uT", [128, 8, L], BF16)
            hts = [kb.sb(st, f"A_h{i}", [128, D], F32) for i in range(3)]
            uns = [kb.sb(st, f"A_un{i}", [128, D], BF16) for i in range(2)]
            junk = kb.sb(st, "A_junk", [128, D], BF16)
            ssqs = [kb.sb(st, f"A_ssq{i}", [128, 1], F32) for i in range(3)]
            pst = [kb.ps(st, f"A_pst{i}", [128, 8, 128], BF16) for i in range(2)] if False else None
            for i, (t0, t1) in enumerate(TT):
                R = t1 - t0
                ht = hts[i % 3]
                un = uns[i % 2]
                ssq = ssqs[i % 3]
                kb.dma("sp", ht[:R, :], self.h[t0:t1, :])
                kb.act(junk[:R, :], ht[:R, :], AF.Square, accum=ssq[:R, :])
                kb.rsqrt(ssq[:R, :], ssq[:R, :], 1.0 / D, EPS)
                kb.stt(un[:R, :], ht[:R, :], ssq[:R, 0:1], g_bc[:R, :], ALU.mult, ALU.mult)
                ps = self.psb[i % 2].bitcast(BF16)
                for c in range(8):
                    kb.tr(ps[:, c * 128:c * 128 + R], un[:R, c * 128:(c + 1) * 128], self.ident_bf[:R, :R])
                psv = ps.rearrange("p (c t) -> p c t", c=8)
                kb.copy(uT_sb[:, :, t0:t1], psv[:, :, 0:R], eng="act" if i % 2 else "dve")
            kb.dma("sp", uT_out[:, :, :], uT_sb[:, :, :])
        kb.barrier()


def _chunkcols(v):
    v = np.asarray(v, np.float32)
    n = v.shape[-1] // 128
    return np.moveaxis(v.reshape(v.shape[:-1] + (n, 128)), -1, 0)


def _prep_shared(inputs):
    m = {}
    m["meta"] = np.ascontiguousarray(inputs["meta"])
    m["c_ident"] = np.eye(128, dtype=np.float32)
    m["norm1_bc"] = np.ascontiguousarray(np.broadcast_to(inputs["norm1_g"][:, None, :], (DEPTH, 128, D)))
    m["w_in"] = np.ascontiguousarray(inputs["w_in"])
    cols = np.zeros((DEPTH, 128, C_NCOL), np.float32)
    for l in range(DEPTH):
        sh = _chunkcols(inputs["rw_shift"][l])
        cols[l, :, C_MU0:C_MU0 + 9] = sh[:, 0]
        cols[l, :, C_MU1:C_MU1 + 9] = sh[:, 1]
        cols[l, :, C_W0:C_W0 + 4] = _chunkcols(inputs["rw_w0"][l]).reshape(128, 4)
        cols[l, :, C_A0:C_A0 + 4] = _chunkcols(inputs["rw_a0"][l]).reshape(128, 4)
        for nm, ci in (("rw_k_k", C_KK), ("rw_k_a", C_KA), ("rw_r_k", C_RK), ("rw_gn_g", C_GNG), ("rw_gn_b", C_GNB)):
            cols[l, :, ci:ci + 2] = _chunkcols(inputs[nm][l])
    m["rw_cols"] = cols
    m["rw_wup"] = np.ascontiguousarray(inputs["rw_w_up"].reshape(DEPTH, 128, 256))
    m["rw_aup"] = np.ascontiguousarray(inputs["rw_a_up"].reshape(DEPTH, 128, 256))
    m["rw_gup"] = np.ascontiguousarray(inputs["rw_g_up"])
    m.update(_rwkv_consts())
    m.update(_attn_consts())
    m["df_lam_bc"] = np.ascontiguousarray(np.broadcast_to(inputs["df_lam"].reshape(DEPTH, 1, 256), (DEPTH, 128, 256)))
    m["df_subln_bc"] = np.ascontiguousarray(np.broadcast_to(inputs["df_subln_g"][:, None, :], (DEPTH, 128, 128)))
    m["w_out"] = np.ascontiguousarray(inputs["w_out"])
    m.update(_gla_consts())
    m.update(_moe_consts())
    m["norm2_bc"] = np.ascontiguousarray(np.broadcast_to(inputs["norm2_g"][:, None, :], (DEPTH, 128, D)))
    m["final_bc"] = np.ascontiguousarray(np.broadcast_to(inputs["final_g"][None, :], (128, D)))
    for k_ in ("router", "e_w1", "e_w3", "e_w2"):
        if k_ in inputs:
            m[k_] = np.ascontiguousarray(inputs[k_])
    m["gl_cols"] = np.ascontiguousarray(np.transpose(inputs["gl_gate_b"], (0, 2, 1)))
    m["gl_norm_bc"] = np.ascontiguousarray(np.broadcast_to(np.tile(inputs["gl_norm_g"], (1, 4))[:, None, :], (DEPTH, 64, 256)))
    m["gl_gate_up"] = np.ascontiguousarray(inputs["gl_gate_up"])
    return m


def _prep_inputs(inputs, b, shared=None):
    m = dict(shared if shared is not None else _prep_shared(inputs))
    m["x"] = np.ascontiguousarray(inputs["x"][b])
    return m


C_MU0, C_MU1, C_W0, C_A0, C_KK, C_KA, C_RK, C_GNG, C_GNB, C_NCOL = 0, 9, 18, 22, 26, 28, 30, 32, 34, 36
A_DECAY_SCALE = 0.6065306597126334
GROUPS = [[0]] + [list(range(1 + 4 * g, 5 + 4 * g)) for g in range(8)]
PBLK = [(0, 16)] + [(16 + 512 * b, 16 + 512 * (b + 1)) for b in range(4)]


def _rwkv_consts():
    c = {}
    bo = np.zeros((128, 128), np.float32)
    bo[:64, :64] = 1
    bo[64:, 64:] = 1
    c["c_blockones"] = bo
    i = np.arange(64)
    U = (i[:, None] < i[None, :]).astype(np.float32)
    Lo = (i[:, None] > i[None, :]).astype(np.float32)
    Ui = (i[:, None] <= i[None, :]).astype(np.float32)
    Li = (i[:, None] >= i[None, :]).astype(np.float32)
    I = np.eye(64, dtype=np.float32)
    t4 = lambda m: np.tile(m, (1, 4))
    m = np.zeros((64, 2, 6, 256), np.float32)
    m[:, 0, 0], m[:, 0, 1], m[:, 0, 2], m[:, 0, 3], m[:, 0, 4], m[:, 0, 5] = t4(-U), t4(-Lo), t4(U), t4(Ui), t4(Ui), t4(I)
    m[:, 1, 0], m[:, 1, 1], m[:, 1, 2], m[:, 1, 3], m[:, 1, 4], m[:, 1, 5] = t4(-Lo), t4(-U), t4(Lo), t4(Li), t4(Li), t4(I)
    c["c_masks"] = m
    rm = np.ones((128, 512), np.float32)
    rm[:, ::64] = 0.0
    c["c_rm"] = rm
    return c


def phase_rwkv(self, l):
    kb = self.kb
    kb.barrier()
    psb = self.psb
    pi = [0]

    def nps():
        pi[0] = (pi[0] + 1) % 8
        return psb[pi[0]]

    with contextlib.ExitStack() as st:
        cols = kb.sb(st, "B_cols", [128, C_NCOL], F32)
        kb.dma("sp", cols[:], self.rw_cols[l, :, :])
        muc = kb.sb(st, "B_muc", [128, 9], F32)
        kb.ts(muc[:], cols[:, C_MU0:C_MU0 + 9], -1.0, ALU.mult, 1.0, ALU.add)
        kb.tt(muc[:], muc[:], cols[:, C_MU1:C_MU1 + 9], ALU.subtract)
        omka = kb.sb(st, "B_omka", [128, 2], F32)
        kb.ts(omka[:], cols[:, C_KA:C_KA + 2], -1.0, ALU.mult, 1.0, ALU.add)
        wup = kb.sb(st, "B_wup", [128, 256], BF16)
        aup = kb.sb(st, "B_aup", [128, 256], BF16)
        gup = kb.sb(st, "B_gup", [128, 256], BF16)
        kb.load_cast(wup[:, :], self.rw_wup[l, :, :])
        kb.load_cast(aup[:, :], self.rw_aup[l, :, :])
        kb.load_cast(gup[:, :], self.rw_gup[l, :, :])
        bo = kb.sb(st, "B_bo", [128, 128], BF16)
        kb.load_cast(bo[:, :], self.c_blockones[:, :])
        masks = kb.sb(st, "B_masks", [64, 2, 6, 256], BF16)
        for dd in range(2):
            kb.load_cast(masks[:, dd, :, :].rearrange("p a b -> p (a b)"), self.c_masks[:, dd, :, :].rearrange("p a b -> p (a b)"), parts=64)
        rm = kb.sb(st, "B_rm", [128, 512], F32)
        kb.dma("sp", rm[:], self.c_rm[:, :])

        pT = kb.sb(st, "B_pT", [128, 9, L], BF16)
        with contextlib.ExitStack() as s1:
            uT = kb.sb(s1, "B_uT", [128, 8, L], BF16)
            kb.dma("sp", uT[:], self.uT[:, :, :])
            wa = kb.sb(s1, "B_wa", [128, 8, A_TOT], BF16)
            for c in range(8):
                kb.load_cast(wa[:, c, :], self.w_in[l, c * 128:(c + 1) * 128, 0:A_TOT])
            tmp = [kb.sb(s1, f"B_tmp{i}", [128, 512], F32) for i in range(2)]
            k = 0
            import os
            BIS = os.environ.get("K_BISECT", "")
            for j in range(9 if not BIS else int(BIS)):
                mu0 = cols[:, C_MU0 + j:C_MU0 + j + 1]
                mu1 = cols[:, C_MU1 + j:C_MU1 + j + 1]
                for b in range(5):
                    o0 = 510 * b
                    o1 = min(o0 + 510, L)
                    lo, hi = max(o0 - 1, 0), min(o1 + 1, L)
                    n, no, off = hi - lo, o1 - o0, o0 - lo
                    ps = nps()
                    for c in range(8):
                        kb.mm(ps[:, 0:n], wa[:, c, j * 128:(j + 1) * 128], uT[:, c, lo:hi], start=(c == 0), stop=(c == 7))
                    t = tmp[k % 2]
                    k += 1
                    kb.ts(t[:, 0:no], ps[:, off:off + no], muc[:, j:j + 1], ALU.mult)
                    a = 1 if o0 == 0 else 0
                    kb.stt(t[:, a:no], ps[:, off - 1 + a:off - 1 + no], mu0, t[:, a:no], ALU.mult, ALU.add)
                    nn = no - 1 if o1 == L else no
                    kb.stt(pT[:, j, o0:o0 + nn], ps[:, off + 1:off + 1 + nn], mu1, t[:, 0:nn], ALU.mult, ALU.add)
                    if nn < no:
                        kb.copy(pT[:, j, L - 1:L], t[:, no - 1:no])
        kb.barrier()
        if "pT" in self.debug:
            kb.dma("sp", self.dbg_out["pT"][:, :, :], pT[:, :, :])
            kb.barrier()
        if self.bstop == 1:
            return

        tw = kb.sb(st, "B_tw", [128, L], BF16)
        sg = kb.sb(st, "B_sg", [128, L], BF16)
        kb.act(tw[:], pT[:, 6, :], AF.Tanh)
        kb.act(sg[:], pT[:, 8, :], AF.Sigmoid)
        kk = kb.sb(st, "B_kk", [128, 2, L], BF16)
        gate = kb.sb(st, "B_gate", [128, 2, L], BF16)
        kdsum = kb.sb(st, "B_kdsum", [128, 2, L], BF16)
        o_fm = kb.sb(st, "B_ofm", [128, 2, L], F32)
        kb.memset(o_fm[:], 0.0)
        with contextlib.ExitStack() as s2:
            kkr = kb.sb(s2, "B_kkr", [128, L], BF16)
            sq = kb.sb(s2, "B_sq", [128, L], BF16)
            rn = [kb.sb(s2, f"B_rn{i}", [128, 512], F32) for i in range(2)]
            k = 0
            for j in range(2):
                kb.ts(kkr[:], pT[:, 2 + j, :], cols[:, C_KK + j:C_KK + j + 1], ALU.mult)
                kb.tt(sq[:], kkr[:], kkr[:], ALU.mult)
                for (b0, b1) in NB:
                    n = b1 - b0
                    ps = nps()
                    kb.mm(ps[:, 0:n], bo[:], sq[:, b0:b1])
                    r_ = rn[k % 2]
                    k += 1
                    kb.rsqrt_act(r_[:, 0:n], ps[:, 0:n], 1.0, 1e-30)
                    kb.tt(kk[:, j, b0:b1], kkr[:, b0:b1], r_[:, 0:n], ALU.mult)
                for (b0, b1) in NB:
                    n = b1 - b0
                    ps = nps()
                    kb.mm(ps[:, 0:n], gup[:, j * 128:(j + 1) * 128], sg[:, b0:b1])
                    kb.copy(gate[:, j, b0:b1], ps[:, 0:n], eng="act")
        kb.barrier()
        if self.bstop == 2:
            return

        for d in range(self.bdirs):
            with contextlib.ExitStack() as sd:
                PhiT = kb.sb(sd, "B_PhiT", [128, 33, 2, 128], BF16)
                Hs = kb.sb(sd, "B_Hs", [128, 34, 2, 128], BF16)
                QT = kb.sb(sd, "B_QT", [128, 2, L], BF16)
                WCc = kb.sb(sd, "B_WCc", [128, 2, 33], F32)
                fmn = ["ApT", "BpT", "CpT", "RpT", "BnT"]
                fm = {nm: kb.sb(sd, f"B_{nm}", [128, 2, 512], BF16) for nm in fmn}
                tf = {nm: kb.sb(sd, f"B_t_{nm}", [128, 512], F32) for nm in ["lw", "pre", "xp", "TP", "TX", "ex"]}
                tb = {nm: kb.sb(sd, f"B_t_{nm}", [128, 512], BF16) for nm in ["icl", "kd", "ka", "tb"]}
                NS = 3
                tm_all = [kb.sb(sd, f"B_tm{s}", [64, 4, 256], BF16) for s in range(NS)]
                Vpad = [kb.sb(sd, f"B_Vpad{s}", [64, 4, 128], BF16) for s in range(NS)]
                PP = [[kb.sb(sd, f"B_PP{s}_{q}", [64, 2, 256], BF16) for q in range(2)] for s in range(NS)]
                XX = [[kb.sb(sd, f"B_XX{s}_{q}", [64, 256], BF16) for q in range(2)] for s in range(NS)]
                MP = [kb.sb(sd, f"B_MP{s}", [64, 3, 256], BF16) for s in range(NS)]
                M2Vn = [kb.sb(sd, f"B_M2Vn{s}", [64, 256], BF16) for s in range(NS)]
                GU = [kb.sb(sd, f"B_GU{s}", [64, 2, 256], BF16) for s in range(NS)]
                GUpad = [kb.sb(sd, f"B_GUpad{s}", [64, 2, 4, 128], BF16) for s in range(NS)]
                for s in range(NS):
                    kb.memset(Vpad[s][:], 0.0)
                    kb.memset(GUpad[s][:], 0.0)
                kb.memset(Hs[:, 0 if d == 0 else 33, :, :], 0.0)

                for bi, (b0, b1) in enumerate(PBLK):
                    n = b1 - b0
                    chunks = [0] if bi == 0 else list(range(1 + 8 * (bi - 1), 9 + 8 * (bi - 1)))
                    for j in range(2):
                        lw, pre, xp, TP, TX, ex = (tf[x] for x in ["lw", "pre", "xp", "TP", "TX", "ex"])
                        icl, kd, ka, tbb = (tb[x] for x in ["icl", "kd", "ka", "tb"])
                        ps = nps()
                        kb.mm(ps[:, 0:n], wup[d * 64:(d + 1) * 64, j * 128:(j + 1) * 128], tw[d * 64:(d + 1) * 64, b0:b1])
                        kb.act(lw[:, 0:n], ps[:, 0:n], AF.Sigmoid, bias=cols[:, C_W0 + d * 2 + j:C_W0 + d * 2 + j + 1])
                        kb.ts(lw[:, 0:n], lw[:, 0:n], -A_DECAY_SCALE, ALU.mult)
                        ps = nps()
                        kb.mm(ps[:, 0:n], aup[d * 64:(d + 1) * 64, j * 128:(j + 1) * 128], pT[d * 64:(d + 1) * 64, 7, b0:b1])
                        kb.act(icl[:, 0:n], ps[:, 0:n], AF.Sigmoid, bias=cols[:, C_A0 + d * 2 + j:C_A0 + d * 2 + j + 1])
                        kb.ts(tbb[:, 0:n], icl[:, 0:n], cols[:, C_KA + j:C_KA + j + 1], ALU.mult, omka[:, j:j + 1], ALU.add)
                        kb.tt(kd[:, 0:n], tbb[:, 0:n], pT[:, 2 + j, b0:b1], ALU.mult)
                        kb.tt(ka[:, 0:n], kk[:, j, b0:b1], icl[:, 0:n], ALU.mult)
                        if d == 0:
                            kb.copy(kdsum[:, j, b0:b1], kd[:, 0:n])
                        else:
                            kb.tt(kdsum[:, j, b0:b1], kdsum[:, j, b0:b1], kd[:, 0:n], ALU.add)
                        kb.scan(pre[:, 0:n], rm[:, 0:n], lw[:, 0:n], 0.0, ALU.mult, ALU.add)
                        kb.tt(xp[:, 0:n], pre[:, 0:n], lw[:, 0:n], ALU.subtract)
                        if bi == 0:
                            tot = pre[:, n - 1:n].to_broadcast([128, n])
                            kb.tt(TP[:, 0:n], tot, pre[:, 0:n], ALU.subtract)
                            kb.tt(TX[:, 0:n], tot, xp[:, 0:n], ALU.subtract)
                            kb.act(WCc[:, j, 0:1], pre[:, n - 1:n], AF.Exp)
                        else:
                            v3 = lambda t: t[:, 0:512].rearrange("p (n c) -> p n c", c=64)
                            tot = v3(pre)[:, :, 63:64].to_broadcast([128, 8, 64])
                            kb.tt(v3(TP), tot, v3(pre), ALU.subtract)
                            kb.tt(v3(TX), tot, v3(xp), ALU.subtract)
                            kb.act(WCc[:, j, chunks[0]:chunks[0] + 8], v3(pre)[:, :, 63], AF.Exp)
                        r_ = pT[:, 0 + j, b0:b1]
                        kkj = kk[:, j, b0:b1]
                        if d == 0:
                            srcs = dict(E1=(TP, 1.0), E2=(TP, -1.0), E2w=(TX, -1.0), E3=(pre, 1.0), E3w=(xp, 1.0))
                        else:
                            srcs = dict(E1=(xp, 1.0), E2=(xp, -1.0), E2w=(pre, -1.0), E3=(TX, 1.0), E3w=(TP, 1.0))
                        kb.act(ex[:, 0:n], srcs["E1"][0][:, 0:n], AF.Exp, scale=srcs["E1"][1])
                        kb.tt(fm["ApT"][:, j, 0:n], ka[:, 0:n], ex[:, 0:n], ALU.mult)
                        kb.tt(fm["CpT"][:, j, 0:n], kd[:, 0:n], ex[:, 0:n], ALU.mult)
                        kb.act(ex[:, 0:n], srcs["E2"][0][:, 0:n], AF.Exp, scale=srcs["E2"][1])
                        kb.tt(fm["RpT"][:, j, 0:n], r_, ex[:, 0:n], ALU.mult)
                        kb.act(ex[:, 0:n], srcs["E2w"][0][:, 0:n], AF.Exp, scale=srcs["E2w"][1])
                        kb.tt(fm["BpT"][:, j, 0:n], kkj, ex[:, 0:n], ALU.mult)
                        kb.act(ex[:, 0:n], srcs["E3"][0][:, 0:n], AF.Exp, scale=srcs["E3"][1])
                        kb.tt(QT[:, j, b0:b1], r_, ex[:, 0:n], ALU.mult)
                        kb.act(ex[:, 0:n], srcs["E3w"][0][:, 0:n], AF.Exp, scale=srcs["E3w"][1])
                        kb.stt(fm["BnT"][:, j, 0:n], kkj, -1.0, ex[:, 0:n], ALU.mult, ALU.mult)

                    for g0 in range(0, len(chunks), NS):
                        grp = chunks[g0:g0 + NS]
                        info = []
                        for s, cn in enumerate(grp):
                            c0, c1 = CH[cn]
                            info.append((s, cn, c1 - c0, c0 - b0))
                        hv = lambda ap, C: ap.rearrange("p (h c) -> p h c", h=4)[:, :, 0:C]
                        for (s, cn, C, o) in info:
                            ps = nps().bitcast(BF16)
                            for ti, nm in enumerate(["BnT", "ApT", "CpT", None]):
                                for j in range(2):
                                    src = fm[nm][:, j, o:o + C] if nm else pT[:, 4 + j, b0 + o:b0 + o + C]
                                    kb.tr(ps[0:C, ti * 256 + j * 128:ti * 256 + (j + 1) * 128], src, self.ident_bf[:, :])
                            kb.copy(tm_all[s][0:C, :, :], ps[0:C, :].rearrange("p (a b) -> p a b", a=4), eng="act")
                            for hp in range(2):
                                vsrc = tm_all[s][0:C, 3, :].rearrange("p (j q k) -> p j q k", j=2, q=2)[:, :, hp, :]
                                vdst = Vpad[s][0:C, :, :].rearrange("p (j q) m -> p j q m", q=2)[:, :, hp, hp * 64:(hp + 1) * 64]
                                kb.copy(vdst, vsrc, eng="dve")
                        for (s, cn, C, o) in info:
                            pa, pb, pc = nps(), nps(), nps()
                            for h in (0, 2, 1, 3):
                                j, q = h // 2, (h % 2) * 64
                                if h == 1:
                                    kb.pe_fence()
                                A_ = fm["ApT"][q:q + 64, j, o:o + C]
                                B_ = fm["BpT"][q:q + 64, j, o:o + C]
                                C_ = fm["CpT"][q:q + 64, j, o:o + C]
                                R_ = fm["RpT"][q:q + 64, j, o:o + C]
                                kb.mm(pa[0:C, h * 64:h * 64 + C], A_, B_)
                                kb.mm(pa[0:C, 256 + h * 64:256 + h * 64 + C], B_, A_)
                                kb.mm(pb[0:C, h * 64:h * 64 + C], C_, B_)
                                kb.mm(pb[0:C, 256 + h * 64:256 + h * 64 + C], A_, R_)
                                kb.mm(pc[0:C, h * 64:h * 64 + C], C_, R_)
                            P0 = PP[s][0]
                            kb.tt(hv(P0[0:C, 0, :], C), hv(pa[0:C, 0:256], C), hv(masks[0:C, d, 0, :], C), ALU.mult)
                            kb.tt(hv(P0[0:C, 1, :], C), hv(pa[0:C, 256:512], C), hv(masks[0:C, d, 1, :], C), ALU.mult)
                            kb.tt(hv(XX[s][0][0:C, :], C), hv(P0[0:C, 0, :], C), hv(masks[0:C, d, 5, :], C), ALU.add)
                            kb.tt(hv(MP[s][0:C, 0, :], C), hv(pb[0:C, 0:256], C), hv(masks[0:C, d, 2, :], C), ALU.mult)
                            kb.tt(hv(MP[s][0:C, 1, :], C), hv(pb[0:C, 256:512], C), hv(masks[0:C, d, 3, :], C), ALU.mult)
                            kb.tt(hv(MP[s][0:C, 2, :], C), hv(pc[0:C, 0:256], C), hv(masks[0:C, d, 4, :], C), ALU.mult)
                        nlev = 5 if len(grp) > 1 or grp[0] != 0 else 3
                        for lev in range(nlev):
                            cur, nxt = lev % 2, (lev + 1) % 2
                            last = lev == nlev - 1
                            for (s, cn, C, o) in info:
                                ps = nps()
                                Pc, Pn = PP[s][cur], PP[s][nxt]
                                for h in range(4):
                                    P_ = Pc[0:C, 0, h * 64:h * 64 + C]
                                    PT_ = Pc[0:C, 1, h * 64:h * 64 + C]
                                    kb.mm(ps[0:C, 256 + h * 64:256 + h * 64 + C], P_, PT_)
                                    if not last:
                                        kb.mm(ps[0:C, h * 64:h * 64 + C], PT_, P_)
                                if last:
                                    kb.copy(hv(Pn[0:C, 1, :], C), hv(ps[0:C, 256:512], C), eng="act")
                                else:
                                    kb.copy(Pn[0:C, :, :].rearrange("p a (h c) -> p a h c", h=4)[:, :, :, 0:C],
                                            ps[0:C, :].rearrange("p (a h c) -> p a h c", a=2, h=4)[:, :, :, 0:C], eng="act")
                            for (s, cn, C, o) in info:
                                ps = nps()
                                Pn = PP[s][nxt]
                                Xc, Xn = XX[s][cur], XX[s][nxt]
                                for h in range(4):
                                    sl = slice(h * 64, h * 64 + C)
                                    kb.mm(ps[0:C, sl], self.ident_bf[0:C, 0:C], Xc[0:C, sl], start=True, stop=False)
                                    kb.mm(ps[0:C, sl], Pn[0:C, 1, sl], Xc[0:C, sl], start=False, stop=True)
                                kb.copy(hv(Xn[0:C, :], C), hv(ps[0:C, 0:256], C), eng="dve")
                        xf = nlev % 2
                        for (s, cn, C, o) in info:
                            ps = nps()
                            for h in range(4):
                                sl = slice(h * 64, h * 64 + C)
                                kb.mm(ps[0:C, h * 64:(h + 1) * 64], MP[s][0:C, 0, sl], tm_all[s][0:C, 3, h * 64:(h + 1) * 64])
                            kb.act(M2Vn[s][0:C, :], ps[0:C, 0:256], AF.Copy, scale=-1.0)
                        for (s, cn, C, o) in info:
                            ps = nps()
                            TT_ = XX[s][xf]
                            for h in range(4):
                                sl = slice(h * 64, h * 64 + C)
                                kb.mm(ps[0:C, h * 64:(h + 1) * 64], TT_[0:C, sl], tm_all[s][0:C, 0, h * 64:(h + 1) * 64])
                                kb.mm(ps[0:C, 256 + h * 64:256 + (h + 1) * 64], TT_[0:C, sl], M2Vn[s][0:C, h * 64:(h + 1) * 64])
                            kb.copy(GU[s][0:C, :, :], ps[0:C, :].rearrange("p (a b) -> p a b", a=2), eng="act")
                            for hp in range(2):
                                src = GU[s][0:C, :, :].rearrange("p a (j q k) -> p a j q k", j=2, q=2)[:, :, :, hp, :]
                                dst = GUpad[s][0:C, :, :, :].rearrange("p a (j q) m -> p a j q m", q=2)[:, :, :, hp, hp * 64:(hp + 1) * 64]
                                kb.copy(dst, src, eng="dve")
                        for (s, cn, C, o) in info:
                            ps = nps()
                            for j in range(2):
                                pr = slice(j * 128, (j + 1) * 128)
                                kb.mm(ps[:, j * 128:(j + 1) * 128], GU[s][0:C, 0, pr], tm_all[s][0:C, 1, pr])
                                kb.mm(ps[:, 256 + j * 128:256 + (j + 1) * 128], tm_all[s][0:C, 1, pr], GU[s][0:C, 1, pr], start=True, stop=False)
                                kb.mm(ps[:, 256 + j * 128:256 + (j + 1) * 128], tm_all[s][0:C, 2, pr], tm_all[s][0:C, 3, pr], start=False, stop=True)
                            bo2 = bo[:, :].to_broadcast([128, 128]) if False else None
                            hslot = cn + 1 if d == 0 else cn
                            for j in range(2):
                                kb.tt(PhiT[:, cn, j, :], ps[:, j * 128:(j + 1) * 128], bo[:, :], ALU.mult)
                                kb.stt(PhiT[:, cn, j, :], self.ident_bf[:, :], WCc[:, j, cn:cn + 1], PhiT[:, cn, j, :], ALU.mult, ALU.add)
                                kb.tt(Hs[:, hslot, j, :], ps[:, 256 + j * 128:256 + (j + 1) * 128], bo[:, :], ALU.mult)
                        for (s, cn, C, o) in info:
                            ps = nps()
                            for j in range(2):
                                for q in range(2):
                                    h = 2 * j + q
                                    kb.mm(ps[:, j * 64:j * 64 + C], GUpad[s][0:C, 0, h, :], MP[s][0:C, 1, h * 64:h * 64 + C],
                                          start=(q == 0), stop=(q == 1))
                            c0 = CH[cn][0]
                            qv = QT[:, :, c0:c0 + C]
                            kb.tt(qv, qv, ps[:, 0:128].rearrange("p (j c) -> p j c", j=2)[:, :, 0:C], ALU.add)
                        for (s, cn, C, o) in info:
                            ps = nps()
                            for j in range(2):
                                for q in range(2):
                                    h = 2 * j + q
                                    kb.mm(ps[:, j * 64:j * 64 + C], GUpad[s][0:C, 1, h, :], MP[s][0:C, 1, h * 64:h * 64 + C],
                                          start=(q == 0), stop=False)
                                    kb.mm(ps[:, j * 64:j * 64 + C], Vpad[s][0:C, h, :], MP[s][0:C, 2, h * 64:h * 64 + C],
                                          start=False, stop=(q == 1))
                            c0 = CH[cn][0]
                            ov = o_fm[:, :, c0:c0 + C]
                            kb.tt(ov, ov, ps[:, 0:128].rearrange("p (j c) -> p j c", j=2)[:, :, 0:C], ALU.add)

                order = list(range(33)) if d == 0 else list(range(32, -1, -1))
                for cn in order:
                    hin = cn if d == 0 else cn + 1
                    hout = cn + 1 if d == 0 else cn
                    ps = nps()
                    for j in range(2):
                        kb.mm(ps[:, j * 128:(j + 1) * 128], PhiT[:, cn, j, :], Hs[:, hin, j, :])
                    hv_ = Hs[:, hout, :, :]
                    kb.tt(hv_, hv_, ps[:, 0:256].rearrange("p (j m) -> p j m", j=2), ALU.add)
                for cn in range(33):
                    c0, c1 = CH[cn]
                    C = c1 - c0
                    hin = cn if d == 0 else cn + 1
                    ps = nps()
                    for j in range(2):
                        kb.mm(ps[:, j * 64:j * 64 + C], Hs[:, hin, j, :], QT[:, j, c0:c1])
                    ov = o_fm[:, :, c0:c1]
                    kb.tt(ov, ov, ps[:, 0:128].rearrange("p (j c) -> p j c", j=2)[:, :, 0:C], ALU.add)
            kb.barrier()
        if "ofm" in self.debug:
            kb.dma("sp", self.dbg_out["ofm"][:, :, :], o_fm[:, :, :])
            kb.barrier()
        if self.bstop == 3:
            return

        with contextlib.ExitStack() as s3:
            ob = kb.sb(s3, "B_ob", [128, L], BF16)
            cen = kb.sb(s3, "B_cen", [128, L], F32)
            sqb = kb.sb(s3, "B_sqb", [128, L], BF16)
            rstd = kb.sb(s3, "B_rstd", [128, L], F32)
            prod = kb.sb(s3, "B_prod", [128, L], BF16)
            ya = kb.sb(s3, "B_ya", [128, 2, L], BF16)
            for j in range(2):
                kb.copy(ob[:], o_fm[:, j, :], eng="act")
                for (b0, b1) in NB:
                    n = b1 - b0
                    ps = nps()
                    kb.mm(ps[:, 0:n], bo[:], ob[:, b0:b1])
                    kb.stt(cen[:, b0:b1], ps[:, 0:n], -1.0 / 64, o_fm[:, j, b0:b1], ALU.mult, ALU.add)
                kb.tt(sqb[:], cen[:], cen[:], ALU.mult)
                for (b0, b1) in NB:
                    n = b1 - b0
                    ps = nps()
                    kb.mm(ps[:, 0:n], bo[:], sqb[:, b0:b1])
                    kb.rsqrt_act(rstd[:, b0:b1], ps[:, 0:n], 1.0 / 64, 64e-5)
                kb.tt(cen[:], cen[:], rstd[:], ALU.mult)
                kb.ts(cen[:], cen[:], cols[:, C_GNG + j:C_GNG + j + 1], ALU.mult, cols[:, C_GNB + j:C_GNB + j + 1], ALU.add)
                kb.stt(prod[:], pT[:, 0 + j, :], cols[:, C_RK + j:C_RK + j + 1], kdsum[:, j, :], ALU.mult, ALU.mult)
                for (b0, b1) in NB:
                    n = b1 - b0
                    ps = nps()
                    kb.mm(ps[:, 0:n], bo[:], prod[:, b0:b1])
                    kb.tt(rstd[:, b0:b1], ps[:, 0:n], pT[:, 4 + j, b0:b1], ALU.mult)
                kb.tt(cen[:], cen[:], rstd[:], ALU.add)
                kb.tt(ya[:, j, :], cen[:], gate[:, j, :], ALU.mult)
            kb.dma("sp", self.mixT[:, 0:2, :], ya[:, :, :])
    kb.barrier()


Model.phase_rwkv = phase_rwkv


ROPE_THETA = 10000.0


def _attn_consts():
    c = {}
    pos = np.arange(L, dtype=np.float32)
    inv = (ROPE_THETA ** (-np.arange(32, dtype=np.float32) / 32)).astype(np.float32)
    ang = pos[None, :] * inv[:, None]
    cos = np.cos(ang).astype(np.float32)
    sin = np.sin(ang).astype(np.float32)
    c["c_cos"] = np.ascontiguousarray(np.tile(cos, (4, 1)))
    c["c_sin"] = np.ascontiguousarray(np.tile(sin, (4, 1)))
    P = np.zeros((128, 128), np.float32)
    for blk in range(2):
        o = blk * 64
        for i in range(32):
            P[o + i, o + 32 + i] = -1.0
            P[o + 32 + i, o + i] = 1.0
    c["c_rotperm"] = np.ascontiguousarray(P.T)
    return c


def phase_attn(self, l):
    kb = self.kb
    kb.barrier()
    psb = self.psb
    lam_init = 0.8 - 0.6 * math.exp(-0.3 * l)
    pi = [0]

    def nps():
        pi[0] = (pi[0] + 1) % 8
        return psb[pi[0]]

    with contextlib.ExitStack() as st:
        lamb = kb.sb(st, "C_lamb", [128, 256], F32)
        kb.dma("sp", lamb[:], self.df_lam_bc[l, :, :])
        sub_g = kb.sb(st, "C_subg", [128, 128], F32)
        kb.dma("sp", sub_g[:], self.df_subln_bc[l, :, :])
        kb.ts(sub_g[:], sub_g[:], 1.0 - lam_init, ALU.mult)
        lt = kb.sb(st, "C_lt", [128, 128], F32)
        ee = kb.sb(st, "C_ee", [128, 2], F32)
        kb.tt(lt[:, 0:64], lamb[:, 0:64], lamb[:, 64:128], ALU.mult)
        kb.tt(lt[:, 64:128], lamb[:, 128:192], lamb[:, 192:256], ALU.mult)
        kb.reduce(ee[:, :], lt[:, :].rearrange("p (a b) -> p a b", a=2), ALU.add)
        kb.act(ee[:, :], ee[:, :], AF.Exp)
        nlam = kb.sb(st, "C_nlam", [128, 1], F32)
        kb.tt(nlam[:], ee[:, 1:2], ee[:, 0:1], ALU.subtract)
        kb.ts(nlam[:], nlam[:], -lam_init, ALU.add)
        cos = kb.sb(st, "C_cos", [128, L], F32)
        sin = kb.sb(st, "C_sin", [128, L], F32)
        kb.dma("sp", cos[:], self.c_cos[:, :])
        kb.dma("sp", sin[:], self.c_sin[:, :])
        perm = kb.sb(st, "C_perm", [128, 128], BF16)
        kb.load_cast(perm[:, :], self.c_rotperm[:, :])
        qkT = kb.sb(st, "C_qkT", [128, 8, L], BF16)
        v_aug = kb.sb(st, "C_vaug", [128, 17, 4, 132], BF16)
        kb.memset(v_aug[:, :, :, 128:129], 1.0)
        with contextlib.ExitStack() as s1:
            uT = kb.sb(s1, "C_uT", [128, 8, L], BF16)
            kb.dma("sp", uT[:], self.uT[:, :, :])
            wb = kb.sb(s1, "C_wb", [128, 8, B_TOT], BF16)
            for c in range(8):
                kb.load_cast(wb[:, c, :], self.w_in[l, c * 128:(c + 1) * 128, A_TOT:A_TOT + B_TOT])
            raw = [kb.sb(s1, f"C_raw{i}", [128, 512], BF16) for i in range(2)]
            t1 = [kb.sb(s1, f"C_t1{i}", [128, 512], F32) for i in range(2)]
            t2 = [kb.sb(s1, f"C_t2{i}", [128, 512], F32) for i in range(2)]
            k = 0
            for jj in range(8):
                for (b0, b1) in NB:
                    n = b1 - b0
                    ps = nps()
                    for c in range(8):
                        kb.mm(ps[:, 0:n], wb[:, c, jj * 128:(jj + 1) * 128], uT[:, c, b0:b1], start=(c == 0), stop=(c == 7))
                    r_, a_, b_ = raw[k % 2], t1[k % 2], t2[k % 2]
                    k += 1
                    kb.act(r_[:, 0:n], ps[:, 0:n], AF.Copy, scale=(0.125 if jj < 4 else 1.0))
                    ps2 = nps()
                    kb.mm(ps2[:, 0:n], perm[:, :], r_[:, 0:n])
                    kb.tt(a_[:, 0:n], r_[:, 0:n], cos[:, b0:b1], ALU.mult)
                    kb.tt(b_[:, 0:n], ps2[:, 0:n], sin[:, b0:b1], ALU.mult)
                    kb.tt(qkT[:, jj, b0:b1], a_[:, 0:n], b_[:, 0:n], ALU.add)
            for i, (t0, t1_) in enumerate(TT):
                R = t1_ - t0
                ps = nps()
                for c in range(8):
                    kb.mm(ps[0:R, :], uT[:, c, t0:t1_], wb[:, c, 1024:1536], start=(c == 0), stop=(c == 7))
                kb.copy(v_aug[0:R, i, :, 0:128], ps[0:R, :].rearrange("p (h d) -> p h d", h=4), eng="act")
        kb.barrier()
        yb = kb.sb(st, "C_yb", [128, 4, L], BF16)
        pTs = [kb.sb(st, f"C_pT{i}", [128, 512], BF16) for i in range(3)]
        oc = [[kb.sb(st, f"C_oc{c}_{q}", [128, 132], F32) for q in range(4)] for c in range(2)]
        rr = kb.sb(st, "C_rr", [128, 4], F32)
        od = [kb.sb(st, f"C_od{q}", [128, 128], F32) for q in range(2)]
        ob = [kb.sb(st, f"C_ob{q}", [128, 128], BF16) for q in range(2)]
        junk = kb.sb(st, "C_junk", [128, 128], F32)
        ssq = kb.sb(st, "C_ssq", [128, 2], F32)
        S_banks = [psb[0], psb[1]]
        A_banks = [psb[2], psb[3], psb[4], psb[5]]
        T_banks = [psb[6], psb[7]]
        si = 0
        pk = 0
        dk = 0
        for h in range(4):
            for (b0, b1) in NB:
                nq = b1 - b0
                subs = [(q0, min(q0 + 128, nq)) for q0 in range(0, nq, 128)]
                for c in range(2):
                    kb.pe_fence()
                    for i, (t0, t1_) in enumerate(TT):
                        R = t1_ - t0
                        S = S_banks[si % 2]
                        si += 1
                        kb.mm(S[0:R, 0:nq], qkT[c * 64:(c + 1) * 64, 4 + h, t0:t1_], qkT[c * 64:(c + 1) * 64, h, b0:b1])
                        p_ = pTs[pk % 3]
                        pk += 1
                        kb.act(p_[0:R, 0:nq], S[0:R, 0:nq], AF.Exp)
                        for qi, (q0, q1) in enumerate(subs):
                            kb.mm(A_banks[qi][0:q1 - q0, 0:129], p_[0:R, q0:q1], v_aug[0:R, i, h, 0:129], start=(i == 0), stop=(i == 16))
                    for qi, (q0, q1) in enumerate(subs):
                        kb.copy(oc[c][qi][0:q1 - q0, 0:129], A_banks[qi][0:q1 - q0, 0:129], eng=("act" if qi % 2 else "dve"))
                for qi, (q0, q1) in enumerate(subs):
                    m = q1 - q0
                    o0, o1 = oc[0][qi], oc[1][qi]
                    d_ = od[dk % 2]
                    b_ = ob[dk % 2]
                    dk += 1
                    kb.op("dve", lambda e, o0=o0, m=m: e.reciprocal(out=rr[0:m, 0:1], in_=o0[0:m, 128:129]), [o0], [rr])
                    kb.op("dve", lambda e, o1=o1, m=m: e.reciprocal(out=rr[0:m, 1:2], in_=o1[0:m, 128:129]), [o1], [rr])
                    kb.tt(rr[0:m, 1:2], rr[0:m, 1:2], nlam[0:m, 0:1], ALU.mult)
                    kb.ts(d_[0:m, :], o0[0:m, 0:128], rr[0:m, 0:1], ALU.mult)
                    kb.stt(d_[0:m, :], o1[0:m, 0:128], rr[0:m, 1:2], d_[0:m, :], ALU.mult, ALU.add)
                    kb.act(junk[0:m, :], d_[0:m, :], AF.Square, accum=ssq[0:m, 0:1])
                    kb.rsqrt(ssq[0:m, 0:1], ssq[0:m, 0:1], 1.0 / 128, EPS)
                    kb.stt(b_[0:m, :], d_[0:m, :], ssq[0:m, 0:1], sub_g[0:m, :], ALU.mult, ALU.mult)
                    tb_ = T_banks[dk % 2].bitcast(BF16)
                    kb.tr(tb_[:, 0:m], b_[0:m, :], self.ident_bf[0:m, 0:m])
                    kb.copy(yb[:, h, b0 + q0:b0 + q1], tb_[:, 0:m], eng="act")
        kb.dma("sp", self.mixT[:, 2:6, :], yb[:, :, :])
    kb.barrier()


Model.phase_attn = phase_attn


def phase_outproj(self, l):
    kb = self.kb
    kb.barrier()
    with contextlib.ExitStack() as st:
        mixT = kb.sb(st, "E_mixT", [128, 8, L], BF16)
        kb.dma("sp", mixT[:], self.mixT[:, :, :])
        wo = kb.sb(st, "E_wo", [128, 8, D], BF16)
        for c in range(8):
            kb.load_cast(wo[:, c, :], self.w_out[l, c * 128:(c + 1) * 128, :])
        hts = [kb.sb(st, f"E_h{i}", [128, D], F32) for i in range(3)]
        k = 0
        for i, (t0, t1) in enumerate(TT):
            R = t1 - t0
            ht = hts[i % 3]
            kb.dma("sp", ht[0:R, :], self.h[t0:t1, :])
            for half in range(2):
                ps = self.psb[k % 4]
                k += 1
                for c in range(8):
                    kb.mm(ps[0:R, :], mixT[:, c, t0:t1], wo[:, c, half * 512:(half + 1) * 512], start=(c == 0), stop=(c == 7))
                kb.tt(ht[0:R, half * 512:(half + 1) * 512], ht[0:R, half * 512:(half + 1) * 512], ps[0:R, :], ALU.add)
            kb.dma("sp", self.h[t0:t1, :], ht[0:R, :])
    kb.barrier()


Model.phase_outproj = phase_outproj


def _gla_consts():
    c = {}
    kvm = np.zeros((128, 256), np.float32)
    hm = np.zeros((128, 4), np.float32)
    for h in range(4):
        kvm[h * 32:(h + 1) * 32, h * 64:(h + 1) * 64] = 1.0
        hm[h * 32:(h + 1) * 32, h] = 1.0
    c["c_kvmask"] = kvm
    c["c_headmask"] = hm
    rm = np.ones((128, L), np.float32)
    for (c0, c1) in CH:
        rm[:, c0] = 0.0
    c["c_rmL"] = rm
    return c


def phase_gla(self, l):
    kb = self.kb
    kb.barrier()
    psb = self.psb
    pi = [0]

    def nps():
        pi[0] = (pi[0] + 1) % 8
        return psb[pi[0]]

    with contextlib.ExitStack() as st:
        gcols = kb.sb(st, "D_gcols", [128, 2], F32)
        kb.dma("sp", gcols[:], self.gl_cols[l, :, :])
        kb.ts(gcols[:], gcols[:], -1.0, ALU.mult)
        ng = kb.sb(st, "D_ng", [64, 256], F32)
        kb.dma("sp", ng[:], self.gl_norm_bc[l, :, :])
        kvm = kb.sb(st, "D_kvm", [128, 256], F32)
        kb.dma("sp", kvm[:], self.c_kvmask[:, :])
        hm = kb.sb(st, "D_hm", [128, 4], F32)
        kb.dma("sp", hm[:], self.c_headmask[:, :])
        rmL = kb.sb(st, "D_rmL", [128, L], F32)
        kb.dma("sp", rmL[:], self.c_rmL[:, :])
        amask = kb.sb(st, "D_amask", [64, 2, 256], BF16)
        for d in range(2):
            kb.load_cast(amask[:, d, :], self.c_masks[:, d, 3, :], parts=64)
        gu = [kb.sb(st, f"D_gu{d}", [16, 128], BF16) for d in range(2)]
        for d in range(2):
            kb.load_cast(gu[d][:, :], self.gl_gate_up[l, d, :, :], parts=16)
        qT = kb.sb(st, "D_qT", [128, L], F32)
        kT = kb.sb(st, "D_kT", [128, L], F32)
        adT = [kb.sb(st, f"D_adT{d}", [16, L], BF16) for d in range(2)]
        v_tm = kb.sb(st, "D_vtm", [64, 33, 256], BF16)
        sg_tm = kb.sb(st, "D_sgtm", [64, 33, 256], BF16)
        with contextlib.ExitStack() as s1:
            uT = kb.sb(s1, "D_uT", [128, 8, L], BF16)
            kb.dma("sp", uT[:], self.uT[:, :, :])
            wc = kb.sb(s1, "D_wc", [128, 8, C_TOT], BF16)
            for c in range(8):
                kb.load_cast(wc[:, c, :], self.w_in[l, c * 128:(c + 1) * 128, A_TOT + B_TOT:IN_COLS])
            for (dst, col0, ncol, scale) in ((qT, 0, 128, 32 ** -0.5), (kT, 128, 128, 1.0), (adT[0], 768, 16, 1.0), (adT[1], 784, 16, 1.0)):
                for (b0, b1) in NB:
                    n = b1 - b0
                    ps = nps()
                    for c in range(8):
                        kb.mm(ps[0:ncol, 0:n], wc[:, c, col0:col0 + ncol], uT[:, c, b0:b1], start=(c == 0), stop=(c == 7))
                    kb.act(dst[0:ncol, b0:b1], ps[0:ncol, 0:n], AF.Copy, scale=scale)
            for n_, (c0, c1) in enumerate(CH):
                C = c1 - c0
                ps = nps()
                for c in range(8):
                    kb.mm(ps[0:C, :], uT[:, c, c0:c1], wc[:, c, 256:768], start=(c == 0), stop=(c == 7))
                kb.copy(v_tm[0:C, n_, :], ps[0:C, 0:256], eng="dve")
                kb.act(sg_tm[0:C, n_, :], ps[0:C, 256:512], AF.Silu)
        kb.barrier()
        qd = [kb.sb(st, f"D_qd{d}", [128, L], BF16) for d in range(2)]
        kim = [[kb.sb(st, f"D_kim{d}_{h}", [128, L], BF16) for h in range(4)] for d in range(2)]
        Ss = [kb.sb(st, f"D_Ss{d}", [128, 34, 256], BF16) for d in range(2)]
        with contextlib.ExitStack() as s2:
            lw = kb.sb(s2, "D_lw", [128, L], F32)
            pre = kb.sb(s2, "D_pre", [128, L], F32)
            a1 = kb.sb(s2, "D_a1", [128, L], F32)
            ex = kb.sb(s2, "D_ex", [128, L], F32)
            ki = kb.sb(s2, "D_ki", [128, L], BF16)
            ke = kb.sb(s2, "D_ke", [128, L], BF16)
            dec = kb.sb(s2, "D_dec", [128, 33], F32)
            ketm = [kb.sb(s2, f"D_ketm{i}", [64, 128], BF16) for i in range(2)]
            for d in range(2):
                for (b0, b1) in NB:
                    n = b1 - b0
                    ps = nps()
                    kb.mm(ps[:, 0:n], gu[d][:, :], adT[d][:, b0:b1])
                    kb.act(lw[:, b0:b1], ps[:, 0:n], AF.Exp, scale=-1.0, bias=gcols[:, d:d + 1])
                kb.act(lw[:], lw[:], AF.Ln, bias=1.0)
                kb.ts(lw[:], lw[:], -1.0 / 16.0, ALU.mult)
                kb.scan(pre[:], rmL[:], lw[:], 0.0, ALU.mult, ALU.add)
                v3 = lambda t: t[:, 16:L].rearrange("p (n c) -> p n c", c=64)
                tot3 = v3(pre)[:, :, 63:64].to_broadcast([128, 32, 64])
                tot0 = pre[:, 15:16].to_broadcast([128, 16])
                kb.act(dec[:, 0:1], pre[:, 15:16], AF.Exp)
                kb.act(dec[:, 1:33], v3(pre)[:, :, 63], AF.Exp)
                if d == 0:
                    kb.act(ex[:], pre[:], AF.Exp)
                    kb.tt(qd[d][:], qT[:], ex[:], ALU.mult)
                    kb.act(ex[:], pre[:], AF.Exp, scale=-1.0)
                    kb.tt(ki[:], kT[:], ex[:], ALU.mult)
                    kb.tt(v3(a1), tot3, v3(pre), ALU.subtract)
                    kb.tt(a1[:, 0:16], tot0, pre[:, 0:16], ALU.subtract)
                    kb.act(ex[:], a1[:], AF.Exp)
                    kb.tt(ke[:], kT[:], ex[:], ALU.mult)
                else:
                    kb.tt(pre[:], pre[:], lw[:], ALU.subtract)
                    kb.act(ex[:], pre[:], AF.Exp)
                    kb.tt(ke[:], kT[:], ex[:], ALU.mult)
                    kb.tt(a1[:], pre[:], lw[:], ALU.add)
                    a3 = v3(a1)[:, :, 63:64].to_broadcast([128, 32, 64])
                    a0 = a1[:, 15:16].to_broadcast([128, 16])
                    kb.tt(v3(lw), a3, v3(pre), ALU.subtract)
                    kb.tt(lw[:, 0:16], a0, pre[:, 0:16], ALU.subtract)
                    kb.act(ex[:], lw[:], AF.Exp)
                    kb.tt(qd[d][:], qT[:], ex[:], ALU.mult)
                    kb.act(ex[:], lw[:], AF.Exp, scale=-1.0)
                    kb.tt(ki[:], kT[:], ex[:], ALU.mult)
                for h in range(4):
                    kb.ts(kim[d][h][:], ki[:], hm[:, h:h + 1], ALU.mult)
                for n_, (c0, c1) in enumerate(CH):
                    C = c1 - c0
                    tb_ = nps().bitcast(BF16)
                    kb.tr(tb_[0:C, 0:128], ke[:, c0:c1], self.ident_bf[:, :])
                    kt_ = ketm[n_ % 2]
                    kb.copy(kt_[0:C, :], tb_[0:C, 0:128], eng="act")
                    ps = nps()
                    kb.mm(ps[:, 0:256], kt_[0:C, :], v_tm[0:C, n_, :])
                    kb.tt(Ss[d][:, (n_ + 1 if d == 0 else n_), :], ps[:, 0:256], kvm[:, :], ALU.mult)
                if d == 0:
                    kb.memset(Ss[d][:, 0, :], 0.0)
                    for n_ in range(33):
                        kb.stt(Ss[d][:, n_ + 1, :], Ss[d][:, n_, :], dec[:, n_:n_ + 1], Ss[d][:, n_ + 1, :], ALU.mult, ALU.add)
                else:
                    kb.memset(Ss[d][:, 33, :], 0.0)
                    for n_ in range(32, -1, -1):
                        kb.stt(Ss[d][:, n_, :], Ss[d][:, n_ + 1, :], dec[:, n_:n_ + 1], Ss[d][:, n_, :], ALU.mult, ALU.add)
        kb.barrier()
        yc = kb.sb(st, "D_yc", [128, 2, L], BF16)
        att = [[kb.sb(st, f"D_att{i}_{d}", [64, 4, 64], BF16) for d in range(2)] for i in range(2)]
        osb = [kb.sb(st, f"D_osb{i}", [64, 256], F32) for i in range(2)]
        sq = kb.sb(st, "D_sq", [64, 256], F32)
        ssq = [kb.sb(st, f"D_ssq{i}", [64, 4], F32) for i in range(2)]
        ycb = [kb.sb(st, f"D_ycb{i}", [64, 256], BF16) for i in range(2)]
        for n_, (c0, c1) in enumerate(CH):
            C = c1 - c0
            at = att[n_ % 2]
            for d in range(2):
                ps = nps()
                for h in range(4):
                    kb.mm(ps[0:C, h * 64:h * 64 + C], kim[d][h][:, c0:c1], qd[d][:, c0:c1])
                kb.tt(at[d][0:C, :, 0:C], ps[0:C, 0:256].rearrange("p (h c) -> p h c", h=4)[:, :, 0:C],
                      amask[0:C, d, :].rearrange("p (h c) -> p h c", h=4)[:, :, 0:C], ALU.mult)
            po = nps()
            for d in range(2):
                hin = n_ if d == 0 else n_ + 1
                kb.mm(po[0:C, 0:256], qd[d][:, c0:c1], Ss[d][:, hin, :], start=(d == 0), stop=False)
                for h in range(4):
                    kb.mm(po[0:C, h * 64:(h + 1) * 64], at[d][0:C, h, 0:C], v_tm[0:C, n_, h * 64:(h + 1) * 64],
                          start=False, stop=(d == 1 and h == 3))
            o_ = osb[n_ % 2]
            s_ = ssq[n_ % 2]
            y_ = ycb[n_ % 2]
            kb.copy(o_[0:C, :], po[0:C, 0:256], eng="act")
            kb.tt(sq[0:C, :], o_[0:C, :], o_[0:C, :], ALU.mult)
            kb.reduce(s_[0:C, :], sq[0:C, :].rearrange("p (h d) -> p h d", h=4), ALU.add)
            kb.rsqrt(s_[0:C, :], s_[0:C, :], 1.0 / 64, EPS)
            for h in range(4):
                hs = slice(h * 64, (h + 1) * 64)
                kb.stt(o_[0:C, hs], o_[0:C, hs], s_[0:C, h:h + 1], ng[0:C, hs], ALU.mult, ALU.mult)
            kb.tt(y_[0:C, :], o_[0:C, :], sg_tm[0:C, n_, :], ALU.mult)
            tb_ = nps().bitcast(BF16)
            for j in range(2):
                kb.tr(tb_[:, j * 64:j * 64 + C], y_[0:C, j * 128:(j + 1) * 128], self.ident_bf[0:C, 0:C])
            kb.copy(yc[:, :, c0:c1], tb_[:, 0:128].rearrange("p (j c) -> p j c", j=2)[:, :, 0:C], eng="act")
        kb.dma("sp", self.mixT[:, 6:8, :], yc[:, :, :])
    kb.barrier()


Model.phase_gla = phase_gla


def _moe_consts():
    c = {}
    c["c_iota_c"] = np.ascontiguousarray(np.broadcast_to(np.arange(CAP, dtype=np.float32)[None, :], (128, CAP)))
    ip = np.zeros((128, 3), np.float32)
    for cc in range(3):
        ip[:, cc] = np.arange(128) + 128 * cc
    c["c_iota_p"] = ip
    oh = np.zeros((16, 16, 128), np.float32)
    for e in range(16):
        oh[e, e, :] = 1.0
    c["c_onehot"] = oh
    return c


def phase_moe(self, l):
    kb = self.kb
    kb.barrier()
    psb = self.psb
    pi = [0]

    def nps():
        pi[0] = (pi[0] + 1) % 8
        return psb[pi[0]]

    with contextlib.ExitStack() as st:
        hacc = kb.sb(st, "G_hacc", [128, 17, D], F32)
        u2 = kb.sb(st, "G_u2", [128, 17, D], BF16)
        posm_tok = kb.sb(st, "G_posm", [128, 17, 16], F32)
        gate_tok = kb.sb(st, "G_gate", [128, 17, 16], F32)
        iota_c = kb.sb(st, "G_iotac", [128, CAP], F32)
        kb.dma("sp", iota_c[:], self.c_iota_c[:, :])
        iota_p = kb.sb(st, "G_iotap", [128, 3], F32)
        kb.dma("sp", iota_p[:], self.c_iota_p[:, :])
        with contextlib.ExitStack() as s1:
            g_bc = kb.sb(s1, "F_g", [128, D], F32)
            kb.dma("sp", g_bc[:], self.norm2_bc[l, :, :])
            rt = kb.sb(s1, "F_rt", [128, 8, 16], F32)
            kb.dma("sp", rt[:], self.router[l].rearrange("(c p) e -> p c e", p=128))
            un = [kb.sb(s1, f"F_un{i}", [128, D], F32) for i in range(2)]
            uTf = [kb.sb(s1, f"F_uTf{i}", [128, 8, 128], F32) for i in range(2)]
            junk = kb.sb(s1, "F_junk", [128, D], BF16)
            ssq = [kb.sb(s1, f"F_ssq{i}", [128, 1], F32) for i in range(2)]
            ex = [kb.sb(s1, f"F_ex{i}", [128, 16], F32) for i in range(2)]
            esum = [kb.sb(s1, f"F_es{i}", [128, 1], F32) for i in range(2)]
            aff = kb.sb(s1, "F_aff", [128, 17, 16], F32)
            affT = kb.sb(s1, "F_affT", [16, L], F32)
            posmT = kb.sb(s1, "F_posmT", [16, L], F32)
            for i, (t0, t1) in enumerate(TT):
                R = t1 - t0
                kb.dma("sp", hacc[0:R, i, :], self.h[t0:t1, :])
                u_ = un[i % 2]
                s_ = ssq[i % 2]
                kb.act(junk[0:R, :], hacc[0:R, i, :], AF.Square, accum=s_[0:R, :])
                kb.rsqrt(s_[0:R, :], s_[0:R, :], 1.0 / D, EPS)
                kb.stt(u_[0:R, :], hacc[0:R, i, :], s_[0:R, 0:1], g_bc[0:R, :], ALU.mult, ALU.mult)
                kb.copy(u2[0:R, i, :], u_[0:R, :], eng="act")
                pa, pb = nps(), nps()
                for c in range(8):
                    pp = pa if c < 4 else pb
                    kb.tr(pp[:, (c % 4) * 128:(c % 4) * 128 + R], u_[0:R, c * 128:(c + 1) * 128], self.ident_f[0:R, 0:R])
                tf_ = uTf[i % 2]
                kb.copy(tf_[:, 0:4, 0:R], pa[:, :].rearrange("p (c t) -> p c t", c=4)[:, :, 0:R], eng="dve")
                kb.copy(tf_[:, 4:8, 0:R], pb[:, :].rearrange("p (c t) -> p c t", c=4)[:, :, 0:R], eng="act")
                pl = nps()
                for c in range(8):
                    kb.mm(pl[0:R, 0:16], tf_[:, c, 0:R], rt[:, c, :], start=(c == 0), stop=(c == 7))
                e_ = ex[i % 2]
                es_ = esum[i % 2]
                kb.act(e_[0:R, :], pl[0:R, 0:16], AF.Exp, accum=es_[0:R, :])
                kb.op("dve", lambda e, es_=es_, R=R: e.reciprocal(out=es_[0:R, :], in_=es_[0:R, :]), [es_], [es_])
                kb.ts(aff[0:R, i, :], e_[0:R, :], es_[0:R, 0:1], ALU.mult)
                pt = nps()
                kb.tr(pt[0:16, 0:R], aff[0:R, i, :], self.ident_f[0:R, 0:R])
                kb.copy(affT[:, t0:t1], pt[0:16, 0:R], eng="act")
            work = kb.sb(s1, "F_work", [16, L], F32)
            m8 = kb.sb(s1, "F_m8", [16, 8], F32)
            kb.copy(work[:], affT[:], eng="dve")
            nit = (CAP + 7) // 8
            for it in range(nit):
                kb.op("dve", lambda e: e.max(out=m8[:, :], in_=work[:, :]), [work], [m8])
                rem = CAP - it * 8
                if rem < 8:
                    kb.memset(m8[:, rem:8], 0.0)
                kb.op("dve", lambda e: e.match_replace(out=work[:, :], in_to_replace=m8[:, :], in_values=work[:, :], imm_value=0.0), [m8, work], [work])
            gatesT = kb.sb(s1, "F_gatesT", [16, L], F32)
            maskT = kb.sb(s1, "F_maskT", [16, L], F32)
            ones = kb.sb(s1, "F_ones", [16, L], F32)
            kb.memset(ones[:], 1.0)
            kb.tt(gatesT[:], affT[:], work[:], ALU.subtract)
            kb.ts(maskT[:], gatesT[:], 0.0, ALU.is_gt)
            kb.scan(posmT[:], ones[:], maskT[:], 0.0, ALU.mult, ALU.add)
            kb.tt(posmT[:], posmT[:], maskT[:], ALU.mult)
            kb.ts(posmT[:], posmT[:], -1.0, ALU.add)
            for i, (t0, t1) in enumerate(TT):
                R = t1 - t0
                pt = nps()
                kb.tr(pt[0:R, 0:16], posmT[:, t0:t1], self.ident_f[0:16, 0:16])
                kb.tr(pt[0:R, 16:32], gatesT[:, t0:t1], self.ident_f[0:16, 0:16])
                kb.copy(posm_tok[0:R, i, :], pt[0:R, 0:16], eng="act")
                kb.copy(gate_tok[0:R, i, :], pt[0:R, 16:32], eng="dve")
        kb.barrier()
        if "route" in self.debug:
            kb.dma("sp", self.dbg_out["route"][0, :, :, :], posm_tok[:, :, :])
            kb.dma("sp", self.dbg_out["route"][1, :, :, :], gate_tok[:, :, :])
            kb.barrier()
        w1 = kb.sb(st, "G_w1", [128, 8, FF], BF16)
        w3 = kb.sb(st, "G_w3", [128, 8, FF], BF16)
        w2 = kb.sb(st, "G_w2", [128, 11, D], BF16)
        Sel = kb.sb(st, "G_Sel", [128, 17, CAP], BF16)
        SelTg = kb.sb(st, "G_SelTg", [128, 3, 1024], BF16)
        xy = kb.sb(st, "G_xy", [128, 3 * D], BF16)
        xT = xy[:, 0:8 * CAP].rearrange("p (c n) -> p c n", c=8)
        yb = xy[:, :].rearrange("p (c d) -> p c d", c=3)
        hT = kb.sb(st, "G_hT", [128, 11, CAP], BF16)
        sil = [kb.sb(st, f"G_sil{i}", [128, CAP], F32) for i in range(1)]
        CC = [(0, 86), (86, 172), (172, 258)]
        ne = self.n_experts
        for e in range(ne):
            for c in range(8):
                kb.load_cast(w1[:, c, :], self.e_w1[l, e, c * 128:(c + 1) * 128, :])
                kb.load_cast(w3[:, c, :], self.e_w3[l, e, c * 128:(c + 1) * 128, :])
            for fc in range(11):
                kb.load_cast(w2[:, fc, :], self.e_w2[l, e, fc * 128:(fc + 1) * 128, :])
            for i, (t0, t1) in enumerate(TT):
                R = t1 - t0
                kb.ts(Sel[0:R, i, :], iota_c[0:R, :], posm_tok[0:R, i, e:e + 1], ALU.is_equal)
            for c in range(8):
                ps = nps()
                for i, (t0, t1) in enumerate(TT):
                    R = t1 - t0
                    kb.mm(ps[:, 0:CAP], u2[0:R, i, c * 128:(c + 1) * 128], Sel[0:R, i, :], start=(i == 0), stop=(i == 16))
                kb.copy(xT[:, c, :], ps[:, 0:CAP], eng="act")
            for fc in range(11):
                p1, p3 = nps(), nps()
                for c in range(8):
                    kb.mm(p1[:, 0:CAP], w1[:, c, fc * 128:(fc + 1) * 128], xT[:, c, :], start=(c == 0), stop=(c == 7))
                for c in range(8):
                    kb.mm(p3[:, 0:CAP], w3[:, c, fc * 128:(fc + 1) * 128], xT[:, c, :], start=(c == 0), stop=(c == 7))
                s_ = sil[0]
                kb.act(s_[:, :], p1[:, 0:CAP], AF.Silu)
                kb.tt(hT[:, fc, :], s_[:, :], p3[:, 0:CAP], ALU.mult)
            for cc, (a0, a1) in enumerate(CC):
                m = a1 - a0
                for half in range(2):
                    ps = nps()
                    for fc in range(11):
                        kb.mm(ps[0:m, :], hT[:, fc, a0:a1], w2[:, fc, half * 512:(half + 1) * 512], start=(fc == 0), stop=(fc == 10))
                    kb.copy(yb[0:m, cc, half * 512:(half + 1) * 512], ps[0:m, :], eng="act")
            for g0 in range(0, 17, 8):
                tiles = list(range(g0, min(g0 + 8, 17)))
                for cc, (a0, a1) in enumerate(CC):
                    m = a1 - a0
                    tb_ = nps().bitcast(BF16)
                    for k_, i in enumerate(tiles):
                        t0, t1 = TT[i]
                        R = t1 - t0
                        kb.tr(tb_[0:m, k_ * 128:k_ * 128 + R], Sel[0:R, i, a0:a1], self.ident_bf[0:R, 0:R])
                    eg = "act" if cc % 2 else "dve"
                    if g0 == 0:
                        kb.copy(SelTg[0:m, cc, 0:16], tb_[0:m, 0:16], eng=eg)
                        kb.copy(SelTg[0:m, cc, 128:1024], tb_[0:m, 128:1024], eng=eg)
                    else:
                        kb.copy(SelTg[0:m, cc, 0:len(tiles) * 128], tb_[0:m, 0:len(tiles) * 128], eng=eg)
                for k_, i in enumerate(tiles):
                    t0, t1 = TT[i]
                    R = t1 - t0
                    for half in range(2):
                        ps = nps()
                        for cc, (a0, a1) in enumerate(CC):
                            m = a1 - a0
                            kb.mm(ps[0:R, :], SelTg[0:m, cc, k_ * 128:k_ * 128 + R], yb[0:m, cc, half * 512:(half + 1) * 512], start=(cc == 0), stop=(cc == 2))
                        hv = hacc[0:R, i, half * 512:(half + 1) * 512]
                        kb.stt(hv, ps[0:R, :], gate_tok[0:R, i, e:e + 1], hv, ALU.mult, ALU.add)
        for i, (t0, t1) in enumerate(TT):
            R = t1 - t0
            kb.dma("sp", self.h[t0:t1, :], hacc[0:R, i, :])
    kb.barrier()


Model.phase_moe = phase_moe


def phase_final(self):
    kb = self.kb
    kb.barrier()
    with contextlib.ExitStack() as st:
        g_bc = kb.sb(st, "Z_g", [128, D], F32)
        kb.dma("sp", g_bc[:], self.final_bc[:, :])
        hts = [kb.sb(st, f"Z_h{i}", [128, D], F32) for i in range(3)]
        junk = kb.sb(st, "Z_junk", [128, D], BF16)
        ssqs = [kb.sb(st, f"Z_ssq{i}", [128, 1], F32) for i in range(3)]
        for i, (t0, t1) in enumerate(TT):
            if i == 0:
                continue
            ht, ssq = hts[i % 3], ssqs[i % 3]
            kb.dma("sp", ht[:, :], self.h[t0:t1, :])
            kb.act(junk[:, :], ht[:, :], AF.Square, accum=ssq[:, :])
            kb.rsqrt(ssq[:, :], ssq[:, :], 1.0 / D, EPS)
            kb.stt(ht[:, :], ht[:, :], ssq[:, 0:1], g_bc[:, :], ALU.mult, ALU.mult)
            kb.dma("sp", self.out[t0 - NM:t1 - NM, :], ht[:, :])
    kb.barrier()


Model.phase_final = phase_final


_CACHE = {}


def kernel(**inputs):
    inputs = {k: np.asarray(v) for k, v in inputs.items()}
    if "model" not in _CACHE:
        _ph = _os.environ.get("K_PHASES", "")
        _CACHE["model"] = Model(phases=_ph) if _ph else Model()
    m = _CACHE["model"]
    shared = _prep_shared(inputs)
    B = inputs["x"].shape[0]
    in_maps = []
    for b in range(B):
        im = _prep_inputs(inputs, b, shared)
        in_maps.append({k: v for k, v in im.items() if k in m.kb.inputs})
    res = run_bass_kernel_spmd(m.kb.nc, in_maps, core_ids=list(range(B)))
    out = np.stack([np.asarray(r["out"]) for r in res.results], axis=0)
    return out.astype(np.float32)
```
